# Optimizing a Trainium2 kernel written in Bass

```python
import jax, jax.numpy as jnp
from jax import lax
import numpy as np

D_MODEL = 1024
BATCH = 8
SEQ = 4096
DEPTH = 2

CHUNK = 64
HEAD_DIM = 64
DSA_HEADS = 8
IDX_HEADS = 8
IDX_DIM = 64
DSA_TOPK_MAX = 256
DSA_BLOCK = 64
FOX_HEADS = 8
FOX_BLOCK = 128
ROPE_THETA = 10000.0
D_FF = 3584
N_EXPERTS = 8
TOP_K = 2
MOE_BLOCK = 256
LN_EPS = 1e-5
DSA_WIDTH = DSA_HEADS * HEAD_DIM
FOX_WIDTH = FOX_HEADS * HEAD_DIM
PROJ_SIZES = (DSA_WIDTH, DSA_WIDTH, DSA_WIDTH, IDX_HEADS * IDX_DIM, IDX_DIM, IDX_HEADS,
              FOX_WIDTH, FOX_WIDTH, FOX_WIDTH, FOX_HEADS, D_MODEL, D_MODEL)
PROJ_TOTAL = sum(PROJ_SIZES)

kernel_name = "hybrid_dsa_fox_gated_moe_deepnorm"


def layer_norm(x, g, b):
    xf = x.astype(jnp.float32)
    mu = jnp.mean(xf, axis=-1, keepdims=True)
    xc = xf - mu
    var = jnp.mean(xc * xc, axis=-1, keepdims=True)
    y = xc * lax.rsqrt(var + LN_EPS)
    return (y * g.astype(jnp.float32) + b.astype(jnp.float32)).astype(x.dtype)


def rotary_tables(positions, dim):
    inv_freq = ROPE_THETA ** (-jnp.arange(0, dim, 2, dtype=jnp.float32) / dim)
    ang = positions.astype(jnp.float32)[..., None] * inv_freq
    return jnp.cos(ang), jnp.sin(ang)


def apply_rotary(x, cos, sin):
    c = cos[:, :, None, :].astype(x.dtype)
    s = sin[:, :, None, :].astype(x.dtype)
    x1, x2 = jnp.split(x, 2, axis=-1)
    return jnp.concatenate([x1 * c - x2 * s, x2 * c + x1 * s], axis=-1)


def split_projection(proj):
    points = [int(p) for p in np.cumsum(PROJ_SIZES)[:-1]]
    return jnp.split(proj, points, axis=-1)


def to_blocks(a, block):
    B, T = a.shape[0], a.shape[1]
    return a.reshape((B, T // block, block) + a.shape[2:]).swapaxes(0, 1)


def from_blocks(a):
    a = a.swapaxes(0, 1)
    return a.reshape((a.shape[0], a.shape[1] * a.shape[2]) + a.shape[3:])


def dsa_attention(q, k, v, iq, ik, iw, top_k):
    B, T, H, dh = q.shape
    nb = T // DSA_BLOCK
    key_pos = jnp.arange(T)
    ik_f = ik.astype(jnp.float32)

    def body(args):
        qb, iqb, iwb, blk = args
        t = blk * DSA_BLOCK + jnp.arange(DSA_BLOCK)
        limit = (t // CHUNK + 1) * CHUNK
        admissible = key_pos[None, :] < limit[:, None]
        head_sc = jnp.einsum('bqhd,bsd->bqhs', iqb.astype(jnp.float32), ik_f) * (IDX_DIM ** -0.5)
        score = jnp.einsum('bqhs,bqh->bqs', jax.nn.relu(head_sc), iwb.astype(jnp.float32))
        score = jnp.where(admissible[None], score, -jnp.inf)
        _, sel = lax.top_k(score, top_k)
        valid = sel < limit[None, :, None]
        k_sel = jax.vmap(lambda kb, ib: kb[ib])(k, sel)
        v_sel = jax.vmap(lambda vb, ib: vb[ib])(v, sel)
        logits = jnp.einsum('bqhd,bqkhd->bhqk', qb, k_sel).astype(jnp.float32) * (dh ** -0.5)
        logits = jnp.where(valid[:, None], logits, -jnp.inf)
        p = jax.nn.softmax(logits, axis=-1).astype(v.dtype)
        return jnp.einsum('bhqk,bqkhd->bqhd', p, v_sel)

    out = lax.map(body, (to_blocks(q, DSA_BLOCK), to_blocks(iq, DSA_BLOCK),
                         to_blocks(iw, DSA_BLOCK), jnp.arange(nb)))
    return from_blocks(out).reshape(B, T, H * dh)


def forgetting_attention(q, k, v, log_f):
    B, T, H, dh = q.shape
    nb = T // FOX_BLOCK
    key_pos = jnp.arange(T)
    c = jnp.cumsum(log_f, axis=1)
    c_k = c.transpose(0, 2, 1)

    def body(args):
        qb, cqb, blk = args
        t = blk * FOX_BLOCK + jnp.arange(FOX_BLOCK)
        causal = key_pos[None, :] <= t[:, None]
        decay = cqb.transpose(0, 2, 1)[..., None] - c_k[:, :, None, :]
        logits = jnp.einsum('bqhd,bshd->bhqs', qb, k).astype(jnp.float32) * (dh ** -0.5) + decay
        logits = jnp.where(causal, logits, -jnp.inf)
        p = jax.nn.softmax(logits, axis=-1).astype(v.dtype)
        return jnp.einsum('bhqs,bshd->bqhd', p, v)

    out = lax.map(body, (to_blocks(q, FOX_BLOCK), to_blocks(c, FOX_BLOCK), jnp.arange(nb)))
    return from_blocks(out).reshape(B, T, H * dh)


def hybrid_mixer(h, cos, sin, w_in, b_forget, w_branch_a, w_branch_b, w_out, top_k):
    B, T, _ = h.shape
    proj = h @ w_in
    dq, dk, dv, iq, ik, iw, fq, fk, fv, fl, ga, gb = split_projection(proj)
    dq = apply_rotary(dq.reshape(B, T, DSA_HEADS, HEAD_DIM), cos, sin)
    dk = apply_rotary(dk.reshape(B, T, DSA_HEADS, HEAD_DIM), cos, sin)
    dv = dv.reshape(B, T, DSA_HEADS, HEAD_DIM)
    iq = apply_rotary(iq.reshape(B, T, IDX_HEADS, IDX_DIM), cos, sin)
    ik = apply_rotary(ik.reshape(B, T, 1, IDX_DIM), cos, sin)[:, :, 0, :]
    iw = iw * (IDX_HEADS ** -0.5)
    o_a = dsa_attention(dq, dk, dv, iq, ik, iw, top_k)

    fq = fq.reshape(B, T, FOX_HEADS, HEAD_DIM)
    fk = fk.reshape(B, T, FOX_HEADS, HEAD_DIM)
    fv = fv.reshape(B, T, FOX_HEADS, HEAD_DIM)
    log_f = jax.nn.log_sigmoid((fl + b_forget).astype(jnp.float32))
    o_b = forgetting_attention(fq, fk, fv, log_f)

    merged = jax.nn.sigmoid(ga) * (o_a @ w_branch_a) + jax.nn.sigmoid(gb) * (o_b @ w_branch_b)
    return merged @ w_out


def dense_swiglu(h, w_gate, w_up, w_down):
    return (jax.nn.silu(h @ w_gate) * (h @ w_up)) @ w_down


def moe_swiglu(h, router, w_gate, w_up, w_down):
    B, T, D = h.shape
    N = B * T
    hf = h.reshape(N, D)
    logits = (hf @ router).astype(jnp.float32)
    top_vals, top_idx = lax.top_k(logits, TOP_K)
    gates = jax.nn.softmax(top_vals, axis=-1).astype(h.dtype)
    A = N * TOP_K
    e_flat = top_idx.reshape(A)
    tok_flat = jnp.repeat(jnp.arange(N, dtype=jnp.int32), TOP_K)
    g_flat = gates.reshape(A)
    order = jnp.argsort(e_flat)
    e_sorted = e_flat[order]
    tok_sorted = tok_flat[order]
    g_sorted = g_flat[order]
    counts = jnp.bincount(e_flat, length=N_EXPERTS).astype(jnp.int32)
    start = jnp.cumsum(counts) - counts
    padded = ((counts + MOE_BLOCK - 1) // MOE_BLOCK) * MOE_BLOCK
    pad_end = jnp.cumsum(padded)
    pad_start = pad_end - padded
    rank = jnp.arange(A, dtype=jnp.int32) - start[e_sorted]
    slot = pad_start[e_sorted] + rank
    P = A + N_EXPERTS * MOE_BLOCK
    slot_tok = jnp.zeros((P,), jnp.int32).at[slot].set(tok_sorted)
    slot_gate = jnp.zeros((P,), h.dtype).at[slot].set(g_sorted)
    n_blocks = P // MOE_BLOCK
    block_expert = jnp.clip(
        jnp.searchsorted(pad_end, jnp.arange(n_blocks, dtype=jnp.int32) * MOE_BLOCK, side='right'),
        0, N_EXPERTS - 1)

    def run_block(args):
        toks, gts, e = args
        xb = hf[toks]
        hid = jax.nn.silu(xb @ w_gate[e]) * (xb @ w_up[e])
        return (hid @ w_down[e]) * gts[:, None]

    yb = lax.map(run_block, (slot_tok.reshape(n_blocks, MOE_BLOCK),
                             slot_gate.reshape(n_blocks, MOE_BLOCK), block_expert))
    out = jnp.zeros((N, D), h.dtype).at[slot_tok].add(yb.reshape(P, D))
    return out.reshape(B, T, D)


def setup_inputs(seed: int = 0) -> dict:
    key = jax.random.key(seed)
    ks = jax.random.split(key, 20)
    n_dense = (DEPTH + 1) // 2
    n_moe = DEPTH // 2
    beta = (8.0 * DEPTH) ** -0.25
    nrm = jax.random.normal
    x = nrm(ks[0], (BATCH, SEQ, D_MODEL), jnp.float32)
    positions = jnp.tile(jnp.arange(SEQ, dtype=jnp.int32)[None, :], (BATCH, 1))
    w_in = nrm(ks[1], (DEPTH, D_MODEL, PROJ_TOTAL), jnp.float32) * D_MODEL ** -0.5
    b_forget = 3.0 + 0.5 * nrm(ks[2], (DEPTH, FOX_HEADS), jnp.float32)
    w_branch_a = nrm(ks[3], (DEPTH, DSA_WIDTH, D_MODEL), jnp.float32) * DSA_WIDTH ** -0.5
    w_branch_b = nrm(ks[4], (DEPTH, FOX_WIDTH, D_MODEL), jnp.float32) * FOX_WIDTH ** -0.5
    w_out = nrm(ks[5], (DEPTH, D_MODEL, D_MODEL), jnp.float32) * (D_MODEL ** -0.5) * beta
    ln_mix_g = 1.0 + 0.02 * nrm(ks[6], (DEPTH, D_MODEL), jnp.float32)
    ln_mix_b = 0.02 * nrm(ks[7], (DEPTH, D_MODEL), jnp.float32)
    ln_ffn_g = 1.0 + 0.02 * nrm(ks[8], (DEPTH, D_MODEL), jnp.float32)
    ln_ffn_b = 0.02 * nrm(ks[9], (DEPTH, D_MODEL), jnp.float32)
    ffn_w_gate = nrm(ks[10], (n_dense, D_MODEL, D_FF), jnp.float32) * D_MODEL ** -0.5
    ffn_w_up = nrm(ks[11], (n_dense, D_MODEL, D_FF), jnp.float32) * D_MODEL ** -0.5
    ffn_w_down = nrm(ks[12], (n_dense, D_FF, D_MODEL), jnp.float32) * (D_FF ** -0.5) * beta
    moe_router = nrm(ks[13], (n_moe, D_MODEL, N_EXPERTS), jnp.float32) * D_MODEL ** -0.5
    moe_w_gate = nrm(ks[14], (n_moe, N_EXPERTS, D_MODEL, D_FF), jnp.float32) * D_MODEL ** -0.5
    moe_w_up = nrm(ks[15], (n_moe, N_EXPERTS, D_MODEL, D_FF), jnp.float32) * D_MODEL ** -0.5
    moe_w_down = nrm(ks[16], (n_moe, N_EXPERTS, D_FF, D_MODEL), jnp.float32) * (D_FF ** -0.5) * beta
    return {"x": x, "positions": positions, "w_in": w_in, "b_forget": b_forget,
            "w_branch_a": w_branch_a, "w_branch_b": w_branch_b, "w_out": w_out,
            "ln_mix_g": ln_mix_g, "ln_mix_b": ln_mix_b, "ln_ffn_g": ln_ffn_g, "ln_ffn_b": ln_ffn_b,
            "ffn_w_gate": ffn_w_gate, "ffn_w_up": ffn_w_up, "ffn_w_down": ffn_w_down,
            "moe_router": moe_router, "moe_w_gate": moe_w_gate, "moe_w_up": moe_w_up,
            "moe_w_down": moe_w_down}


def reference(x, positions, w_in, b_forget, w_branch_a, w_branch_b, w_out,
              ln_mix_g, ln_mix_b, ln_ffn_g, ln_ffn_b, ffn_w_gate, ffn_w_up, ffn_w_down,
              moe_router, moe_w_gate, moe_w_up, moe_w_down):
    T = x.shape[1]
    top_k = min(DSA_TOPK_MAX, T // 4)
    alpha = (2.0 * DEPTH) ** 0.25
    cos, sin = rotary_tables(positions, HEAD_DIM)
    for layer in range(DEPTH):
        mix = hybrid_mixer(x, cos, sin, w_in[layer], b_forget[layer], w_branch_a[layer],
                           w_branch_b[layer], w_out[layer], top_k)
        x = layer_norm(alpha * x + mix, ln_mix_g[layer], ln_mix_b[layer])
        j = layer // 2
        if layer % 2 == 0:
            ff = dense_swiglu(x, ffn_w_gate[j], ffn_w_up[j], ffn_w_down[j])
        else:
            ff = moe_swiglu(x, moe_router[j], moe_w_gate[j], moe_w_up[j], moe_w_down[j])
        x = layer_norm(alpha * x + ff, ln_ffn_g[layer], ln_ffn_b[layer])
    return x
```

```python
import math
from contextlib import ExitStack

import numpy as np
import concourse.bass as bass
import concourse.mybir as mybir
from concourse.bass_utils import run_bass_kernel_spmd

F32 = mybir.dt.float32
BF16 = mybir.dt.bfloat16
I32 = mybir.dt.int32
AF = mybir.ActivationFunctionType
ALU = mybir.AluOpType
AX = mybir.AxisListType

N_CORES = 8
PROJ_SIZES = (512, 512, 512, 512, 64, 8, 512, 512, 512, 8, 1024, 1024)
PROJ_TOTAL = sum(PROJ_SIZES)
OFF = dict(dq=0, dk=512, dv=1024, iq=1536, ik=2048, iw=2112, fq=2120, fk=2632, fv=3144,
           fl=3656, ga=3664, gb=4688)
ROPE_THETA = 10000.0
LN_EPS = 1e-5
NEG = -30000.0


class Res:
    __slots__ = ("name", "w", "r", "x")

    def __init__(self, name=""):
        self.name = name
        self.w = {}
        self.r = {}
        self.x = None


def _key(tok):
    return tok[:2] if tok[0] == "c" else tok[:3]


class Emitter:
    COMPUTE = ("pe", "act", "dve", "pool")
    QUEUES = ("sp", "act", "pool")

    def __init__(self, nc, stack, n_dma_sems=10):
        self.nc = nc
        self.engs = {"pe": nc.tensor, "act": nc.scalar, "dve": nc.vector,
                     "pool": nc.gpsimd, "sp": nc.sync}
        self.sem = {e: stack.enter_context(nc.semaphore("c_" + e)) for e in self.COMPUTE}
        self.cnt = {e: 0 for e in self.COMPUTE}
        self.dsem = {q: [stack.enter_context(nc.semaphore(f"d_{q}{i}")) for i in range(n_dma_sems)]
                     for q in self.QUEUES}
        self.dcnt = {q: 0 for q in self.QUEUES}
        self.nd = n_dma_sems
        self.seen = {e: {} for e in self.engs}
        self.pend = []
        self.n_ins = 0
        self.n_wait = 0

    def _wait(self, e, tok):
        if tok[0] == "c":
            _, f, n = tok
            if f == "pe" and e == "pe":
                return
            key = ("c", f)
            s = self.sem[f]
        else:
            _, q, idx, n = tok
            key = ("d", q, idx)
            s = self.dsem[q][idx]
        if self.seen[e].get(key, 0) >= n:
            return
        self.seen[e][key] = n
        self.pend.append((s, n))

    def _deps(self, e, reads, writes, pwrites):
        for r in reads:
            for t in r.w.values():
                self._wait(e, t)
        for w in writes:
            for t in w.w.values():
                self._wait(e, t)
            for t in w.r.values():
                self._wait(e, t)
        for w in pwrites:
            for t in w.r.values():
                self._wait(e, t)
            if w.x is not None:
                self._wait(e, w.x)

    def _record(self, tok, reads, writes, pwrites):
        k = _key(tok)
        for r in reads:
            r.r[k] = tok
        for w in writes:
            w.w = {k: tok}
            w.r = {}
            w.x = tok
        for w in pwrites:
            w.w[k] = tok

    def _flush(self, e, ins_fn):
        pend, self.pend = self.pend, []
        if pend:
            for (s, n) in pend[:-1]:
                self.engs[e].wait_ge(s, n)
                self.n_wait += 1
            ins = ins_fn()
            ins._wait_ge(pend[-1][0], pend[-1][1])
            return ins
        return ins_fn()

    def op(self, e, fn, reads=(), writes=(), pwrites=(), inc=True):
        self._deps(e, reads, writes, pwrites)
        ins = self._flush(e, lambda: fn(self.engs[e]))
        if inc:
            self.cnt[e] += 1
            ins.then_inc(self.sem[e], 1)
            tok = ("c", e, self.cnt[e])
        else:
            tok = ("c", e, self.cnt[e] + 1)
        self._record(tok, reads, writes, pwrites)
        self.n_ins += 1
        return tok

    def dma(self, q, out, in_, reads=(), writes=(), pwrites=(), **kw):
        i = self.dcnt[q]
        idx = i % self.nd
        prev = 16 * (i // self.nd)
        if prev > 0:
            self._wait(q, ("d", q, idx, prev))
        self._deps(q, reads, writes, pwrites)
        ins = self._flush(q, lambda: self.engs[q].dma_start(out=out, in_=in_, **kw))
        ins.then_inc(self.dsem[q][idx], 16)
        self.dcnt[q] += 1
        tok = ("d", q, idx, prev + 16)
        self._record(tok, reads, writes, pwrites)
        self.n_ins += 1
        return tok

    def dma_fn(self, q, fn, reads=(), writes=(), pwrites=()):
        i = self.dcnt[q]
        idx = i % self.nd
        prev = 16 * (i // self.nd)
        if prev > 0:
            self._wait(q, ("d", q, idx, prev))
        self._deps(q, reads, writes, pwrites)
        ins = self._flush(q, lambda: fn(self.engs[q]))
        ins.then_inc(self.dsem[q][idx], 16)
        self.dcnt[q] += 1
        tok = ("d", q, idx, prev + 16)
        self._record(tok, reads, writes, pwrites)
        self.n_ins += 1
        return tok

    def barrier(self, engines=None):
        for e in (engines or list(self.engs)):
            for f in self.COMPUTE:
                if self.cnt[f] > 0:
                    self._wait(e, ("c", f, self.cnt[f])) if not (e == "pe" and f == "pe") else None
            if e == "pe" and self.cnt["pe"] > 0 and self.seen["pe"].get(("c", "pe"), 0) < self.cnt["pe"]:
                self.seen["pe"][("c", "pe")] = self.cnt["pe"]
                self.pend.append((self.sem["pe"], self.cnt["pe"]))
            for q in self.QUEUES:
                for idx in range(self.nd):
                    n_used = (self.dcnt[q] - idx + self.nd - 1) // self.nd if self.dcnt[q] > idx else 0
                    if n_used > 0:
                        self._wait(e, ("d", q, idx, 16 * n_used))
            pend, self.pend = self.pend, []
            for (s, n) in pend:
                self.engs[e].wait_ge(s, n)
                self.n_wait += 1


def build_program(T, D, DFF, DEPTH, NEXP=8, bisect_iters=14):
    assert T % 512 == 0 and D % 512 == 0 and DFF % 512 == 0
    KT = D // 128
    NTQ = T // 128
    NTB = T // 512
    FT = DFF // 128
    TOPK = min(256, T // 4)
    n_dense = (DEPTH + 1) // 2
    n_moe = DEPTH // 2
    alpha = (2.0 * DEPTH) ** 0.25
    TBF = 1024 if T % 1024 == 0 else 512
    NTBF = T // TBF

    nc = bass.Bass("TRN2", target_bir_lowering=False)

    def din(name, shape, dt=F32):
        return nc.dram_tensor(name, list(shape), dt, kind="ExternalInput").ap()

    def dscr(name, shape, dt):
        return nc.dram_tensor(name, list(shape), dt).ap()

    x_in = din("x", [T, D])
    pos_in = din("positions", [1, T], I32)
    w_in = din("w_in", [DEPTH, D, PROJ_TOTAL])
    b_forget = din("b_forget", [DEPTH, 8])
    w_ba = din("w_branch_a", [DEPTH, 512, D])
    w_bb = din("w_branch_b", [DEPTH, 512, D])
    w_out = din("w_out", [DEPTH, D, D])
    ln_mix_g = din("ln_mix_g", [DEPTH, D])
    ln_mix_b = din("ln_mix_b", [DEPTH, D])
    ln_ffn_g = din("ln_ffn_g", [DEPTH, D])
    ln_ffn_b = din("ln_ffn_b", [DEPTH, D])
    ffn_wg = din("ffn_w_gate", [n_dense, D, DFF])
    ffn_wu = din("ffn_w_up", [n_dense, D, DFF])
    ffn_wd = din("ffn_w_down", [n_dense, DFF, D])
    moe_router = din("moe_router", [max(n_moe, 1), D, NEXP])
    moe_wg = din("moe_w_gate", [max(n_moe, 1), NEXP, D, DFF])
    moe_wu = din("moe_w_up", [max(n_moe, 1), NEXP, D, DFF])
    moe_wd = din("moe_w_down", [max(n_moe, 1), NEXP, DFF, D])
    out_d = nc.dram_tensor("out", [T, D], F32, kind="ExternalOutput").ap()

    xT_d = dscr("xT_d", [D, T], BF16)
    x1T_d = dscr("x1T_d", [D, T], BF16)
    xcur_d = dscr("xcur_d", [T, D], F32)
    x1_d = dscr("x1_d", [T, D], F32)
    cosT_d = dscr("cosT_d", [128, T], F32)
    sinT_d = dscr("sinT_d", [128, T], F32)
    dqT_d = dscr("dqT_d", [512, T], BF16)
    dkT_d = dscr("dkT_d", [512, T], BF16)
    iqT_d = dscr("iqT_d", [512, T], BF16)
    ikT_d = dscr("ikT_d", [64, T], BF16)
    fqT_d = dscr("fqT_d", [512, T], BF16)
    fkT_d = dscr("fkT_d", [512, T], BF16)
    dv_d = dscr("dv_d", [T, 768], BF16)
    fv_d = dscr("fv_d", [T, 768], BF16)
    wabs_d = dscr("wabs_d", [128, NTQ * 8], F32)
    wsgn_d = dscr("wsgn_d", [128, NTQ * 8], F32)
    aug_d = dscr("aug_d", [96, T], BF16)
    sgaT_d = dscr("sgaT_d", [D, T], BF16)
    sgbT_d = dscr("sgbT_d", [D, T], BF16)
    oaT_d = dscr("oaT_d", [512, T], BF16)
    obT_d = dscr("obT_d", [512, T], BF16)
    gateT_d = dscr("gateT_d", [NEXP, T], F32)
    CAP = max(512, ((T * 3 // 8) + 511) // 512 * 512)
    xe_d = dscr("xe_d", [NEXP * CAP, D], BF16)
    ye_d = dscr("ye_d", [NEXP * CAP, D], F32)
    moe_g_d = dscr("moe_g_d", [128, NTQ * 2], F32)
    moe_i_d = dscr("moe_i_d", [128, NTQ * 2], I32)

    R = {n: Res(n) for n in ("xT_d", "x1T_d", "xcur_d", "x1_d", "tab_d", "dqT_d", "dkT_d", "iqT_d",
                             "ikT_d", "fqT_d", "fkT_d", "dv_d", "fv_d", "w_d", "aug_d", "sgaT_d",
                             "sgbT_d", "oaT_d", "obT_d", "gateT_d", "out_d", "xe_d", "ye_d", "moe_d")}

    top = ExitStack()
    em = Emitter(nc, top)

    uid = [0]
    bc_reg = [None]

    def sb(st, name, shape, dt):
        uid[0] += 1
        return st.enter_context(nc.sbuf_tensor(f"{name}_{uid[0]}", list(shape), dt))

    def ps(st, name, shape=(128, 512), dt=F32):
        uid[0] += 1
        return st.enter_context(nc.psum_tensor(f"{name}_{uid[0]}", list(shape), dt))

    ident_b = sb(top, "ident_b", [128, 128], BF16)
    ident_f = sb(top, "ident_f", [128, 128], F32)
    tri_b = sb(top, "tri_b", [128, 128], BF16)
    tri_f = sb(top, "tri_f", [128, 128], F32)
    ones_f = sb(top, "ones_f", [128, 128], F32)
    r_const = Res("const")

    with ExitStack() as ph:
        io_i = sb(ph, "io_i", [128, 128], I32)
        r_io = Res()
        em.op("pool", lambda e: e.iota(io_i[:], pattern=[[1, 128]], base=0, channel_multiplier=-1),
              writes=[r_io])
        em.op("dve", lambda e: e.tensor_scalar(out=ident_f[:], in0=io_i[:], scalar1=0.0, scalar2=None,
                                               op0=ALU.is_equal), reads=[r_io], pwrites=[r_const])
        em.op("dve", lambda e: e.tensor_scalar(out=ident_b[:], in0=io_i[:], scalar1=0.0, scalar2=None,
                                               op0=ALU.is_equal), reads=[r_io], pwrites=[r_const])
        em.op("dve", lambda e: e.tensor_scalar(out=tri_f[:], in0=io_i[:], scalar1=0.0, scalar2=None,
                                               op0=ALU.is_ge), reads=[r_io], pwrites=[r_const])
        em.op("dve", lambda e: e.tensor_scalar(out=tri_b[:], in0=io_i[:], scalar1=0.0, scalar2=None,
                                               op0=ALU.is_ge), reads=[r_io], pwrites=[r_const])
        em.op("dve", lambda e: e.memset(ones_f[:], 1.0), pwrites=[r_const])
        em.barrier()

    def phase_tables():
        with ExitStack() as ph:
            posi = sb(ph, "posi", [128, T], I32)
            ang = sb(ph, "ang", [128, T], F32)
            a2 = sb(ph, "a2", [128, T], F32)
            u = sb(ph, "u", [128, T], F32)
            ni = sb(ph, "ni", [128, T], I32)
            nf = sb(ph, "nf", [128, T], F32)
            ji = sb(ph, "ji", [128, 1], I32)
            jf = sb(ph, "jf", [128, 1], F32)
            invf = sb(ph, "invf", [128, 1], F32)
            r_pos, r_ang, r_a2, r_u, r_ni, r_nf, r_j, r_jf, r_inv = (Res() for _ in range(9))
            em.dma("sp", posi[:], pos_in.to_broadcast([128, T]), writes=[r_pos])
            for g in range(4):
                em.op("pool", lambda e: e.iota(ji[32 * g:32 * g + 32, :], pattern=[[0, 1]], base=0,
                                               channel_multiplier=1), pwrites=[r_j])
            em.op("dve", lambda e: e.tensor_copy(out=jf[:], in_=ji[:]), reads=[r_j], writes=[r_jf])
            em.op("act", lambda e: e.activation(out=invf[:], in_=jf[:], func=AF.Exp,
                                                scale=-math.log(ROPE_THETA) / 32.0),
                  reads=[r_jf], writes=[r_inv])
            em.op("dve", lambda e: e.tensor_copy(out=ang[:], in_=posi[:]), reads=[r_pos], writes=[r_ang])
            em.op("dve", lambda e: e.tensor_scalar(out=ang[:], in0=ang[:], scalar1=invf[:, 0:1], scalar2=None,
                                                   op0=ALU.mult), reads=[r_ang, r_inv], writes=[r_ang])
            C1 = 6.28125
            C2 = 2.0 * math.pi - C1
            PI_LO = 3.1415925
            for (dst, shift) in ((sinT_d, 0.0), (cosT_d, math.pi / 2)):
                em.op("dve", lambda e: e.tensor_scalar(out=a2[:], in0=ang[:], scalar1=shift, scalar2=None,
                                                       op0=ALU.add), reads=[r_ang], writes=[r_a2])
                em.op("dve", lambda e: e.tensor_scalar(out=u[:], in0=a2[:], scalar1=1.0 / (2 * math.pi),
                                                       scalar2=0.5, op0=ALU.mult, op1=ALU.add),
                      reads=[r_a2], writes=[r_u])
                em.op("dve", lambda e: e.tensor_copy(out=ni[:], in_=u[:]), reads=[r_u], writes=[r_ni])
                em.op("dve", lambda e: e.tensor_copy(out=nf[:], in_=ni[:]), reads=[r_ni], writes=[r_nf])
                em.op("dve", lambda e: e.scalar_tensor_tensor(out=a2[:], in0=nf[:], scalar=-C1, in1=a2[:],
                                                              op0=ALU.mult, op1=ALU.add),
                      reads=[r_nf, r_a2], writes=[r_a2])
                em.op("dve", lambda e: e.scalar_tensor_tensor(out=a2[:], in0=nf[:], scalar=-C2, in1=a2[:],
                                                              op0=ALU.mult, op1=ALU.add),
                      reads=[r_nf, r_a2], writes=[r_a2])
                em.op("dve", lambda e: e.tensor_scalar(out=u[:], in0=a2[:], scalar1=-math.pi,
                                                       scalar2=2 * math.pi, op0=ALU.is_lt, op1=ALU.mult),
                      reads=[r_a2], writes=[r_u])
                em.op("dve", lambda e: e.tensor_tensor(out=a2[:], in0=a2[:], in1=u[:], op=ALU.add),
                      reads=[r_a2, r_u], writes=[r_a2])
                em.op("dve", lambda e: e.tensor_scalar(out=u[:], in0=a2[:], scalar1=math.pi,
                                                       scalar2=-2 * math.pi, op0=ALU.is_gt, op1=ALU.mult),
                      reads=[r_a2], writes=[r_u])
                em.op("dve", lambda e: e.tensor_tensor(out=a2[:], in0=a2[:], in1=u[:], op=ALU.add),
                      reads=[r_a2, r_u], writes=[r_a2])
                em.op("dve", lambda e: e.tensor_scalar(out=a2[:], in0=a2[:], scalar1=-PI_LO, scalar2=PI_LO,
                                                       op0=ALU.max, op1=ALU.min), reads=[r_a2], writes=[r_a2])
                em.op("act", lambda e: e.activation(out=u[:], in_=a2[:], func=AF.Sin),
                      reads=[r_a2], writes=[r_u])
                em.dma("sp", dst[:, :], u[:], reads=[r_u], pwrites=[R["tab_d"]])
            em.barrier()

    def emit_transposes(ph_objs, src_tile, r_src, qi):
        xb, r_xb, tp, r_tp, stage, r_stage = ph_objs
        em.op("act", lambda e: e.activation(out=xb[:], in_=src_tile[:], func=AF.Copy),
              reads=[r_src], writes=[r_xb])
        for kt in range(KT):
            em.op("pe", lambda e: e.transpose(tp[:, kt * 128:(kt + 1) * 128], xb[:, kt * 128:(kt + 1) * 128],
                                              ident_b[:]),
                  reads=[r_xb, r_const], writes=[r_tp] if kt == 0 else [], pwrites=[] if kt == 0 else [r_tp],
                  inc=(kt == KT - 1))
        em.op("dve", lambda e: e.tensor_copy(out=stage[:, :, qi * 128:(qi + 1) * 128],
                                             in_=tp[:].rearrange("p (k c) -> p k c", c=128)),
              reads=[r_tp], pwrites=[r_stage])

    def phase_x0():
        with ExitStack() as ph:
            xt = [sb(ph, f"x0_{i}", [128, D], F32) for i in range(2)]
            r_xt = [Res() for _ in range(2)]
            xb = sb(ph, "x0b", [128, D], BF16)
            tp = ps(ph, "x0tp", [128, D], BF16)
            stage = [sb(ph, f"x0s{i}", [128, KT, 512], BF16) for i in range(2)]
            r_stage = [Res() for _ in range(2)]
            r_xb, r_tp = Res(), Res()
            for i in range(NTQ):
                b = i % 2
                tb, qi = i // 4, i % 4
                em.dma("sp", xt[b][:], x_in[i * 128:(i + 1) * 128, :], writes=[r_xt[b]])
                emit_transposes((xb, r_xb, tp, r_tp, stage[tb % 2], r_stage[tb % 2]), xt[b], r_xt[b], qi)
                if qi == 3:
                    em.dma("sp", xT_d.rearrange("(k p) t -> p k t", p=128)[:, :, tb * 512:(tb + 1) * 512],
                           stage[tb % 2][:], reads=[r_stage[tb % 2]], pwrites=[R["xT_d"]])
            em.barrier()

    def phase_proj(l):
        with ExitStack() as ph:
            xT = sb(ph, "p1_xT", [128, KT, T], BF16)
            r_xT = Res()
            cosT = sb(ph, "p1_cos", [128, T], F32)
            sinT = sb(ph, "p1_sin", [128, T], F32)
            r_tab = Res()
            wb = [sb(ph, f"p1_w{i}", [128, KT, 512], BF16) for i in range(2)]
            r_wb = [Res() for _ in range(2)]
            wsw = sb(ph, "p1_wsw", [128, KT, 512], BF16)
            r_wsw = Res()
            stage = [sb(ph, f"p1_st{i}", [128, 512], BF16) for i in range(3)]
            r_st = [Res() for _ in range(3)]
            t1 = sb(ph, "p1_t1", [128, 512], F32)
            t2 = sb(ph, "p1_t2", [128, 512], F32)
            r_t1, r_t2 = Res(), Res()
            stv = [sb(ph, f"p1_sv{i}", [128, 4, 192], BF16) for i in range(2)]
            r_stv = [Res() for _ in range(2)]
            psA = [ps(ph, f"p1_pA{i}") for i in range(2)]
            psB = [ps(ph, f"p1_pB{i}") for i in range(2)]
            r_pA = [Res() for _ in range(2)]
            r_pB = [Res() for _ in range(2)]
            pss = ps(ph, "p1_pss")
            r_pss = Res()
            pst = ps(ph, "p1_pst")
            r_pst = Res()
            bfb = sb(ph, "p1_bf", [128, 8], F32)
            r_bfb = Res()
            logf = sb(ph, "p1_logf", [128, NTQ, 8], F32)
            r_logf = Res()
            wabs = sb(ph, "p1_wabs", [128, NTQ, 8], F32)
            wsgn = sb(ph, "p1_wsgn", [128, NTQ, 8], F32)
            r_wab, r_wsg = Res(), Res()
            augT = sb(ph, "p1_augT", [96, T], BF16)
            r_augT = Res()
            A = sb(ph, "p1_A", [128, 96], F32)
            r_A = Res()
            sm = {n: sb(ph, "p1_" + n, [128, 8], F32) for n in ("z", "e", "c", "hif", "d1", "midf", "d2", "lof")}
            smb = {n: sb(ph, "p1_" + n, [128, 8], BF16) for n in ("hib", "midb", "lob")}
            r_sm = {n: Res() for n in list(sm) + list(smb)}

            em.dma("sp", xT[:], xT_d.rearrange("(k p) t -> p k t", p=128), reads=[R["xT_d"]], writes=[r_xT])
            em.dma("sp", cosT[:], cosT_d[:, :], reads=[R["tab_d"]], pwrites=[r_tab])
            em.dma("sp", sinT[:], sinT_d[:, :], reads=[R["tab_d"]], pwrites=[r_tab])
            em.dma("sp", bfb[:], b_forget[l].partition_broadcast(128), writes=[r_bfb])
            em.op("dve", lambda e: e.memset(A[:], 1.0), writes=[r_A])
            for i in range(2):
                em.op("pool", lambda e: e.memset(stv[i][:], 1.0), writes=[r_stv[i]])

            wl = w_in[l]
            groups = [("dq", 512, "rot", dqT_d, 0.125), ("dk", 512, "rot", dkT_d, 1.0),
                      ("dv", 512, "tok", dv_d, 1.0), ("iq", 512, "rot", iqT_d, 0.125),
                      ("ik", 72, "ikiw", ikT_d, 1.0),
                      ("fq", 512, "plain", fqT_d, 0.125), ("fk", 512, "plain", fkT_d, 1.0),
                      ("fv", 512, "tok", fv_d, 1.0), ("fl", 8, "fl", None, 1.0),
                      ("ga", 512, "sig", sgaT_d, 0), ("ga2", 512, "sig", sgaT_d, 512),
                      ("gb", 512, "sig", sgbT_d, 0), ("gb2", 512, "sig", sgbT_d, 512)]
            if D != 1024:
                raise NotImplementedError

            def colstart(name):
                if name == "ga2":
                    return OFF["ga"] + 512
                if name == "gb2":
                    return OFF["gb"] + 512
                return OFF[name]

            def load_w(gi):
                name, ncol = groups[gi][0], groups[gi][1]
                c0 = colstart(name)
                b = gi % 2
                em.dma("pool", wb[b][:, :, 0:ncol],
                       wl[:, c0:c0 + ncol].rearrange("(k p) c -> p k c", p=128), writes=[r_wb[b]])

            cnt = {"st": 0, "pp": 0, "sv": 0}

            def fm_tile(W, r_W, Wsw, col0, M, tb, kind, scale, dst, drow0):
                p = cnt["pp"] % 2
                cnt["pp"] += 1
                tsl = slice(tb * 512, (tb + 1) * 512)
                for kt in range(KT):
                    em.op("pe", lambda e: e.matmul(psA[p][0:M, :], lhsT=W[:, kt, col0:col0 + M],
                                                   rhs=xT[:, kt, tsl], start=(kt == 0), stop=(kt == KT - 1)),
                          reads=[r_W, r_xT], writes=[r_pA[p]] if kt == 0 else [], pwrites=[] if kt == 0 else [r_pA[p]],
                          inc=(kt == KT - 1))
                if kind == "rot":
                    for kt in range(KT):
                        em.op("pe", lambda e: e.matmul(psB[p][0:M, :], lhsT=Wsw[:, kt, col0:col0 + M],
                                                       rhs=xT[:, kt, tsl], start=(kt == 0), stop=(kt == KT - 1)),
                              reads=[r_wsw, r_xT], writes=[r_pB[p]] if kt == 0 else [],
                              pwrites=[] if kt == 0 else [r_pB[p]], inc=(kt == KT - 1))
                s = cnt["st"] % 3
                cnt["st"] += 1
                if kind == "rot":
                    em.op("dve", lambda e: e.scalar_tensor_tensor(out=t1[0:M, :], in0=psA[p][0:M, :], scalar=scale,
                                                                  in1=cosT[0:M, tsl], op0=ALU.mult, op1=ALU.mult),
                          reads=[r_pA[p], r_tab], writes=[r_t1])
                    em.op("dve", lambda e: e.scalar_tensor_tensor(out=t2[0:M, :], in0=psB[p][0:M, :], scalar=scale,
                                                                  in1=sinT[0:M, tsl], op0=ALU.mult, op1=ALU.mult),
                          reads=[r_pB[p], r_tab], writes=[r_t2])
                    em.op("pool", lambda e: e.tensor_tensor(out=stage[s][0:M, :], in0=t1[0:M, :], in1=t2[0:M, :],
                                                            op=ALU.add),
                          reads=[r_t1, r_t2], writes=[r_st[s]])
                elif kind == "plain":
                    em.op("act", lambda e: e.activation(out=stage[s][0:M, :], in_=psA[p][0:M, :], func=AF.Copy,
                                                        scale=scale), reads=[r_pA[p]], writes=[r_st[s]])
                else:
                    em.op("act", lambda e: e.activation(out=stage[s][0:M, :], in_=psA[p][0:M, :], func=AF.Sigmoid),
                          reads=[r_pA[p]], writes=[r_st[s]])
                em.dma("sp", dst[drow0:drow0 + M, tsl], stage[s][0:M, :], reads=[r_st[s]], pwrites=[R[_rn(dst)]])

            names = {id(dqT_d): "dqT_d", id(dkT_d): "dkT_d", id(iqT_d): "iqT_d", id(ikT_d): "ikT_d",
                     id(fqT_d): "fqT_d", id(fkT_d): "fkT_d", id(sgaT_d): "sgaT_d", id(sgbT_d): "sgbT_d",
                     id(dv_d): "dv_d", id(fv_d): "fv_d"}

            def _rn(d):
                return names[id(d)]

            def make_swapped(W, r_W, ncol):
                nh = ncol // 64
                Wv = W[:, :, 0:ncol].rearrange("p k (h two j) -> p k h two j", two=2, j=32)
                Sv = wsw[:, :, 0:ncol].rearrange("p k (h two j) -> p k h two j", two=2, j=32)
                for kt in range(KT):
                    em.op("act", lambda e: e.activation(out=Sv[:, kt, :, 0, :], in_=Wv[:, kt, :, 1, :], func=AF.Copy,
                                                        scale=-1.0),
                          reads=[r_W], writes=[r_wsw] if kt == 0 else [], pwrites=[] if kt == 0 else [r_wsw])
                    em.op("pool", lambda e: e.tensor_copy(out=Sv[:, kt, :, 1, :], in_=Wv[:, kt, :, 0, :]),
                          reads=[r_W], pwrites=[r_wsw])

            load_w(0)
            for gi, (name, ncol, kind, dst, extra) in enumerate(groups):
                if gi + 1 < len(groups):
                    load_w(gi + 1)
                W, r_W = wb[gi % 2], r_wb[gi % 2]
                if kind in ("rot",):
                    make_swapped(W, r_W, ncol)
                    for ct in range(ncol // 128):
                        for tb in range(NTB):
                            fm_tile(W, r_W, wsw, ct * 128, 128, tb, "rot", extra, dst, ct * 128)
                elif kind == "plain":
                    for ct in range(ncol // 128):
                        for tb in range(NTB):
                            fm_tile(W, r_W, None, ct * 128, 128, tb, "plain", extra, dst, ct * 128)
                elif kind == "sig":
                    for ct in range(ncol // 128):
                        for tb in range(NTB):
                            fm_tile(W, r_W, None, ct * 128, 128, tb, "sig", 1.0, dst, extra + ct * 128)
                elif kind == "ikiw":
                    make_swapped(W, r_W, 64)
                    for tb in range(NTB):
                        fm_tile(W, r_W, wsw, 0, 64, tb, "rot", 1.0, dst, 0)
                    for i in range(NTQ):
                        for kt in range(KT):
                            em.op("pe", lambda e: e.matmul(pss[:, 0:8], lhsT=xT[:, kt, i * 128:(i + 1) * 128],
                                                           rhs=W[:, kt, 64:72], start=(kt == 0), stop=(kt == KT - 1)),
                                  reads=[r_W, r_xT], writes=[r_pss] if kt == 0 else [],
                                  pwrites=[] if kt == 0 else [r_pss], inc=(kt == KT - 1))
                        em.op("act", lambda e: e.activation(out=wabs[:, i, :], in_=pss[:, 0:8], func=AF.Abs,
                                                            scale=8.0 ** -0.5),
                              reads=[r_pss], pwrites=[r_wab])
                        em.op("act", lambda e: e.activation(out=wsgn[:, i, :], in_=pss[:, 0:8], func=AF.Sign),
                              reads=[r_pss], pwrites=[r_wsg])
                    em.dma("sp", wabs_d[:, :], wabs[:].rearrange("p a b -> p (a b)"), reads=[r_wab], pwrites=[R["w_d"]])
                    em.dma("sp", wsgn_d[:, :], wsgn[:].rearrange("p a b -> p (a b)"), reads=[r_wsg], pwrites=[R["w_d"]])
                elif kind == "tok":
                    for i in range(NTQ):
                        p = cnt["pp"] % 2
                        cnt["pp"] += 1
                        for kt in range(KT):
                            em.op("pe", lambda e: e.matmul(psA[p][:, :], lhsT=xT[:, kt, i * 128:(i + 1) * 128],
                                                           rhs=W[:, kt, 0:512], start=(kt == 0), stop=(kt == KT - 1)),
                                  reads=[r_W, r_xT], writes=[r_pA[p]] if kt == 0 else [],
                                  pwrites=[] if kt == 0 else [r_pA[p]], inc=(kt == KT - 1))
                        s = cnt["sv"] % 2
                        cnt["sv"] += 1
                        sv = stv[s][:].rearrange("p a (three c) -> p a three c", c=64)
                        pv = psA[p][:, :].rearrange("p (a two c) -> p a two c", two=2, c=64)
                        em.op("act", lambda e: e.activation(out=sv[:, :, 0, :], in_=pv[:, :, 0, :], func=AF.Copy),
                              reads=[r_pA[p]], writes=[r_stv[s]])
                        em.op("dve", lambda e: e.tensor_copy(out=sv[:, :, 2, :], in_=pv[:, :, 1, :]),
                              reads=[r_pA[p]], pwrites=[r_stv[s]])
                        em.dma("sp", dst[i * 128:(i + 1) * 128, :], stv[s][:].rearrange("p a c -> p (a c)"),
                               reads=[r_stv[s]], pwrites=[R[_rn(dst)]])
                elif kind == "fl":
                    for i in range(NTQ):
                        for kt in range(KT):
                            em.op("pe", lambda e: e.matmul(pss[:, 0:8], lhsT=xT[:, kt, i * 128:(i + 1) * 128],
                                                           rhs=W[:, kt, 0:8], start=(kt == 0), stop=(kt == KT - 1)),
                                  reads=[r_W, r_xT], writes=[r_pss] if kt == 0 else [],
                                  pwrites=[] if kt == 0 else [r_pss], inc=(kt == KT - 1))
                        em.op("dve", lambda e: e.tensor_tensor(out=sm["z"][:], in0=pss[:, 0:8], in1=bfb[:], op=ALU.add),
                              reads=[r_pss, r_bfb], writes=[r_sm["z"]])
                        em.op("act", lambda e: e.activation(out=sm["e"][:], in_=sm["z"][:], func=AF.Exp, scale=-1.0),
                              reads=[r_sm["z"]], writes=[r_sm["e"]])
                        em.op("act", lambda e: e.activation(out=sm["z"][:], in_=sm["e"][:], func=AF.Ln, bias=1.0),
                              reads=[r_sm["e"]], writes=[r_sm["z"]])
                        em.op("dve", lambda e: e.tensor_scalar(out=logf[:, i, :], in0=sm["z"][:], scalar1=-1.0,
                                                               scalar2=None, op0=ALU.mult),
                              reads=[r_sm["z"]], pwrites=[r_logf])
                        for j in range(i + 1):
                            em.op("pe", lambda e: e.matmul(pss[:, 8:16], lhsT=(tri_f if j == i else ones_f)[:, :],
                                                           rhs=logf[:, j, :], start=(j == 0), stop=(j == i)),
                                  reads=[r_logf, r_const], writes=[r_pss] if j == 0 else [],
                                  pwrites=[] if j == 0 else [r_pss], inc=(j == i))
                        em.op("dve", lambda e: e.tensor_copy(out=sm["c"][:], in_=pss[:, 8:16]),
                              reads=[r_pss], writes=[r_sm["c"]])
                        Av = A[:].rearrange("p (s h r) -> p s h r", s=2, r=6)
                        em.op("dve", lambda e: e.tensor_copy(out=smb["hib"][:], in_=sm["c"][:]),
                              reads=[r_sm["c"]], writes=[r_sm["hib"]])
                        em.op("dve", lambda e: e.tensor_copy(out=sm["hif"][:], in_=smb["hib"][:]),
                              reads=[r_sm["hib"]], writes=[r_sm["hif"]])
                        em.op("dve", lambda e: e.tensor_tensor(out=sm["d1"][:], in0=sm["c"][:], in1=sm["hif"][:],
                                                               op=ALU.subtract),
                              reads=[r_sm["c"], r_sm["hif"]], writes=[r_sm["d1"]])
                        em.op("dve", lambda e: e.tensor_copy(out=smb["midb"][:], in_=sm["d1"][:]),
                              reads=[r_sm["d1"]], writes=[r_sm["midb"]])
                        em.op("dve", lambda e: e.tensor_copy(out=sm["midf"][:], in_=smb["midb"][:]),
                              reads=[r_sm["midb"]], writes=[r_sm["midf"]])
                        em.op("dve", lambda e: e.tensor_tensor(out=sm["d2"][:], in0=sm["d1"][:], in1=sm["midf"][:],
                                                               op=ALU.subtract),
                              reads=[r_sm["d1"], r_sm["midf"]], writes=[r_sm["d2"]])
                        em.op("dve", lambda e: e.tensor_copy(out=smb["lob"][:], in_=sm["d2"][:]),
                              reads=[r_sm["d2"]], writes=[r_sm["lob"]])
                        em.op("dve", lambda e: e.tensor_copy(out=sm["lof"][:], in_=smb["lob"][:]),
                              reads=[r_sm["lob"]], writes=[r_sm["lof"]])
                        for ri, nm in enumerate(("hif", "midf", "lof")):
                            em.op("dve", lambda e: e.tensor_copy(out=Av[:, 0, :, ri], in_=sm[nm][:]),
                                  reads=[r_sm[nm]], writes=[r_A] if ri == 0 else [], pwrites=[] if ri == 0 else [r_A])
                            em.op("dve", lambda e: e.tensor_scalar(out=Av[:, 1, :, 3 + ri], in0=sm[nm][:], scalar1=-1.0,
                                                                   scalar2=None, op0=ALU.mult),
                                  reads=[r_sm[nm]], pwrites=[r_A])
                        em.op("pe", lambda e: e.transpose(pst[0:96, 0:128], A[:, :], ident_f[:]),
                              reads=[r_A, r_const], writes=[r_pst])
                        em.op("act", lambda e: e.activation(out=augT[:, i * 128:(i + 1) * 128], in_=pst[0:96, 0:128],
                                                            func=AF.Copy), reads=[r_pst], pwrites=[r_augT])
                    em.dma("sp", aug_d[:, :], augT[:], reads=[r_augT], writes=[R["aug_d"]])
            em.barrier()

    def attn_alloc(ph, pfx, ns=3):
        o = dict(
            ns=ns,
            sps=[ps(ph, f"{pfx}_s{i}") for i in range(ns)], r_sps=[Res() for _ in range(ns)],
            pexp=[sb(ph, f"{pfx}_p{i}", [128, 512], BF16) for i in range(3)], r_pexp=[Res() for _ in range(3)],
            ops=[ps(ph, f"{pfx}_o{i}") for i in range(2)], r_ops=[Res() for _ in range(2)],
            rden=sb(ph, f"{pfx}_rden", [128, 512], F32), r_rden=Res(),
            oT=[sb(ph, f"{pfx}_oT{i}", [128, 512], BF16) for i in range(2)], r_oT=[Res() for _ in range(2)],
            ctr={"o": 0, "s": 0})
        return o

    def attn_block(o, kT, r_kT, qT, r_qT, q0, Kc, prow, V, r_V, odd, n_ts, col_lo_fn, diag_fn, mask_mm_fn,
                   out_ap, r_out, mul_fn=None):
        ob = o["ctr"]["o"] % 2
        o["ctr"]["o"] += 1
        ops, r_ops = o["ops"][ob], o["r_ops"][ob]
        oT, r_oT = o["oT"][ob], o["r_oT"][ob]
        vc = 64 if odd else 0
        base = o["ctr"]["s"]
        o["ctr"]["s"] += n_ts
        has_mask = mask_mm_fn is not None

        def emit_S(i):
            lo = col_lo_fn(i)
            s = (base + i) % o["ns"]
            sps, r_sps = o["sps"][s], o["r_sps"][s]
            em.op("pe", lambda e: e.matmul(sps[:, lo:512], lhsT=kT[prow:prow + Kc, i * 128:(i + 1) * 128],
                                           rhs=qT[prow:prow + Kc, q0 + lo:q0 + 512], start=True, stop=not has_mask),
                  reads=[r_kT, r_qT], writes=[r_sps], inc=not has_mask)
            if has_mask:
                mask_mm_fn(i, lo, sps, r_sps)

        def emit_rest(i):
            lo = col_lo_fn(i)
            s = (base + i) % o["ns"]
            s3 = (base + i) % 3
            sps, r_sps, pexp, r_pexp = o["sps"][s], o["r_sps"][s], o["pexp"][s3], o["r_pexp"][s3]
            em.op("act", lambda e: e.activation(out=pexp[:, lo:512], in_=sps[:, lo:512], func=AF.Exp),
                  reads=[r_sps], writes=[r_pexp])
            if mul_fn is not None:
                mul_fn(i, lo, pexp, r_pexp)
            dc = diag_fn(i) if diag_fn is not None else None
            if dc is not None:
                em.op("pool", lambda e: e.tensor_tensor(out=pexp[:, dc:dc + 128], in0=pexp[:, dc:dc + 128],
                                                        in1=tri_b[:, :], op=ALU.mult),
                      reads=[r_pexp, r_const], writes=[r_pexp])
            em.op("pe", lambda e: e.matmul(ops[:, lo:512], lhsT=V[:, i, vc:vc + 128], rhs=pexp[:, lo:512],
                                           start=(i == 0), stop=(i == n_ts - 1)),
                  reads=[r_V, r_pexp], writes=[r_ops] if i == 0 else [], pwrites=[] if i == 0 else [r_ops],
                  inc=(i == n_ts - 1))

        emit_S(0)
        for i in range(n_ts):
            if i + 1 < n_ts:
                emit_S(i + 1)
            emit_rest(i)
        (orow, drow) = (64, 0) if odd else (0, 64)
        em.op("dve", lambda e: e.reciprocal(out=o["rden"][orow:orow + 64, :], in_=ops[drow:drow + 64, :]),
              reads=[r_ops], writes=[o["r_rden"]])
        em.op("dve", lambda e: e.tensor_tensor(out=oT[orow:orow + 64, :], in0=ops[orow:orow + 64, :],
                                               in1=o["rden"][orow:orow + 64, :], op=ALU.mult),
              reads=[r_ops, o["r_rden"]], writes=[r_oT])
        em.dma("sp", out_ap, oT[orow:orow + 64, :], reads=[r_oT], pwrites=[r_out])

    def phase_fox():
        with ExitStack() as ph:
            o = attn_alloc(ph, "fx")
            qa = [sb(ph, f"fx_q{i}", [70, T], BF16) for i in range(2)]
            ka = [sb(ph, f"fx_k{i}", [70, T], BF16) for i in range(2)]
            r_qa = [Res() for _ in range(2)]
            r_ka = [Res() for _ in range(2)]
            V = sb(ph, "fx_V", [128, NTQ, 768], BF16)
            r_V = Res()
            em.dma("sp", V[:], fv_d.rearrange("(i p) c -> p i c", p=128), reads=[R["fv_d"]], writes=[r_V])

            def load_head(h):
                b = h % 2
                em.dma("sp", qa[b][0:64, :], fqT_d[h * 64:(h + 1) * 64, :], reads=[R["fqT_d"]], writes=[r_qa[b]])
                em.dma("sp", qa[b][64:70, :], aug_d[h * 6:h * 6 + 6, :], reads=[R["aug_d"]], pwrites=[r_qa[b]])
                em.dma("sp", ka[b][0:64, :], fkT_d[h * 64:(h + 1) * 64, :], reads=[R["fkT_d"]], writes=[r_ka[b]])
                em.dma("sp", ka[b][64:70, :], aug_d[48 + h * 6:48 + h * 6 + 6, :], reads=[R["aug_d"]],
                       pwrites=[r_ka[b]])

            load_head(0)
            for h in range(8):
                if h + 1 < 8:
                    load_head(h + 1)
                b = h % 2
                pair, odd = h // 2, h % 2
                Vp = V[:, :, pair * 192:(pair + 1) * 192]
                for tb in range(NTB):
                    attn_block(o, ka[b], r_ka[b], qa[b], r_qa[b], tb * 512, 70, 0, Vp, r_V, odd, 4 * (tb + 1),
                               lambda i: max(0, (i - 4 * tb) * 128),
                               lambda i: ((i - 4 * tb) * 128 if i >= 4 * tb else None),
                               None, obT_d[h * 64:(h + 1) * 64, tb * 512:(tb + 1) * 512], R["obT_d"])
            em.barrier()

    def phase_dsa():
        with ExitStack() as ph:
            o = attn_alloc(ph, "ds", ns=2)
            mtp = ps(ph, "ds_mtp", [128, 512], BF16)
            r_mtp = Res()
            mbq = sb(ph, "ds_mbq", [128, T], BF16)
            r_mbq = Res()
            iqs = [sb(ph, f"ds_iq{i}", [128, 4, 128], BF16) for i in range(2)]
            r_iqs = [Res() for _ in range(2)]
            dgs = [sb(ph, f"ds_dg{i}", [128, 8, 128], BF16) for i in range(2)]
            r_dgs = [Res() for _ in range(2)]
            accps = ps(ph, "ds_acc")
            r_accps = Res()
            ik2 = sb(ph, "ds_ik", [128, T], BF16)
            wabs = sb(ph, "ds_wabs", [128, NTQ, 8], F32)
            wsgn = sb(ph, "ds_wsgn", [128, NTQ, 8], F32)
            r_iq, r_ik, r_w = Res(), Res(), Res()
            scs = [sb(ph, f"ds_sc{i}", [128, T], F32) for i in range(2)]
            r_scs = [Res() for _ in range(2)]
            junk = sb(ph, "ds_junk", [128, T], BF16)
            r_junk = Res()
            mbs = [sb(ph, f"ds_mbT{s_}", [128, NTQ, 512], BF16) for s_ in range(2)]
            r_mbs = [[Res() for _ in range(4)] for _ in range(2)]
            rl = [sb(ph, f"ds_rl{i}", [128, 512], BF16) for i in range(4)]
            r_rl = [Res() for _ in range(4)]
            scps = [ps(ph, f"ds_sp{i}") for i in range(2)]
            r_scps = [Res() for _ in range(2)]
            sm = {n: sb(ph, "ds_" + n, [128, 1], F32) for n in ("lo", "w0", "wk", "mid", "cnt", "gw", "mx", "thr0")}
            r_sm = {n: Res() for n in sm}
            Vp = [sb(ph, f"ds_V{i}", [128, NTQ, 192], BF16) for i in range(2)]
            r_Vp = [Res() for _ in range(2)]
            qh = [sb(ph, f"ds_q{i}", [128, 512], BF16) for i in range(2)]
            kh = [sb(ph, f"ds_k{i}", [128, T], BF16) for i in range(2)]
            r_qh = [Res() for _ in range(2)]
            r_kh = [Res() for _ in range(2)]

            em.dma("sp", ik2[0:64, :], ikT_d[:, :], reads=[R["ikT_d"]], pwrites=[r_ik])
            em.dma("sp", ik2[64:128, :], ikT_d[:, :], reads=[R["ikT_d"]], pwrites=[r_ik])
            em.dma("sp", wabs[:].rearrange("p a b -> p (a b)"), wabs_d[:, :], reads=[R["w_d"]], pwrites=[r_w])
            em.dma("sp", wsgn[:].rearrange("p a b -> p (a b)"), wsgn_d[:, :], reads=[R["w_d"]], pwrites=[r_w])
            em.op("dve", lambda e: e.memset(sm["thr0"][:], -1e29), writes=[r_sm["thr0"]])
            ctr = {"p": 0, "v": 0, "h": 0, "sc": 0}
            dv_v = dv_d.rearrange("(i p) c -> p i c", p=128)

            def prep_q(tb, qi, mb, r_mb):
                g = tb * 4 + qi
                L2 = 128 * (g + 1)
                L1 = L2 - 64
                sb_ = ctr["sc"] % 2
                ctr["sc"] += 1
                sc, r_sc = scs[sb_], r_scs[sb_]
                dg, r_dg = dgs[sb_], r_dgs[sb_]
                iqt, r_iqt = iqs[sb_], r_iqs[sb_]
                em.dma("sp", iqt[:], iqT_d.rearrange("(k p) t -> p k t", p=128)[:, :, g * 128:(g + 1) * 128],
                       reads=[R["iqT_d"]], writes=[r_iqt])
                for h in range(8):
                    em.op("pool", lambda e: e.tensor_scalar(out=dg[:, h, :], in0=ident_b[:, :], scalar1=wsgn[:, g, h:h + 1],
                                                            scalar2=None, op0=ALU.mult),
                          reads=[r_const, r_w], writes=[r_dg] if h == 0 else [], pwrites=[] if h == 0 else [r_dg])
                for kb in range((L2 + 511) // 512):
                    c0 = kb * 512
                    cw = min(512, L2 - c0)
                    base = ctr["p"]
                    ctr["p"] += 8

                    def emit_x(h):
                        pair, prow = h // 2, (h % 2) * 64
                        p = (base + h) % 2
                        em.op("pe", lambda e: e.matmul(scps[p][:, 0:cw], lhsT=iqt[prow:prow + 64, pair, :],
                                                       rhs=ik2[prow:prow + 64, c0:c0 + cw], start=True, stop=True),
                              reads=[r_iqt, r_ik], writes=[r_scps[p]])

                    def emit_acc(h):
                        p = (base + h) % 2
                        p4 = (base + h) % 4
                        em.op("act", lambda e: e.activation(out=rl[p4][:, 0:cw], in_=scps[p][:, 0:cw], func=AF.Relu,
                                                            scale=wabs[:, g, h:h + 1]),
                              reads=[r_scps[p], r_w], writes=[r_rl[p4]])
                        em.op("pe", lambda e: e.matmul(accps[:, 0:cw], lhsT=dg[:, h, :], rhs=rl[p4][:, 0:cw],
                                                       start=(h == 0), stop=(h == 7)),
                              reads=[r_dg, r_rl[p4]], writes=[r_accps] if h == 0 else [],
                              pwrites=[] if h == 0 else [r_accps], inc=(h == 7))

                    emit_x(0)
                    for h in range(8):
                        if h + 1 < 8:
                            emit_x(h + 1)
                        emit_acc(h)
                    em.op("act", lambda e: e.activation(out=sc[:, c0:c0 + cw], in_=accps[:, 0:cw], func=AF.Copy),
                          reads=[r_accps], writes=[r_sc] if kb == 0 else [], pwrites=[] if kb == 0 else [r_sc])
                em.op("dve", lambda e: e.memset(sc[0:64, L1:L2], -1e30), reads=[r_sc], pwrites=[r_sc])
                if L2 > TOPK:
                    em.op("dve", lambda e: e.tensor_reduce(out=sm["mx"][:], in_=sc[:, 0:L2], axis=AX.X, op=ALU.max),
                          reads=[r_sc], writes=[r_sm["mx"]])
                    em.op("dve", lambda e: e.tensor_reduce(out=sm["lo"][:], in_=sc[:, 0:L1], axis=AX.X, op=ALU.min),
                          reads=[r_sc], writes=[r_sm["lo"]])
                    em.op("dve", lambda e: e.tensor_tensor(out=sm["w0"][:], in0=sm["mx"][:], in1=sm["lo"][:],
                                                           op=ALU.subtract),
                          reads=[r_sm["mx"], r_sm["lo"]], writes=[r_sm["w0"]])
                    em.op("dve", lambda e: e.tensor_scalar(out=sm["w0"][:], in0=sm["w0"][:], scalar1=1.0001,
                                                           scalar2=1e-12, op0=ALU.mult, op1=ALU.add),
                          reads=[r_sm["w0"]], writes=[r_sm["w0"]])
                    for k in range(bisect_iters):
                        em.op("dve", lambda e: e.scalar_tensor_tensor(out=sm["mid"][:], in0=sm["w0"][:],
                                                                      scalar=2.0 ** -(k + 1), in1=sm["lo"][:],
                                                                      op0=ALU.mult, op1=ALU.add),
                              reads=[r_sm["w0"], r_sm["lo"]], writes=[r_sm["mid"]])
                        em.op("dve", lambda e: e.tensor_scalar(out=junk[:, 0:L2], in0=sc[:, 0:L2],
                                                               scalar1=sm["mid"][:, 0:1], scalar2=None,
                                                               op0=ALU.is_ge, op1=ALU.add,
                                                               accum_out=sm["cnt"][:, 0:1]),
                              reads=[r_sc, r_sm["mid"]], writes=[r_junk, r_sm["cnt"]])
                        em.op("dve", lambda e: e.scalar_tensor_tensor(out=sm["gw"][:], in0=sm["cnt"][:],
                                                                      scalar=float(TOPK), in1=sm["w0"][:],
                                                                      op0=ALU.is_ge, op1=ALU.mult),
                              reads=[r_sm["cnt"], r_sm["w0"]], writes=[r_sm["gw"]])
                        em.op("dve", lambda e: e.scalar_tensor_tensor(out=sm["lo"][:], in0=sm["gw"][:],
                                                                      scalar=2.0 ** -(k + 1), in1=sm["lo"][:],
                                                                      op0=ALU.mult, op1=ALU.add),
                              reads=[r_sm["gw"], r_sm["lo"]], writes=[r_sm["lo"]])
                    thr, r_thr = sm["lo"], r_sm["lo"]
                else:
                    thr, r_thr = sm["thr0"], r_sm["thr0"]
                em.op("dve", lambda e: e.tensor_scalar(out=mbq[:, 0:L2], in0=sc[:, 0:L2], scalar1=thr[:, 0:1],
                                                       scalar2=None, op0=ALU.is_ge),
                      reads=[r_sc, r_thr], writes=[r_mbq])
                for j0 in range(0, g + 1, 4):
                    nt = min(4, g + 1 - j0)
                    for t_ in range(nt):
                        em.op("pe", lambda e: e.transpose(mtp[:, t_ * 128:(t_ + 1) * 128],
                                                          mbq[:, (j0 + t_) * 128:(j0 + t_ + 1) * 128], ident_b[:]),
                              reads=[r_mbq, r_const], writes=[r_mtp] if t_ == 0 else [],
                              pwrites=[] if t_ == 0 else [r_mtp], inc=(t_ == nt - 1))
                    em.op("dve", lambda e: e.tensor_copy(out=mb[:, j0:j0 + nt, qi * 128:(qi + 1) * 128],
                                                         in_=mtp[:, 0:nt * 128].rearrange("p (t c) -> p t c", c=128)),
                          reads=[r_mtp], writes=[r_mb[qi]] if j0 == 0 else [], pwrites=[] if j0 == 0 else [r_mb[qi]])

            def attend_pair(tb, pair, mb, r_mb):
                n_ts = 4 * (tb + 1)


                def mask_mul(i, lo, pexp, r_pexp):
                    eng = "pool" if (i % 2 == 0) else "dve"
                    q_first = lo // 128
                    em.op(eng, lambda e: e.tensor_tensor(out=pexp[:, lo:512], in0=pexp[:, lo:512], in1=mb[:, i, lo:512],
                                                         op=ALU.mult),
                          reads=[r_pexp] + [r_mb[qq] for qq in range(q_first, 4)], writes=[r_pexp])

                vb = ctr["v"] % 2
                ctr["v"] += 1
                em.dma("sp", Vp[vb][:, 0:n_ts, :], dv_v[:, 0:n_ts, pair * 192:(pair + 1) * 192],
                       reads=[R["dv_d"]], writes=[r_Vp[vb]])
                for odd in range(2):
                    h = pair * 2 + odd
                    prow = odd * 64
                    hb = ctr["h"] % 2
                    ctr["h"] += 1
                    em.dma("sp", qh[hb][prow:prow + 64, :], dqT_d[h * 64:(h + 1) * 64, tb * 512:(tb + 1) * 512],
                           reads=[R["dqT_d"]], writes=[r_qh[hb]])
                    em.dma("sp", kh[hb][prow:prow + 64, 0:n_ts * 128], dkT_d[h * 64:(h + 1) * 64, 0:n_ts * 128],
                           reads=[R["dkT_d"]], writes=[r_kh[hb]])
                    attn_block(o, kh[hb], r_kh[hb], qh[hb], r_qh[hb], 0, 64, prow, Vp[vb], r_Vp[vb], odd, n_ts,
                               lambda i: max(0, (i - 4 * tb) * 128), None, None,
                               oaT_d[h * 64:(h + 1) * 64, tb * 512:(tb + 1) * 512], R["oaT_d"], mul_fn=mask_mul)

            order = list(range(NTB - 1, -1, -1))
            for qi in range(4):
                prep_q(order[0], qi, mbs[0], r_mbs[0])
            for n_, tb in enumerate(order):
                for qi in range(4):
                    if n_ + 1 < NTB:
                        prep_q(order[n_ + 1], qi, mbs[(n_ + 1) % 2], r_mbs[(n_ + 1) % 2])
                    attend_pair(tb, qi, mbs[n_ % 2], r_mbs[n_ % 2])
            em.barrier()

    def ln_alloc(ph, pfx, g_d, b_d):
        o = dict(
            y=[sb(ph, f"{pfx}_y{i}", [128, D], F32) for i in range(2)], r_y=[Res() for _ in range(2)],
            xo=[sb(ph, f"{pfx}_xo{i}", [128, D], F32) for i in range(2)], r_xo=[Res() for _ in range(2)],
            xr=[sb(ph, f"{pfx}_xr{i}", [128, D], F32) for i in range(2)], r_xr=[Res() for _ in range(2)],
            st=sb(ph, f"{pfx}_st", [128, 6 * (D // 512)], F32), r_st=Res(),
            mv=sb(ph, f"{pfx}_mv", [128, 2], F32), r_mv=Res(),
            sd=sb(ph, f"{pfx}_sd", [128, 1], F32), r_sd=Res(),
            gb=sb(ph, f"{pfx}_gb", [128, D], F32), bb=sb(ph, f"{pfx}_bb", [128, D], F32), r_gb=Res(),
            xb=sb(ph, f"{pfx}_xb", [128, D], BF16), r_xb=Res(),
            tp=ps(ph, f"{pfx}_tp", [128, D], BF16), r_tp=Res(),
            stage=[sb(ph, f"{pfx}_sg{i}", [128, KT, 512], BF16) for i in range(2)], r_stage=[Res() for _ in range(2)],
            n=0)
        em.dma("sp", o["gb"][:], g_d.partition_broadcast(128), pwrites=[o["r_gb"]])
        em.dma("sp", o["bb"][:], b_d.partition_broadcast(128), pwrites=[o["r_gb"]])
        return o

    def ln_stage(o, i, halves, xres_d, store_d, r_store, xT_dst, r_xT_dst, after_fn=None):
        b = o["n"] % 2
        o["n"] += 1
        y, r_y, xo, r_xo, xr, r_xr = o["y"][b], o["r_y"][b], o["xo"][b], o["r_xo"][b], o["xr"][b], o["r_xr"][b]
        em.dma("sp", xr[:], xres_d[i * 128:(i + 1) * 128, :], reads=[R["x1_d"], R["xcur_d"]], writes=[r_xr])
        for hf, (pa, r_pa) in enumerate(halves):
            sl = slice(hf * 512, (hf + 1) * 512)
            em.op("dve", lambda e: e.scalar_tensor_tensor(out=y[:, sl], in0=xr[:, sl], scalar=alpha, in1=pa,
                                                          op0=ALU.mult, op1=ALU.add),
                  reads=[r_xr, r_pa], writes=[r_y] if hf == 0 else [], pwrites=[] if hf == 0 else [r_y])
        for c in range(D // 512):
            em.op("dve", lambda e: e.bn_stats(out=o["st"][:, c * 6:(c + 1) * 6], in_=y[:, c * 512:(c + 1) * 512]),
                  reads=[r_y], writes=[o["r_st"]] if c == 0 else [], pwrites=[] if c == 0 else [o["r_st"]])
        em.op("dve", lambda e: e.bn_aggr(out=o["mv"][:, 0:2], in_=o["st"][:, :]), reads=[o["r_st"]], writes=[o["r_mv"]])
        em.op("dve", lambda e: e.tensor_scalar(out=o["sd"][:], in0=o["mv"][:, 1:2], scalar1=LN_EPS, scalar2=None,
                                               op0=ALU.add), reads=[o["r_mv"]], writes=[o["r_sd"]])
        em.op("act", lambda e: e.activation(out=o["sd"][:], in_=o["sd"][:], func=AF.Sqrt),
              reads=[o["r_sd"]], writes=[o["r_sd"]])
        em.op("dve", lambda e: e.reciprocal(out=o["sd"][:], in_=o["sd"][:]), reads=[o["r_sd"]], writes=[o["r_sd"]])
        em.op("dve", lambda e: e.tensor_scalar(out=y[:], in0=y[:], scalar1=o["mv"][:, 0:1], scalar2=o["sd"][:, 0:1],
                                               op0=ALU.subtract, op1=ALU.mult),
              reads=[r_y, o["r_mv"], o["r_sd"]], writes=[r_y])
        em.op("pool", lambda e: e.tensor_tensor(out=xo[:], in0=y[:], in1=o["gb"][:], op=ALU.mult),
              reads=[r_y, o["r_gb"]], writes=[r_xo])
        em.op("pool", lambda e: e.tensor_tensor(out=xo[:], in0=xo[:], in1=o["bb"][:], op=ALU.add),
              reads=[r_xo, o["r_gb"]], writes=[r_xo])
        em.dma("sp", store_d[i * 128:(i + 1) * 128, :], xo[:], reads=[r_xo], pwrites=[r_store])
        if xT_dst is not None:
            tbk, qi = i // 4, i % 4
            stg, r_stg = o["stage"][tbk % 2], o["r_stage"][tbk % 2]
            emit_transposes((o["xb"], o["r_xb"], o["tp"], o["r_tp"], stg, r_stg), xo, r_xo, qi)
            if qi == 3:
                em.dma("sp", xT_dst.rearrange("(k p) t -> p k t", p=128)[:, :, tbk * 512:(tbk + 1) * 512], stg[:],
                       reads=[r_stg], pwrites=[r_xT_dst])
        if after_fn is not None:
            after_fn(i, xo, r_xo, o["xb"], o["r_xb"])

    def phase_merge(l, moe_j):
        with ExitStack() as ph:
            lo_ = ln_alloc(ph, "p4", ln_mix_g[l], ln_mix_b[l])
            Wa = sb(ph, "p4_Wa", [128, 4, D], BF16)
            Wb = sb(ph, "p4_Wb", [128, 4, D], BF16)
            Wo = sb(ph, "p4_Wo", [128, KT, D], BF16)
            r_W = Res()
            oa = [sb(ph, f"p4_oa{i}", [128, 4, 512], BF16) for i in range(2)]
            ob = [sb(ph, f"p4_ob{i}", [128, 4, 512], BF16) for i in range(2)]
            sga = [sb(ph, f"p4_sga{i}", [128, KT, 512], BF16) for i in range(2)]
            sgb = [sb(ph, f"p4_sgb{i}", [128, KT, 512], BF16) for i in range(2)]
            r_in = [Res() for _ in range(2)]
            mg = sb(ph, "p4_mg", [128, KT, 512], BF16)
            r_mg = Res()
            t1 = sb(ph, "p4_t1", [128, 512], F32)
            t2 = sb(ph, "p4_t2", [128, 512], F32)
            r_t1, r_t2 = Res(), Res()
            psa = ps(ph, "p4_psa")
            psb = ps(ph, "p4_psb")
            r_psa, r_psb = Res(), Res()
            nbm = 1 if moe_j is not None else 2
            psm = [ps(ph, f"p4_psm{i}") for i in range(nbm * (D // 512))]
            r_psm = [Res() for _ in range(nbm * (D // 512))]
            em.dma("pool", Wa[:], w_ba[l].rearrange("(k p) d -> p k d", p=128), pwrites=[r_W])
            em.dma("pool", Wb[:], w_bb[l].rearrange("(k p) d -> p k d", p=128), pwrites=[r_W])
            for kt in range(KT):
                em.dma("pool", Wo[:, kt, :], w_out[l][kt * 128:(kt + 1) * 128, :], pwrites=[r_W])
            after = None
            if moe_j is not None:
                rt = sb(ph, "p4_rt", [128, KT, NEXP], F32)
                r_rt = Res()
                em.dma("sp", rt[:], moe_router[moe_j].rearrange("(k p) e -> p k e", p=128), writes=[r_rt])
                tpf = ps(ph, "p4_tpf", [128, D], F32)
                r_tpf = Res()
                xTf = sb(ph, "p4_xTf", [128, D], F32)
                r_xTf = Res()
                pcs = ps(ph, "p4_pcs")
                r_pcs = Res()
                zt = sb(ph, "p4_zt", [128, 2 * D], BF16)
                r_zt = Res()
                r_zf = Res()
                em.op("pool", lambda e: e.memset(zt[:], 0.0), writes=[r_zt])
                for r0 in range(0, NEXP * CAP, 256):
                    em.dma("sp", xe_d[r0:r0 + 256, :].rearrange("(p a) d -> p (a d)", a=2), zt[:], reads=[r_zt],
                           pwrites=[R["xe_d"], r_zf])
                mall = sb(ph, "p4_mall", [128, NTQ, NEXP], F32)
                r_mall = Res()
                gi = sb(ph, "p4_gi", [128, NTQ, 2], F32)
                ii = sb(ph, "p4_ii", [128, NTQ, 2], I32)
                r_gi, r_ii = Res(), Res()
                eoff_i = sb(ph, "p4_eoffi", [128, NEXP], I32)
                eoff = sb(ph, "p4_eoff", [128, NEXP], F32)
                r_eoff = Res()
                em.op("pool", lambda e: e.iota(eoff_i[:], pattern=[[CAP, NEXP]], base=0, channel_multiplier=0),
                      writes=[r_eoff])
                em.op("dve", lambda e: e.tensor_copy(out=eoff[:], in_=eoff_i[:]), reads=[r_eoff], writes=[r_eoff])
                s8 = {n: sb(ph, "p4_" + n, [128, NEXP], F32) for n in ("lg", "is1", "l2", "is2", "pos", "v", "ov", "tmp")}
                s1 = {n: sb(ph, "p4_" + n, [128, 1], F32) for n in ("m1", "m2", "d")}
                r_s = {n: Res() for n in list(s8) + list(s1)}

                def after(i, xo, r_xo, xb, r_xb):
                    for kt in range(KT):
                        em.op("pe", lambda e: e.transpose(tpf[:, kt * 128:(kt + 1) * 128],
                                                          xo[:, kt * 128:(kt + 1) * 128], ident_f[:]),
                              reads=[r_xo, r_const], writes=[r_tpf] if kt == 0 else [],
                              pwrites=[] if kt == 0 else [r_tpf], inc=(kt == KT - 1))
                    em.op("act", lambda e: e.activation(out=xTf[:], in_=tpf[:], func=AF.Copy),
                          reads=[r_tpf], writes=[r_xTf])
                    for kt in range(KT):
                        em.op("pe", lambda e: e.matmul(pcs[:, 0:NEXP], lhsT=xTf[:, kt * 128:(kt + 1) * 128],
                                                       rhs=rt[:, kt, :], start=(kt == 0), stop=(kt == KT - 1)),
                              reads=[r_xTf, r_rt], writes=[r_pcs] if kt == 0 else [],
                              pwrites=[] if kt == 0 else [r_pcs], inc=(kt == KT - 1))
                    em.op("dve", lambda e: e.tensor_copy(out=s8["lg"][:], in_=pcs[:, 0:NEXP]),
                          reads=[r_pcs], writes=[r_s["lg"]])
                    em.op("dve", lambda e: e.tensor_reduce(out=s1["m1"][:], in_=s8["lg"][:], axis=AX.X, op=ALU.max),
                          reads=[r_s["lg"]], writes=[r_s["m1"]])
                    em.op("dve", lambda e: e.tensor_scalar(out=s8["is1"][:], in0=s8["lg"][:], scalar1=s1["m1"][:, 0:1],
                                                           scalar2=None, op0=ALU.is_equal),
                          reads=[r_s["lg"], r_s["m1"]], writes=[r_s["is1"]])
                    em.op("dve", lambda e: e.scalar_tensor_tensor(out=s8["l2"][:], in0=s8["is1"][:], scalar=-1e30,
                                                                  in1=s8["lg"][:], op0=ALU.mult, op1=ALU.add),
                          reads=[r_s["is1"], r_s["lg"]], writes=[r_s["l2"]])
                    em.op("dve", lambda e: e.tensor_reduce(out=s1["m2"][:], in_=s8["l2"][:], axis=AX.X, op=ALU.max),
                          reads=[r_s["l2"]], writes=[r_s["m2"]])
                    em.op("dve", lambda e: e.tensor_scalar(out=s8["is2"][:], in0=s8["l2"][:], scalar1=s1["m2"][:, 0:1],
                                                           scalar2=None, op0=ALU.is_equal),
                          reads=[r_s["l2"], r_s["m2"]], writes=[r_s["is2"]])
                    em.op("dve", lambda e: e.tensor_tensor(out=s1["d"][:], in0=s1["m2"][:], in1=s1["m1"][:],
                                                           op=ALU.subtract),
                          reads=[r_s["m1"], r_s["m2"]], writes=[r_s["d"]])
                    em.op("act", lambda e: e.activation(out=gi[:, i, 1:2], in_=s1["d"][:], func=AF.Sigmoid),
                          reads=[r_s["d"]], pwrites=[r_gi])
                    em.op("dve", lambda e: e.tensor_scalar(out=gi[:, i, 0:1], in0=gi[:, i, 1:2], scalar1=-1.0, scalar2=1.0,
                                                           op0=ALU.mult, op1=ALU.add),
                          reads=[r_gi], pwrites=[r_gi])
                    em.op("dve", lambda e: e.tensor_tensor(out=mall[:, i, :], in0=s8["is1"][:], in1=s8["is2"][:],
                                                           op=ALU.add),
                          reads=[r_s["is1"], r_s["is2"]], pwrites=[r_mall])
                    for j in range(i + 1):
                        em.op("pe", lambda e: e.matmul(pcs[:, 8:8 + NEXP], lhsT=(tri_f if j == i else ones_f)[:, :],
                                                       rhs=mall[:, j, :], start=(j == 0), stop=(j == i)),
                              reads=[r_mall, r_const], writes=[r_pcs] if j == 0 else [],
                              pwrites=[] if j == 0 else [r_pcs], inc=(j == i))
                    em.op("dve", lambda e: e.tensor_scalar(out=s8["pos"][:], in0=pcs[:, 8:8 + NEXP], scalar1=-1.0,
                                                           scalar2=None, op0=ALU.add),
                          reads=[r_pcs], writes=[r_s["pos"]])
                    em.op("dve", lambda e: e.tensor_scalar(out=s8["ov"][:], in0=s8["pos"][:], scalar1=float(CAP),
                                                           scalar2=1e6, op0=ALU.is_ge, op1=ALU.mult),
                          reads=[r_s["pos"]], writes=[r_s["ov"]])
                    em.op("dve", lambda e: e.tensor_tensor(out=s8["v"][:], in0=s8["pos"][:], in1=eoff[:], op=ALU.add),
                          reads=[r_s["pos"], r_eoff], writes=[r_s["v"]])
                    em.op("dve", lambda e: e.tensor_tensor(out=s8["v"][:], in0=s8["v"][:], in1=s8["ov"][:], op=ALU.add),
                          reads=[r_s["v"], r_s["ov"]], writes=[r_s["v"]])
                    for w_, nm in enumerate(("is1", "is2")):
                        em.op("dve", lambda e: e.tensor_tensor(out=s8["tmp"][:], in0=s8[nm][:], in1=s8["v"][:],
                                                               op=ALU.mult),
                              reads=[r_s[nm], r_s["v"]], writes=[r_s["tmp"]])
                        em.op("dve", lambda e: e.tensor_reduce(out=s1["m1"][:], in_=s8["tmp"][:], axis=AX.X, op=ALU.add),
                              reads=[r_s["tmp"]], writes=[r_s["m1"]])
                        em.op("dve", lambda e: e.tensor_copy(out=ii[:, i, w_:w_ + 1], in_=s1["m1"][:]),
                              reads=[r_s["m1"]], pwrites=[r_ii])
                        em.dma_fn("pool", lambda g: g.indirect_dma_start(
                            out=xe_d[:, :], out_offset=bass.IndirectOffsetOnAxis(ap=ii[:, i, w_:w_ + 1], axis=0),
                            in_=xb[:, :], in_offset=None, bounds_check=bc_reg[0], oob_is_err=False),
                            reads=[r_ii, r_xb, r_zf], pwrites=[R["xe_d"]])

            xres_d = x_in if l == 0 else xcur_d

            def load_in(tb):
                b = tb % 2
                tsl = slice(tb * 512, (tb + 1) * 512)
                em.dma("sp", oa[b][:], oaT_d.rearrange("(k p) t -> p k t", p=128)[:, :, tsl], reads=[R["oaT_d"]],
                       writes=[r_in[b]])
                em.dma("sp", ob[b][:], obT_d.rearrange("(k p) t -> p k t", p=128)[:, :, tsl], reads=[R["obT_d"]],
                       pwrites=[r_in[b]])
                em.dma("sp", sga[b][:], sgaT_d.rearrange("(k p) t -> p k t", p=128)[:, :, tsl], reads=[R["sgaT_d"]],
                       pwrites=[r_in[b]])
                em.dma("sp", sgb[b][:], sgbT_d.rearrange("(k p) t -> p k t", p=128)[:, :, tsl], reads=[R["sgbT_d"]],
                       pwrites=[r_in[b]])

            load_in(0)
            for tb in range(NTB):
                if tb + 1 < NTB:
                    load_in(tb + 1)
                b = tb % 2
                for dm in range(KT):
                    for k in range(4):
                        em.op("pe", lambda e: e.matmul(psa[:, :], lhsT=Wa[:, k, dm * 128:(dm + 1) * 128],
                                                       rhs=oa[b][:, k, :], start=(k == 0), stop=(k == 3)),
                              reads=[r_W, r_in[b]], writes=[r_psa] if k == 0 else [], pwrites=[] if k == 0 else [r_psa],
                              inc=(k == 3))
                    for k in range(4):
                        em.op("pe", lambda e: e.matmul(psb[:, :], lhsT=Wb[:, k, dm * 128:(dm + 1) * 128],
                                                       rhs=ob[b][:, k, :], start=(k == 0), stop=(k == 3)),
                              reads=[r_W, r_in[b]], writes=[r_psb] if k == 0 else [], pwrites=[] if k == 0 else [r_psb],
                              inc=(k == 3))
                    em.op("dve", lambda e: e.tensor_tensor(out=t1[:], in0=psa[:, :], in1=sga[b][:, dm, :], op=ALU.mult),
                          reads=[r_psa, r_in[b]], writes=[r_t1])
                    em.op("dve", lambda e: e.tensor_tensor(out=t2[:], in0=psb[:, :], in1=sgb[b][:, dm, :], op=ALU.mult),
                          reads=[r_psb, r_in[b]], writes=[r_t2])
                    em.op("pool", lambda e: e.tensor_tensor(out=mg[:, dm, :], in0=t1[:], in1=t2[:], op=ALU.add),
                          reads=[r_t1, r_t2], writes=[r_mg] if dm == 0 else [], pwrites=[] if dm == 0 else [r_mg])
                for qi in range(4):
                    i = tb * 4 + qi
                    halves = []
                    for hf in range(D // 512):
                        pi = (i % nbm) * (D // 512) + hf
                        for k in range(KT):
                            em.op("pe", lambda e: e.matmul(psm[pi][:, :], lhsT=mg[:, k, qi * 128:(qi + 1) * 128],
                                                           rhs=Wo[:, k, hf * 512:(hf + 1) * 512], start=(k == 0),
                                                           stop=(k == KT - 1)),
                                  reads=[r_mg, r_W], writes=[r_psm[pi]] if k == 0 else [],
                                  pwrites=[] if k == 0 else [r_psm[pi]], inc=(k == KT - 1))
                        halves.append((psm[pi][:, :], r_psm[pi]))
                    ln_stage(lo_, i, halves, xres_d, x1_d, R["x1_d"], x1T_d, R["x1T_d"], after)
            if moe_j is not None:
                em.dma("sp", moe_g_d[:, :], gi[:].rearrange("p a b -> p (a b)"), reads=[r_gi], pwrites=[R["moe_d"]])
                em.dma("sp", moe_i_d[:, :], ii[:].rearrange("p a b -> p (a b)"), reads=[r_ii], pwrites=[R["moe_d"]])
            em.barrier()

    def phase_ffn(l, experts, gated, final):
        with ExitStack() as ph:
            lo_ = ln_alloc(ph, "p5", ln_ffn_g[l], ln_ffn_b[l])
            NQ = TBF // 128
            x1T = sb(ph, "p5_xT", [128, KT, TBF], BF16)
            r_x1T = Res()
            yacc = sb(ph, "p5_yacc", [128, NQ, D], F32)
            r_yacc = Res()
            hT = [sb(ph, f"p5_hT{i}", [128, 4, TBF], BF16) for i in range(2)]
            r_hT = [Res() for _ in range(2)]
            Wg = [sb(ph, f"p5_Wg{i}", [128, KT, 512], BF16) for i in range(2)]
            Wu = [sb(ph, f"p5_Wu{i}", [128, KT, 512], BF16) for i in range(2)]
            Wd = [sb(ph, f"p5_Wd{i}", [128, 4, D], BF16) for i in range(2)]
            r_Wc = [Res() for _ in range(2)]
            gbc = sb(ph, "p5_gbc", [128, TBF], F32)
            r_gbc = Res()
            sg = [sb(ph, f"p5_sg{i}", [128, 512], F32) for i in range(2)]
            r_sg = [Res() for _ in range(2)]
            tm = [sb(ph, f"p5_tm{i}", [128, 512], F32) for i in range(2)]
            r_tm = [Res() for _ in range(2)]
            psg = [ps(ph, f"p5_pg{i}") for i in range(2)]
            psu = [ps(ph, f"p5_pu{i}") for i in range(2)]
            psd = [ps(ph, f"p5_pd{i}") for i in range(2)]
            r_psg = [Res() for _ in range(2)]
            r_psu = [Res() for _ in range(2)]
            r_psd = [Res() for _ in range(2)]
            NCH = FT // 4
            work = [(tbf, ei, c) for tbf in range(NTBF) for ei in range(len(experts)) for c in range(NCH)]
            ctr = {"p": 0, "d": 0}

            def load_chunk(n):
                tbf, ei, c = work[n]
                wg_d, wu_d, wd_d = experts[ei]
                b = n % 2
                em.dma("pool", Wg[b][:], wg_d[:, c * 512:(c + 1) * 512].rearrange("(k p) f -> p k f", p=128),
                       writes=[r_Wc[b]])
                em.dma("pool", Wu[b][:], wu_d[:, c * 512:(c + 1) * 512].rearrange("(k p) f -> p k f", p=128),
                       pwrites=[r_Wc[b]])
                em.dma("pool", Wd[b][:], wd_d[c * 512:(c + 1) * 512, :].rearrange("(k p) d -> p k d", p=128),
                       pwrites=[r_Wc[b]])

            load_chunk(0)
            for n, (tbf, ei, c) in enumerate(work):
                if n + 1 < len(work):
                    load_chunk(n + 1)
                b = n % 2
                t0 = tbf * TBF
                if ei == 0 and c == 0:
                    em.dma("sp", x1T[:], x1T_d.rearrange("(k p) t -> p k t", p=128)[:, :, t0:t0 + TBF],
                           reads=[R["x1T_d"]], writes=[r_x1T])
                if gated and c == 0:
                    em.dma("sp", gbc[:], gateT_d[ei, t0:t0 + TBF].partition_broadcast(128), reads=[R["gateT_d"]],
                           writes=[r_gbc])
                for f4 in range(4):
                    for tsub in range(TBF // 512):
                        p = ctr["p"] % 2
                        ctr["p"] += 1
                        tsl = slice(tsub * 512, (tsub + 1) * 512)
                        for kt in range(KT):
                            em.op("pe", lambda e: e.matmul(psg[p][:, :], lhsT=Wg[b][:, kt, f4 * 128:(f4 + 1) * 128],
                                                           rhs=x1T[:, kt, tsl], start=(kt == 0), stop=(kt == KT - 1)),
                                  reads=[r_Wc[b], r_x1T], writes=[r_psg[p]] if kt == 0 else [],
                                  pwrites=[] if kt == 0 else [r_psg[p]], inc=(kt == KT - 1))
                        for kt in range(KT):
                            em.op("pe", lambda e: e.matmul(psu[p][:, :], lhsT=Wu[b][:, kt, f4 * 128:(f4 + 1) * 128],
                                                           rhs=x1T[:, kt, tsl], start=(kt == 0), stop=(kt == KT - 1)),
                                  reads=[r_Wc[b], r_x1T], writes=[r_psu[p]] if kt == 0 else [],
                                  pwrites=[] if kt == 0 else [r_psu[p]], inc=(kt == KT - 1))
                        em.op("act", lambda e: e.activation(out=sg[p][:], in_=psg[p][:, :], func=AF.Silu),
                              reads=[r_psg[p]], writes=[r_sg[p]])
                        first_h = (f4 == 0 and tsub == 0)
                        if gated:
                            em.op("dve", lambda e: e.tensor_tensor(out=tm[p][:], in0=psu[p][:, :], in1=sg[p][:],
                                                                   op=ALU.mult),
                                  reads=[r_psu[p], r_sg[p]], writes=[r_tm[p]])
                            em.op("pool", lambda e: e.tensor_tensor(out=hT[b][:, f4, tsl], in0=tm[p][:],
                                                                    in1=gbc[:, tsl], op=ALU.mult),
                                  reads=[r_tm[p], r_gbc], writes=[r_hT[b]] if first_h else [],
                                  pwrites=[] if first_h else [r_hT[b]])
                        else:
                            em.op("dve", lambda e: e.tensor_tensor(out=hT[b][:, f4, tsl], in0=psu[p][:, :], in1=sg[p][:],
                                                                   op=ALU.mult),
                                  reads=[r_psu[p], r_sg[p]], writes=[r_hT[b]] if first_h else [],
                                  pwrites=[] if first_h else [r_hT[b]])
                first_acc = (ei == 0 and c == 0)
                for q in range(NQ):
                    for hf in range(D // 512):
                        d = ctr["d"] % 2
                        ctr["d"] += 1
                        for f4 in range(4):
                            em.op("pe", lambda e: e.matmul(psd[d][:, :], lhsT=hT[b][:, f4, q * 128:(q + 1) * 128],
                                                           rhs=Wd[b][:, f4, hf * 512:(hf + 1) * 512], start=(f4 == 0),
                                                           stop=(f4 == 3)),
                                  reads=[r_hT[b], r_Wc[b]], writes=[r_psd[d]] if f4 == 0 else [],
                                  pwrites=[] if f4 == 0 else [r_psd[d]], inc=(f4 == 3))
                        ysl = yacc[:, q, hf * 512:(hf + 1) * 512]
                        if first_acc:
                            em.op("dve", lambda e: e.tensor_copy(out=ysl, in_=psd[d][:, :]),
                                  reads=[r_psd[d]], writes=[r_yacc] if (q == 0 and hf == 0) else [],
                                  pwrites=[] if (q == 0 and hf == 0) else [r_yacc])
                        else:
                            em.op("dve", lambda e: e.tensor_tensor(out=ysl, in0=ysl, in1=psd[d][:, :], op=ALU.add),
                                  reads=[r_psd[d], r_yacc], pwrites=[r_yacc])
                if ei == len(experts) - 1 and c == NCH - 1:
                    for q in range(NQ):
                        i = tbf * NQ + q
                        halves = [(yacc[:, q, hf * 512:(hf + 1) * 512], r_yacc) for hf in range(D // 512)]
                        if final:
                            ln_stage(lo_, i, halves, x1_d, out_d, R["out_d"], None, None)
                        else:
                            ln_stage(lo_, i, halves, x1_d, xcur_d, R["xcur_d"], xT_d, R["xT_d"])
            em.barrier()

    def phase_moe(l, j, final):
        NQ = CAP // 128
        NSUB = CAP // 512
        NCH = FT // 4
        with ExitStack() as ph:
            xeT = sb(ph, "pm_xeT", [128, KT, CAP], BF16)
            r_xeT = Res()
            yacc = sb(ph, "pm_yacc", [128, NQ, D], F32)
            r_yacc = Res()
            hT = [sb(ph, f"pm_hT{i}", [128, 4, CAP], BF16) for i in range(2)]
            r_hT = [Res() for _ in range(2)]
            Wg = [sb(ph, f"pm_Wg{i}", [128, KT, 512], BF16) for i in range(2)]
            Wu = [sb(ph, f"pm_Wu{i}", [128, KT, 512], BF16) for i in range(2)]
            Wd = [sb(ph, f"pm_Wd{i}", [128, 4, D], BF16) for i in range(2)]
            r_Wc = [Res() for _ in range(2)]
            sg = [sb(ph, f"pm_sg{i}", [128, 512], F32) for i in range(2)]
            r_sg = [Res() for _ in range(2)]
            xr = [sb(ph, f"pm_xr{i}", [128, D], BF16) for i in range(2)]
            r_xr = [Res() for _ in range(2)]
            tp = ps(ph, "pm_tp", [128, D], BF16)
            r_tp = Res()
            psg = [ps(ph, f"pm_pg{i}") for i in range(2)]
            psu = [ps(ph, f"pm_pu{i}") for i in range(2)]
            psd = [ps(ph, f"pm_pd{i}") for i in range(2)]
            r_psg = [Res() for _ in range(2)]
            r_psu = [Res() for _ in range(2)]
            r_psd = [Res() for _ in range(2)]
            work = [(e, c) for e in range(NEXP) for c in range(NCH)]
            ctr = {"p": 0, "d": 0, "x": 0}

            def load_chunk(n):
                e, c = work[n]
                b = n % 2
                em.dma("pool", Wg[b][:], moe_wg[j][e][:, c * 512:(c + 1) * 512].rearrange("(k p) f -> p k f", p=128),
                       writes=[r_Wc[b]])
                em.dma("pool", Wu[b][:], moe_wu[j][e][:, c * 512:(c + 1) * 512].rearrange("(k p) f -> p k f", p=128),
                       pwrites=[r_Wc[b]])
                em.dma("pool", Wd[b][:], moe_wd[j][e][c * 512:(c + 1) * 512, :].rearrange("(k p) d -> p k d", p=128),
                       pwrites=[r_Wc[b]])

            load_chunk(0)
            for n, (e_, c) in enumerate(work):
                if n + 1 < len(work):
                    load_chunk(n + 1)
                b = n % 2
                if c == 0:
                    for q in range(NQ):
                        xb_ = ctr["x"] % 2
                        ctr["x"] += 1
                        r0 = e_ * CAP + q * 128
                        em.dma("sp", xr[xb_][:], xe_d[r0:r0 + 128, :], reads=[R["xe_d"]], writes=[r_xr[xb_]])
                        for kt in range(KT):
                            em.op("pe", lambda e: e.transpose(tp[:, kt * 128:(kt + 1) * 128],
                                                              xr[xb_][:, kt * 128:(kt + 1) * 128], ident_b[:]),
                                  reads=[r_xr[xb_], r_const], writes=[r_tp] if kt == 0 else [],
                                  pwrites=[] if kt == 0 else [r_tp], inc=(kt == KT - 1))
                        em.op("dve", lambda e: e.tensor_copy(out=xeT[:, :, q * 128:(q + 1) * 128],
                                                             in_=tp[:].rearrange("p (k c) -> p k c", c=128)),
                              reads=[r_tp], writes=[r_xeT] if q == 0 else [], pwrites=[] if q == 0 else [r_xeT])
                for f4 in range(4):
                    for tsub in range(NSUB):
                        p = ctr["p"] % 2
                        ctr["p"] += 1
                        tsl = slice(tsub * 512, (tsub + 1) * 512)
                        for kt in range(KT):
                            em.op("pe", lambda e: e.matmul(psg[p][:, :], lhsT=Wg[b][:, kt, f4 * 128:(f4 + 1) * 128],
                                                           rhs=xeT[:, kt, tsl], start=(kt == 0), stop=(kt == KT - 1)),
                                  reads=[r_Wc[b], r_xeT], writes=[r_psg[p]] if kt == 0 else [],
                                  pwrites=[] if kt == 0 else [r_psg[p]], inc=(kt == KT - 1))
                        for kt in range(KT):
                            em.op("pe", lambda e: e.matmul(psu[p][:, :], lhsT=Wu[b][:, kt, f4 * 128:(f4 + 1) * 128],
                                                           rhs=xeT[:, kt, tsl], start=(kt == 0), stop=(kt == KT - 1)),
                                  reads=[r_Wc[b], r_xeT], writes=[r_psu[p]] if kt == 0 else [],
                                  pwrites=[] if kt == 0 else [r_psu[p]], inc=(kt == KT - 1))
                        em.op("act", lambda e: e.activation(out=sg[p][:], in_=psg[p][:, :], func=AF.Silu),
                              reads=[r_psg[p]], writes=[r_sg[p]])
                        first_h = (f4 == 0 and tsub == 0)
                        em.op("dve", lambda e: e.tensor_tensor(out=hT[b][:, f4, tsl], in0=psu[p][:, :], in1=sg[p][:],
                                                               op=ALU.mult),
                              reads=[r_psu[p], r_sg[p]], writes=[r_hT[b]] if first_h else [],
                              pwrites=[] if first_h else [r_hT[b]])
                for q in range(NQ):
                    for hf in range(D // 512):
                        d = ctr["d"] % 2
                        ctr["d"] += 1
                        for f4 in range(4):
                            em.op("pe", lambda e: e.matmul(psd[d][:, :], lhsT=hT[b][:, f4, q * 128:(q + 1) * 128],
                                                           rhs=Wd[b][:, f4, hf * 512:(hf + 1) * 512], start=(f4 == 0),
                                                           stop=(f4 == 3)),
                                  reads=[r_hT[b], r_Wc[b]], writes=[r_psd[d]] if f4 == 0 else [],
                                  pwrites=[] if f4 == 0 else [r_psd[d]], inc=(f4 == 3))
                        ysl = yacc[:, q, hf * 512:(hf + 1) * 512]
                        if c == 0:
                            em.op("act", lambda e: e.activation(out=ysl, in_=psd[d][:, :], func=AF.Copy),
                                  reads=[r_psd[d]], writes=[r_yacc] if (q == 0 and hf == 0) else [],
                                  pwrites=[] if (q == 0 and hf == 0) else [r_yacc])
                        else:
                            em.op("dve", lambda e: e.tensor_tensor(out=ysl, in0=ysl, in1=psd[d][:, :], op=ALU.add),
                                  reads=[r_psd[d], r_yacc], pwrites=[r_yacc])
                if c == NCH - 1:
                    em.dma("sp", ye_d[e_ * CAP:(e_ + 1) * CAP, :].rearrange("(q p) d -> p q d", p=128), yacc[:],
                           reads=[r_yacc], pwrites=[R["ye_d"]])
            em.barrier()
        with ExitStack() as ph:
            lo_ = ln_alloc(ph, "pc", ln_ffn_g[l], ln_ffn_b[l])
            gi = sb(ph, "pc_gi", [128, NTQ, 2], F32)
            ii = sb(ph, "pc_ii", [128, NTQ, 2], I32)
            r_gi = Res()
            em.dma("sp", gi[:].rearrange("p a b -> p (a b)"), moe_g_d[:, :], reads=[R["moe_d"]], pwrites=[r_gi])
            em.dma("sp", ii[:].rearrange("p a b -> p (a b)"), moe_i_d[:, :], reads=[R["moe_d"]], pwrites=[r_gi])
            y1 = [sb(ph, f"pc_y1{i}", [128, D], F32) for i in range(2)]
            y2 = [sb(ph, f"pc_y2{i}", [128, D], F32) for i in range(2)]
            r_y1 = [Res() for _ in range(2)]
            r_y2 = [Res() for _ in range(2)]
            for i in range(2):
                em.op("pool", lambda e: e.memset(y1[i][:], 0.0), writes=[r_y1[i]])
                em.op("pool", lambda e: e.memset(y2[i][:], 0.0), writes=[r_y2[i]])
            for i in range(NTQ):
                b = i % 2
                em.dma_fn("pool", lambda g: g.indirect_dma_start(
                    out=y1[b][:, :], out_offset=None, in_=ye_d[:, :],
                    in_offset=bass.IndirectOffsetOnAxis(ap=ii[:, i, 0:1], axis=0),
                    bounds_check=bc_reg[0], oob_is_err=False),
                    reads=[R["ye_d"], r_gi], writes=[r_y1[b]])
                em.dma_fn("pool", lambda g: g.indirect_dma_start(
                    out=y2[b][:, :], out_offset=None, in_=ye_d[:, :],
                    in_offset=bass.IndirectOffsetOnAxis(ap=ii[:, i, 1:2], axis=0),
                    bounds_check=bc_reg[0], oob_is_err=False),
                    reads=[R["ye_d"], r_gi], writes=[r_y2[b]])
                em.op("dve", lambda e: e.tensor_scalar(out=y1[b][:], in0=y1[b][:], scalar1=gi[:, i, 0:1], scalar2=None,
                                                       op0=ALU.mult), reads=[r_y1[b], r_gi], writes=[r_y1[b]])
                em.op("dve", lambda e: e.scalar_tensor_tensor(out=y1[b][:], in0=y2[b][:], scalar=gi[:, i, 1:2],
                                                              in1=y1[b][:], op0=ALU.mult, op1=ALU.add),
                      reads=[r_y1[b], r_y2[b], r_gi], writes=[r_y1[b]])
                halves = [(y1[b][:, hf * 512:(hf + 1) * 512], r_y1[b]) for hf in range(D // 512)]
                if final:
                    ln_stage(lo_, i, halves, x1_d, out_d, R["out_d"], None, None)
                else:
                    ln_stage(lo_, i, halves, x1_d, xcur_d, R["xcur_d"], xT_d, R["xT_d"])
            em.barrier()

    bc_reg[0] = nc.gpsimd.to_reg(NEXP * CAP - 1)
    phase_tables()
    phase_x0()
    for l in range(DEPTH):
        j = l // 2
        is_moe = (l % 2 == 1)
        phase_proj(l)
        phase_fox()
        phase_dsa()
        phase_merge(l, j if is_moe else None)
        if is_moe:
            phase_moe(l, j, l == DEPTH - 1)
        else:
            phase_ffn(l, [(ffn_wg[j], ffn_wu[j], ffn_wd[j])], False, l == DEPTH - 1)
    top.close()
    return nc, em


_INPUT_ORDER = ["x", "positions", "w_in", "b_forget", "w_branch_a", "w_branch_b", "w_out", "ln_mix_g", "ln_mix_b",
                "ln_ffn_g", "ln_ffn_b", "ffn_w_gate", "ffn_w_up", "ffn_w_down", "moe_router", "moe_w_gate",
                "moe_w_up", "moe_w_down"]


def kernel(**inputs):
    x = np.asarray(inputs["x"])
    B, T, D = x.shape
    depth = int(np.asarray(inputs["w_in"]).shape[0])
    DFF = int(np.asarray(inputs["ffn_w_gate"]).shape[-1])
    nc, em = build_program(T, D, DFF, depth)
    shared = {k: np.ascontiguousarray(np.asarray(inputs[k])) for k in _INPUT_ORDER if k not in ("x", "positions")}
    in_maps = []
    for b in range(B):
        m = dict(shared)
        m["x"] = np.ascontiguousarray(x[b])
        m["positions"] = np.ascontiguousarray(np.asarray(inputs["positions"])[b:b + 1]).astype(np.int32)
        in_maps.append(m)
    res = run_bass_kernel_spmd(nc, in_maps, core_ids=list(range(B)))
    return np.stack([np.asarray(r["out"]) for r in res.results], axis=0).astype(np.float32)
```

```python
import math
from contextlib import ExitStack

import numpy as np
import concourse.bass as bass
import concourse.mybir as mybir
from concourse.bass_utils import run_bass_kernel_spmd

F32 = mybir.dt.float32
BF16 = mybir.dt.bfloat16
I32 = mybir.dt.int32
AF = mybir.ActivationFunctionType
ALU = mybir.AluOpType
AX = mybir.AxisListType

N_CORES = 8
PROJ_SIZES = (512, 512, 512, 512, 64, 8, 512, 512, 512, 8, 1024, 1024)
PROJ_TOTAL = sum(PROJ_SIZES)
OFF = dict(dq=0, dk=512, dv=1024, iq=1536, ik=2048, iw=2112, fq=2120, fk=2632, fv=3144,
           fl=3656, ga=3664, gb=4688)
ROPE_THETA = 10000.0
LN_EPS = 1e-5
NEG = -30000.0


class Res:
    __slots__ = ("name", "w", "r", "x")

    def __init__(self, name=""):
        self.name = name
        self.w = {}
        self.r = {}
        self.x = None


def _key(tok):
    return tok[:2] if tok[0] == "c" else tok[:3]


class Emitter:
    COMPUTE = ("pe", "act", "dve", "pool")
    QUEUES = ("sp", "act", "pool")

    def __init__(self, nc, stack, n_dma_sems=10):
        self.nc = nc
        self.engs = {"pe": nc.tensor, "act": nc.scalar, "dve": nc.vector,
                     "pool": nc.gpsimd, "sp": nc.sync}
        self.sem = {e: stack.enter_context(nc.semaphore("c_" + e)) for e in self.COMPUTE}
        self.cnt = {e: 0 for e in self.COMPUTE}
        self.dsem = {q: [stack.enter_context(nc.semaphore(f"d_{q}{i}")) for i in range(n_dma_sems)]
                     for q in self.QUEUES}
        self.dcnt = {q: 0 for q in self.QUEUES}
        self.nd = n_dma_sems
        self.seen = {e: {} for e in self.engs}
        self.pend = []
        self.n_ins = 0
        self.n_wait = 0

    def _wait(self, e, tok):
        if tok[0] == "c":
            _, f, n = tok
            if f == "pe" and e == "pe":
                return
            key = ("c", f)
            s = self.sem[f]
        else:
            _, q, idx, n = tok
            key = ("d", q, idx)
            s = self.dsem[q][idx]
        if self.seen[e].get(key, 0) >= n:
            return
        self.seen[e][key] = n
        self.pend.append((s, n))

    def _deps(self, e, reads, writes, pwrites):
        for r in reads:
            for t in r.w.values():
                self._wait(e, t)
        for w in writes:
            for t in w.w.values():
                self._wait(e, t)
            for t in w.r.values():
                self._wait(e, t)
        for w in pwrites:
            for t in w.r.values():
                self._wait(e, t)
            if w.x is not None:
                self._wait(e, w.x)

    def _record(self, tok, reads, writes, pwrites):
        k = _key(tok)
        for r in reads:
            r.r[k] = tok
        for w in writes:
            w.w = {k: tok}
            w.r = {}
            w.x = tok
        for w in pwrites:
            w.w[k] = tok

    def _flush(self, e, ins_fn):
        pend, self.pend = self.pend, []
        if pend:
            for (s, n) in pend[:-1]:
                self.engs[e].wait_ge(s, n)
                self.n_wait += 1
            ins = ins_fn()
            ins._wait_ge(pend[-1][0], pend[-1][1])
            return ins
        return ins_fn()

    def op(self, e, fn, reads=(), writes=(), pwrites=(), inc=True):
        self._deps(e, reads, writes, pwrites)
        ins = self._flush(e, lambda: fn(self.engs[e]))
        if inc:
            self.cnt[e] += 1
            ins.then_inc(self.sem[e], 1)
            tok = ("c", e, self.cnt[e])
        else:
            tok = ("c", e, self.cnt[e] + 1)
        self._record(tok, reads, writes, pwrites)
        self.n_ins += 1
        return tok

    def dma(self, q, out, in_, reads=(), writes=(), pwrites=(), **kw):
        i = self.dcnt[q]
        idx = i % self.nd
        prev = 16 * (i // self.nd)
        if prev > 0:
            self._wait(q, ("d", q, idx, prev))
        self._deps(q, reads, writes, pwrites)
        ins = self._flush(q, lambda: self.engs[q].dma_start(out=out, in_=in_, **kw))
        ins.then_inc(self.dsem[q][idx], 16)
        self.dcnt[q] += 1
        tok = ("d", q, idx, prev + 16)
        self._record(tok, reads, writes, pwrites)
        self.n_ins += 1
        return tok

    def dma_fn(self, q, fn, reads=(), writes=(), pwrites=()):
        i = self.dcnt[q]
        idx = i % self.nd
        prev = 16 * (i // self.nd)
        if prev > 0:
            self._wait(q, ("d", q, idx, prev))
        self._deps(q, reads, writes, pwrites)
        ins = self._flush(q, lambda: fn(self.engs[q]))
        ins.then_inc(self.dsem[q][idx], 16)
        self.dcnt[q] += 1
        tok = ("d", q, idx, prev + 16)
        self._record(tok, reads, writes, pwrites)
        self.n_ins += 1
        return tok

    def barrier(self, engines=None):
        for e in (engines or list(self.engs)):
            for f in self.COMPUTE:
                if self.cnt[f] > 0:
                    self._wait(e, ("c", f, self.cnt[f])) if not (e == "pe" and f == "pe") else None
            if e == "pe" and self.cnt["pe"] > 0 and self.seen["pe"].get(("c", "pe"), 0) < self.cnt["pe"]:
                self.seen["pe"][("c", "pe")] = self.cnt["pe"]
                self.pend.append((self.sem["pe"], self.cnt["pe"]))
            for q in self.QUEUES:
                for idx in range(self.nd):
                    n_used = (self.dcnt[q] - idx + self.nd - 1) // self.nd if self.dcnt[q] > idx else 0
                    if n_used > 0:
                        self._wait(e, ("d", q, idx, 16 * n_used))
            pend, self.pend = self.pend, []
            for (s, n) in pend:
                self.engs[e].wait_ge(s, n)
                self.n_wait += 1


def build_program(T, D, DFF, DEPTH, NEXP=8, bisect_iters=13):
    assert T % 512 == 0 and D % 512 == 0 and DFF % 512 == 0
    KT = D // 128
    NTQ = T // 128
    NTB = T // 512
    FT = DFF // 128
    TOPK = min(256, T // 4)
    n_dense = (DEPTH + 1) // 2
    n_moe = DEPTH // 2
    alpha = (2.0 * DEPTH) ** 0.25
    TBF = 1024 if T % 1024 == 0 else 512
    NTBF = T // TBF

    nc = bass.Bass("TRN2", target_bir_lowering=False)

    def din(name, shape, dt=F32):
        return nc.dram_tensor(name, list(shape), dt, kind="ExternalInput").ap()

    def dscr(name, shape, dt):
        return nc.dram_tensor(name, list(shape), dt).ap()

    x_in = din("x", [T, D])
    pos_in = din("positions", [1, T], I32)
    w_in = din("w_in", [DEPTH, D, PROJ_TOTAL])
    b_forget = din("b_forget", [DEPTH, 8])
    w_ba = din("w_branch_a", [DEPTH, 512, D])
    w_bb = din("w_branch_b", [DEPTH, 512, D])
    w_out = din("w_out", [DEPTH, D, D])
    ln_mix_g = din("ln_mix_g", [DEPTH, D])
    ln_mix_b = din("ln_mix_b", [DEPTH, D])
    ln_ffn_g = din("ln_ffn_g", [DEPTH, D])
    ln_ffn_b = din("ln_ffn_b", [DEPTH, D])
    ffn_wg = din("ffn_w_gate", [n_dense, D, DFF])
    ffn_wu = din("ffn_w_up", [n_dense, D, DFF])
    ffn_wd = din("ffn_w_down", [n_dense, DFF, D])
    moe_router = din("moe_router", [max(n_moe, 1), D, NEXP])
    moe_wg = din("moe_w_gate", [max(n_moe, 1), NEXP, D, DFF])
    moe_wu = din("moe_w_up", [max(n_moe, 1), NEXP, D, DFF])
    moe_wd = din("moe_w_down", [max(n_moe, 1), NEXP, DFF, D])
    out_d = nc.dram_tensor("out", [T, D], F32, kind="ExternalOutput").ap()

    xT_d = dscr("xT_d", [D, T], BF16)
    x1T_d = dscr("x1T_d", [D, T], BF16)
    xcur_d = dscr("xcur_d", [T, D], F32)
    x1_d = dscr("x1_d", [T, D], F32)
    cosT_d = dscr("cosT_d", [128, T], F32)
    sinT_d = dscr("sinT_d", [128, T], F32)
    dqT_d = dscr("dqT_d", [512, T], BF16)
    dkT_d = dscr("dkT_d", [512, T], BF16)
    iqT_d = dscr("iqT_d", [512, T], BF16)
    ikT_d = dscr("ikT_d", [64, T], BF16)
    fqT_d = dscr("fqT_d", [512, T], BF16)
    fkT_d = dscr("fkT_d", [512, T], BF16)
    dv_d = dscr("dv_d", [T, 768], BF16)
    fv_d = dscr("fv_d", [T, 768], BF16)
    wabs_d = dscr("wabs_d", [128, NTQ * 8], F32)
    wsgn_d = dscr("wsgn_d", [128, NTQ * 8], F32)
    aug_d = dscr("aug_d", [96, T], BF16)
    sgaT_d = dscr("sgaT_d", [D, T], BF16)
    sgbT_d = dscr("sgbT_d", [D, T], BF16)
    oaT_d = dscr("oaT_d", [512, T], BF16)
    obT_d = dscr("obT_d", [512, T], BF16)
    gateT_d = dscr("gateT_d", [NEXP, T], F32)
    CAP = max(512, ((T * 3 // 8) + 511) // 512 * 512)
    xe_d = dscr("xe_d", [NEXP * CAP, D], BF16)
    ye_d = dscr("ye_d", [NEXP * CAP, D], F32)
    moe_g_d = dscr("moe_g_d", [128, NTQ * 2], F32)
    moe_i_d = dscr("moe_i_d", [128, NTQ * 2], I32)

    R = {n: Res(n) for n in ("xT_d", "x1T_d", "xcur_d", "x1_d", "tab_d", "dqT_d", "dkT_d", "iqT_d",
                             "ikT_d", "fqT_d", "fkT_d", "dv_d", "fv_d", "w_d", "aug_d", "sgaT_d",
                             "sgbT_d", "oaT_d", "obT_d", "gateT_d", "out_d", "xe_d", "ye_d", "moe_d")}

    top = ExitStack()
    em = Emitter(nc, top)

    uid = [0]
    bc_reg = [None]

    def sb(st, name, shape, dt):
        uid[0] += 1
        return st.enter_context(nc.sbuf_tensor(f"{name}_{uid[0]}", list(shape), dt))

    def ps(st, name, shape=(128, 512), dt=F32):
        uid[0] += 1
        return st.enter_context(nc.psum_tensor(f"{name}_{uid[0]}", list(shape), dt))

    ident_b = sb(top, "ident_b", [128, 128], BF16)
    ident_f = sb(top, "ident_f", [128, 128], F32)
    tri_b = sb(top, "tri_b", [128, 128], BF16)
    tri_f = sb(top, "tri_f", [128, 128], F32)
    ones_f = sb(top, "ones_f", [128, 128], F32)
    r_const = Res("const")

    with ExitStack() as ph:
        io_i = sb(ph, "io_i", [128, 128], I32)
        r_io = Res()
        em.op("pool", lambda e: e.iota(io_i[:], pattern=[[1, 128]], base=0, channel_multiplier=-1),
              writes=[r_io])
        em.op("dve", lambda e: e.tensor_scalar(out=ident_f[:], in0=io_i[:], scalar1=0.0, scalar2=None,
                                               op0=ALU.is_equal), reads=[r_io], pwrites=[r_const])
        em.op("dve", lambda e: e.tensor_scalar(out=ident_b[:], in0=io_i[:], scalar1=0.0, scalar2=None,
                                               op0=ALU.is_equal), reads=[r_io], pwrites=[r_const])
        em.op("dve", lambda e: e.tensor_scalar(out=tri_f[:], in0=io_i[:], scalar1=0.0, scalar2=None,
                                               op0=ALU.is_ge), reads=[r_io], pwrites=[r_const])
        em.op("dve", lambda e: e.tensor_scalar(out=tri_b[:], in0=io_i[:], scalar1=0.0, scalar2=None,
                                               op0=ALU.is_ge), reads=[r_io], pwrites=[r_const])
        em.op("dve", lambda e: e.memset(ones_f[:], 1.0), pwrites=[r_const])
        em.barrier()

    def phase_tables():
        with ExitStack() as ph:
            posi = sb(ph, "posi", [128, T], I32)
            ang = sb(ph, "ang", [128, T], F32)
            a2 = sb(ph, "a2", [128, T], F32)
            u = sb(ph, "u", [128, T], F32)
            ni = sb(ph, "ni", [128, T], I32)
            nf = sb(ph, "nf", [128, T], F32)
            ji = sb(ph, "ji", [128, 1], I32)
            jf = sb(ph, "jf", [128, 1], F32)
            invf = sb(ph, "invf", [128, 1], F32)
            r_pos, r_ang, r_a2, r_u, r_ni, r_nf, r_j, r_jf, r_inv = (Res() for _ in range(9))
            em.dma("sp", posi[:], pos_in.to_broadcast([128, T]), writes=[r_pos])
            for g in range(4):
                em.op("pool", lambda e: e.iota(ji[32 * g:32 * g + 32, :], pattern=[[0, 1]], base=0,
                                               channel_multiplier=1), pwrites=[r_j])
            em.op("dve", lambda e: e.tensor_copy(out=jf[:], in_=ji[:]), reads=[r_j], writes=[r_jf])
            em.op("act", lambda e: e.activation(out=invf[:], in_=jf[:], func=AF.Exp,
                                                scale=-math.log(ROPE_THETA) / 32.0),
                  reads=[r_jf], writes=[r_inv])
            em.op("dve", lambda e: e.tensor_copy(out=ang[:], in_=posi[:]), reads=[r_pos], writes=[r_ang])
            em.op("dve", lambda e: e.tensor_scalar(out=ang[:], in0=ang[:], scalar1=invf[:, 0:1], scalar2=None,
                                                   op0=ALU.mult), reads=[r_ang, r_inv], writes=[r_ang])
            C1 = 6.28125
            C2 = 2.0 * math.pi - C1
            PI_LO = 3.1415925
            for (dst, shift) in ((sinT_d, 0.0), (cosT_d, math.pi / 2)):
                em.op("dve", lambda e: e.tensor_scalar(out=a2[:], in0=ang[:], scalar1=shift, scalar2=None,
                                                       op0=ALU.add), reads=[r_ang], writes=[r_a2])
                em.op("dve", lambda e: e.tensor_scalar(out=u[:], in0=a2[:], scalar1=1.0 / (2 * math.pi),
                                                       scalar2=0.5, op0=ALU.mult, op1=ALU.add),
                      reads=[r_a2], writes=[r_u])
                em.op("dve", lambda e: e.tensor_copy(out=ni[:], in_=u[:]), reads=[r_u], writes=[r_ni])
                em.op("dve", lambda e: e.tensor_copy(out=nf[:], in_=ni[:]), reads=[r_ni], writes=[r_nf])
                em.op("dve", lambda e: e.scalar_tensor_tensor(out=a2[:], in0=nf[:], scalar=-C1, in1=a2[:],
                                                              op0=ALU.mult, op1=ALU.add),
                      reads=[r_nf, r_a2], writes=[r_a2])
                em.op("dve", lambda e: e.scalar_tensor_tensor(out=a2[:], in0=nf[:], scalar=-C2, in1=a2[:],
                                                              op0=ALU.mult, op1=ALU.add),
                      reads=[r_nf, r_a2], writes=[r_a2])
                em.op("dve", lambda e: e.tensor_scalar(out=u[:], in0=a2[:], scalar1=-math.pi,
                                                       scalar2=2 * math.pi, op0=ALU.is_lt, op1=ALU.mult),
                      reads=[r_a2], writes=[r_u])
                em.op("dve", lambda e: e.tensor_tensor(out=a2[:], in0=a2[:], in1=u[:], op=ALU.add),
                      reads=[r_a2, r_u], writes=[r_a2])
                em.op("dve", lambda e: e.tensor_scalar(out=u[:], in0=a2[:], scalar1=math.pi,
                                                       scalar2=-2 * math.pi, op0=ALU.is_gt, op1=ALU.mult),
                      reads=[r_a2], writes=[r_u])
                em.op("dve", lambda e: e.tensor_tensor(out=a2[:], in0=a2[:], in1=u[:], op=ALU.add),
                      reads=[r_a2, r_u], writes=[r_a2])
                em.op("dve", lambda e: e.tensor_scalar(out=a2[:], in0=a2[:], scalar1=-PI_LO, scalar2=PI_LO,
                                                       op0=ALU.max, op1=ALU.min), reads=[r_a2], writes=[r_a2])
                em.op("act", lambda e: e.activation(out=u[:], in_=a2[:], func=AF.Sin),
                      reads=[r_a2], writes=[r_u])
                em.dma("sp", dst[:, :], u[:], reads=[r_u], pwrites=[R["tab_d"]])
            em.barrier()

    def emit_transposes(ph_objs, src_tile, r_src, qi):
        xb, r_xb, tp, r_tp, stage, r_stage = ph_objs
        em.op("act", lambda e: e.activation(out=xb[:], in_=src_tile[:], func=AF.Copy),
              reads=[r_src], writes=[r_xb])
        for kt in range(KT):
            em.op("pe", lambda e: e.transpose(tp[:, kt * 128:(kt + 1) * 128], xb[:, kt * 128:(kt + 1) * 128],
                                              ident_b[:]),
                  reads=[r_xb, r_const], writes=[r_tp] if kt == 0 else [], pwrites=[] if kt == 0 else [r_tp],
                  inc=(kt == KT - 1))
        em.op("dve", lambda e: e.tensor_copy(out=stage[:, :, qi * 128:(qi + 1) * 128],
                                             in_=tp[:].rearrange("p (k c) -> p k c", c=128)),
              reads=[r_tp], pwrites=[r_stage])

    def phase_x0():
        with ExitStack() as ph:
            xt = [sb(ph, f"x0_{i}", [128, D], F32) for i in range(2)]
            r_xt = [Res() for _ in range(2)]
            xb = sb(ph, "x0b", [128, D], BF16)
            tp = ps(ph, "x0tp", [128, D], BF16)
            stage = [sb(ph, f"x0s{i}", [128, KT, 512], BF16) for i in range(2)]
            r_stage = [Res() for _ in range(2)]
            r_xb, r_tp = Res(), Res()
            for i in range(NTQ):
                b = i % 2
                tb, qi = i // 4, i % 4
                em.dma("sp", xt[b][:], x_in[i * 128:(i + 1) * 128, :], writes=[r_xt[b]])
                emit_transposes((xb, r_xb, tp, r_tp, stage[tb % 2], r_stage[tb % 2]), xt[b], r_xt[b], qi)
                if qi == 3:
                    em.dma("sp", xT_d.rearrange("(k p) t -> p k t", p=128)[:, :, tb * 512:(tb + 1) * 512],
                           stage[tb % 2][:], reads=[r_stage[tb % 2]], pwrites=[R["xT_d"]])
            em.barrier()

    def phase_proj(l):
        with ExitStack() as ph:
            xT = sb(ph, "p1_xT", [128, KT, T], BF16)
            r_xT = Res()
            cosT = sb(ph, "p1_cos", [128, T], F32)
            sinT = sb(ph, "p1_sin", [128, T], F32)
            r_tab = Res()
            wb = [sb(ph, f"p1_w{i}", [128, KT, 512], BF16) for i in range(2)]
            r_wb = [Res() for _ in range(2)]
            wsw = sb(ph, "p1_wsw", [128, KT, 512], BF16)
            r_wsw = Res()
            stage = [sb(ph, f"p1_st{i}", [128, 512], BF16) for i in range(3)]
            r_st = [Res() for _ in range(3)]
            t1 = sb(ph, "p1_t1", [128, 512], F32)
            t2 = sb(ph, "p1_t2", [128, 512], F32)
            r_t1, r_t2 = Res(), Res()
            stv = [sb(ph, f"p1_sv{i}", [128, 4, 192], BF16) for i in range(2)]
            r_stv = [Res() for _ in range(2)]
            psA = [ps(ph, f"p1_pA{i}") for i in range(2)]
            psB = [ps(ph, f"p1_pB{i}") for i in range(2)]
            r_pA = [Res() for _ in range(2)]
            r_pB = [Res() for _ in range(2)]
            pss = ps(ph, "p1_pss")
            r_pss = Res()
            pst = ps(ph, "p1_pst")
            r_pst = Res()
            bfb = sb(ph, "p1_bf", [128, 8], F32)
            r_bfb = Res()
            logf = sb(ph, "p1_logf", [128, NTQ, 8], F32)
            r_logf = Res()
            wabs = sb(ph, "p1_wabs", [128, NTQ, 8], F32)
            wsgn = sb(ph, "p1_wsgn", [128, NTQ, 8], F32)
            r_wab, r_wsg = Res(), Res()
            augT = sb(ph, "p1_augT", [96, T], BF16)
            r_augT = Res()
            A = sb(ph, "p1_A", [128, 96], F32)
            r_A = Res()
            sm = {n: sb(ph, "p1_" + n, [128, 8], F32) for n in ("z", "e", "c", "hif", "d1", "midf", "d2", "lof")}
            smb = {n: sb(ph, "p1_" + n, [128, 8], BF16) for n in ("hib", "midb", "lob")}
            r_sm = {n: Res() for n in list(sm) + list(smb)}

            em.dma("sp", xT[:], xT_d.rearrange("(k p) t -> p k t", p=128), reads=[R["xT_d"]], writes=[r_xT])
            em.dma("sp", cosT[:], cosT_d[:, :], reads=[R["tab_d"]], pwrites=[r_tab])
            em.dma("sp", sinT[:], sinT_d[:, :], reads=[R["tab_d"]], pwrites=[r_tab])
            em.dma("sp", bfb[:], b_forget[l].partition_broadcast(128), writes=[r_bfb])
            em.op("dve", lambda e: e.memset(A[:], 1.0), writes=[r_A])
            for i in range(2):
                em.op("pool", lambda e: e.memset(stv[i][:], 1.0), writes=[r_stv[i]])

            wl = w_in[l]
            groups = [("dq", 512, "rot", dqT_d, 0.125), ("dk", 512, "rot", dkT_d, 1.0),
                      ("dv", 512, "tok", dv_d, 1.0), ("iq", 512, "rot", iqT_d, 0.125),
                      ("ik", 72, "ikiw", ikT_d, 1.0),
                      ("fq", 512, "plain", fqT_d, 0.125), ("fk", 512, "plain", fkT_d, 1.0),
                      ("fv", 512, "tok", fv_d, 1.0), ("fl", 8, "fl", None, 1.0),
                      ("ga", 512, "sig", sgaT_d, 0), ("ga2", 512, "sig", sgaT_d, 512),
                      ("gb", 512, "sig", sgbT_d, 0), ("gb2", 512, "sig", sgbT_d, 512)]
            if D != 1024:
                raise NotImplementedError

            def colstart(name):
                if name == "ga2":
                    return OFF["ga"] + 512
                if name == "gb2":
                    return OFF["gb"] + 512
                return OFF[name]

            def load_w(gi):
                name, ncol = groups[gi][0], groups[gi][1]
                c0 = colstart(name)
                b = gi % 2
                em.dma("pool", wb[b][:, :, 0:ncol],
                       wl[:, c0:c0 + ncol].rearrange("(k p) c -> p k c", p=128), writes=[r_wb[b]])

            cnt = {"st": 0, "pp": 0, "sv": 0}

            def fm_tile(W, r_W, Wsw, col0, M, tb, kind, scale, dst, drow0):
                p = cnt["pp"] % 2
                cnt["pp"] += 1
                tsl = slice(tb * 512, (tb + 1) * 512)
                for kt in range(KT):
                    em.op("pe", lambda e: e.matmul(psA[p][0:M, :], lhsT=W[:, kt, col0:col0 + M],
                                                   rhs=xT[:, kt, tsl], start=(kt == 0), stop=(kt == KT - 1)),
                          reads=[r_W, r_xT], writes=[r_pA[p]] if kt == 0 else [], pwrites=[] if kt == 0 else [r_pA[p]],
                          inc=(kt == KT - 1))
                if kind == "rot":
                    for kt in range(KT):
                        em.op("pe", lambda e: e.matmul(psB[p][0:M, :], lhsT=Wsw[:, kt, col0:col0 + M],
                                                       rhs=xT[:, kt, tsl], start=(kt == 0), stop=(kt == KT - 1)),
                              reads=[r_wsw, r_xT], writes=[r_pB[p]] if kt == 0 else [],
                              pwrites=[] if kt == 0 else [r_pB[p]], inc=(kt == KT - 1))
                s = cnt["st"] % 3
                cnt["st"] += 1
                if kind == "rot":
                    em.op("dve", lambda e: e.scalar_tensor_tensor(out=t1[0:M, :], in0=psA[p][0:M, :], scalar=scale,
                                                                  in1=cosT[0:M, tsl], op0=ALU.mult, op1=ALU.mult),
                          reads=[r_pA[p], r_tab], writes=[r_t1])
                    em.op("dve", lambda e: e.scalar_tensor_tensor(out=t2[0:M, :], in0=psB[p][0:M, :], scalar=scale,
                                                                  in1=sinT[0:M, tsl], op0=ALU.mult, op1=ALU.mult),
                          reads=[r_pB[p], r_tab], writes=[r_t2])
                    em.op("pool", lambda e: e.tensor_tensor(out=stage[s][0:M, :], in0=t1[0:M, :], in1=t2[0:M, :],
                                                            op=ALU.add),
                          reads=[r_t1, r_t2], writes=[r_st[s]])
                elif kind == "plain":
                    em.op("act", lambda e: e.activation(out=stage[s][0:M, :], in_=psA[p][0:M, :], func=AF.Copy,
                                                        scale=scale), reads=[r_pA[p]], writes=[r_st[s]])
                else:
                    em.op("act", lambda e: e.activation(out=stage[s][0:M, :], in_=psA[p][0:M, :], func=AF.Sigmoid),
                          reads=[r_pA[p]], writes=[r_st[s]])
                em.dma("sp", dst[drow0:drow0 + M, tsl], stage[s][0:M, :], reads=[r_st[s]], pwrites=[R[_rn(dst)]])

            names = {id(dqT_d): "dqT_d", id(dkT_d): "dkT_d", id(iqT_d): "iqT_d", id(ikT_d): "ikT_d",
                     id(fqT_d): "fqT_d", id(fkT_d): "fkT_d", id(sgaT_d): "sgaT_d", id(sgbT_d): "sgbT_d",
                     id(dv_d): "dv_d", id(fv_d): "fv_d"}

            def _rn(d):
                return names[id(d)]

            def make_swapped(W, r_W, ncol):
                nh = ncol // 64
                Wv = W[:, :, 0:ncol].rearrange("p k (h two j) -> p k h two j", two=2, j=32)
                Sv = wsw[:, :, 0:ncol].rearrange("p k (h two j) -> p k h two j", two=2, j=32)
                for kt in range(KT):
                    em.op("act", lambda e: e.activation(out=Sv[:, kt, :, 0, :], in_=Wv[:, kt, :, 1, :], func=AF.Copy,
                                                        scale=-1.0),
                          reads=[r_W], writes=[r_wsw] if kt == 0 else [], pwrites=[] if kt == 0 else [r_wsw])
                    em.op("pool", lambda e: e.tensor_copy(out=Sv[:, kt, :, 1, :], in_=Wv[:, kt, :, 0, :]),
                          reads=[r_W], pwrites=[r_wsw])

            load_w(0)
            for gi, (name, ncol, kind, dst, extra) in enumerate(groups):
                if gi + 1 < len(groups):
                    load_w(gi + 1)
                W, r_W = wb[gi % 2], r_wb[gi % 2]
                if kind in ("rot",):
                    make_swapped(W, r_W, ncol)
                    for ct in range(ncol // 128):
                        for tb in range(NTB):
                            fm_tile(W, r_W, wsw, ct * 128, 128, tb, "rot", extra, dst, ct * 128)
                elif kind == "plain":
                    for ct in range(ncol // 128):
                        for tb in range(NTB):
                            fm_tile(W, r_W, None, ct * 128, 128, tb, "plain", extra, dst, ct * 128)
                elif kind == "sig":
                    for ct in range(ncol // 128):
                        for tb in range(NTB):
                            fm_tile(W, r_W, None, ct * 128, 128, tb, "sig", 1.0, dst, extra + ct * 128)
                elif kind == "ikiw":
                    make_swapped(W, r_W, 64)
                    for tb in range(NTB):
                        fm_tile(W, r_W, wsw, 0, 64, tb, "rot", 1.0, dst, 0)
                    for i in range(NTQ):
                        for kt in range(KT):
                            em.op("pe", lambda e: e.matmul(pss[:, 0:8], lhsT=xT[:, kt, i * 128:(i + 1) * 128],
                                                           rhs=W[:, kt, 64:72], start=(kt == 0), stop=(kt == KT - 1)),
                                  reads=[r_W, r_xT], writes=[r_pss] if kt == 0 else [],
                                  pwrites=[] if kt == 0 else [r_pss], inc=(kt == KT - 1))
                        em.op("act", lambda e: e.activation(out=wabs[:, i, :], in_=pss[:, 0:8], func=AF.Abs,
                                                            scale=8.0 ** -0.5),
                              reads=[r_pss], pwrites=[r_wab])
                        em.op("act", lambda e: e.activation(out=wsgn[:, i, :], in_=pss[:, 0:8], func=AF.Sign),
                              reads=[r_pss], pwrites=[r_wsg])
                    em.dma("sp", wabs_d[:, :], wabs[:].rearrange("p a b -> p (a b)"), reads=[r_wab], pwrites=[R["w_d"]])
                    em.dma("sp", wsgn_d[:, :], wsgn[:].rearrange("p a b -> p (a b)"), reads=[r_wsg], pwrites=[R["w_d"]])
                elif kind == "tok":
                    for i in range(NTQ):
                        p = cnt["pp"] % 2
                        cnt["pp"] += 1
                        for kt in range(KT):
                            em.op("pe", lambda e: e.matmul(psA[p][:, :], lhsT=xT[:, kt, i * 128:(i + 1) * 128],
                                                           rhs=W[:, kt, 0:512], start=(kt == 0), stop=(kt == KT - 1)),
                                  reads=[r_W, r_xT], writes=[r_pA[p]] if kt == 0 else [],
                                  pwrites=[] if kt == 0 else [r_pA[p]], inc=(kt == KT - 1))
                        s = cnt["sv"] % 2
                        cnt["sv"] += 1
                        sv = stv[s][:].rearrange("p a (three c) -> p a three c", c=64)
                        pv = psA[p][:, :].rearrange("p (a two c) -> p a two c", two=2, c=64)
                        em.op("act", lambda e: e.activation(out=sv[:, :, 0, :], in_=pv[:, :, 0, :], func=AF.Copy),
                              reads=[r_pA[p]], writes=[r_stv[s]])
                        em.op("dve", lambda e: e.tensor_copy(out=sv[:, :, 2, :], in_=pv[:, :, 1, :]),
                              reads=[r_pA[p]], pwrites=[r_stv[s]])
                        em.dma("sp", dst[i * 128:(i + 1) * 128, :], stv[s][:].rearrange("p a c -> p (a c)"),
                               reads=[r_stv[s]], pwrites=[R[_rn(dst)]])
                elif kind == "fl":
                    for i in range(NTQ):
                        for kt in range(KT):
                            em.op("pe", lambda e: e.matmul(pss[:, 0:8], lhsT=xT[:, kt, i * 128:(i + 1) * 128],
                                                           rhs=W[:, kt, 0:8], start=(kt == 0), stop=(kt == KT - 1)),
                                  reads=[r_W, r_xT], writes=[r_pss] if kt == 0 else [],
                                  pwrites=[] if kt == 0 else [r_pss], inc=(kt == KT - 1))
                        em.op("dve", lambda e: e.tensor_tensor(out=sm["z"][:], in0=pss[:, 0:8], in1=bfb[:], op=ALU.add),
                              reads=[r_pss, r_bfb], writes=[r_sm["z"]])
                        em.op("act", lambda e: e.activation(out=sm["e"][:], in_=sm["z"][:], func=AF.Exp, scale=-1.0),
                              reads=[r_sm["z"]], writes=[r_sm["e"]])
                        em.op("act", lambda e: e.activation(out=sm["z"][:], in_=sm["e"][:], func=AF.Ln, bias=1.0),
                              reads=[r_sm["e"]], writes=[r_sm["z"]])
                        em.op("dve", lambda e: e.tensor_scalar(out=logf[:, i, :], in0=sm["z"][:], scalar1=-1.0,
                                                               scalar2=None, op0=ALU.mult),
                              reads=[r_sm["z"]], pwrites=[r_logf])
                        for j in range(i + 1):
                            em.op("pe", lambda e: e.matmul(pss[:, 8:16], lhsT=(tri_f if j == i else ones_f)[:, :],
                                                           rhs=logf[:, j, :], start=(j == 0), stop=(j == i)),
                                  reads=[r_logf, r_const], writes=[r_pss] if j == 0 else [],
                                  pwrites=[] if j == 0 else [r_pss], inc=(j == i))
                        em.op("dve", lambda e: e.tensor_copy(out=sm["c"][:], in_=pss[:, 8:16]),
                              reads=[r_pss], writes=[r_sm["c"]])
                        Av = A[:].rearrange("p (s h r) -> p s h r", s=2, r=6)
                        em.op("dve", lambda e: e.tensor_copy(out=smb["hib"][:], in_=sm["c"][:]),
                              reads=[r_sm["c"]], writes=[r_sm["hib"]])
                        em.op("dve", lambda e: e.tensor_copy(out=sm["hif"][:], in_=smb["hib"][:]),
                              reads=[r_sm["hib"]], writes=[r_sm["hif"]])
                        em.op("dve", lambda e: e.tensor_tensor(out=sm["d1"][:], in0=sm["c"][:], in1=sm["hif"][:],
                                                               op=ALU.subtract),
                              reads=[r_sm["c"], r_sm["hif"]], writes=[r_sm["d1"]])
                        em.op("dve", lambda e: e.tensor_copy(out=smb["midb"][:], in_=sm["d1"][:]),
                              reads=[r_sm["d1"]], writes=[r_sm["midb"]])
                        em.op("dve", lambda e: e.tensor_copy(out=sm["midf"][:], in_=smb["midb"][:]),
                              reads=[r_sm["midb"]], writes=[r_sm["midf"]])
                        em.op("dve", lambda e: e.tensor_tensor(out=sm["d2"][:], in0=sm["d1"][:], in1=sm["midf"][:],
                                                               op=ALU.subtract),
                              reads=[r_sm["d1"], r_sm["midf"]], writes=[r_sm["d2"]])
                        em.op("dve", lambda e: e.tensor_copy(out=smb["lob"][:], in_=sm["d2"][:]),
                              reads=[r_sm["d2"]], writes=[r_sm["lob"]])
                        em.op("dve", lambda e: e.tensor_copy(out=sm["lof"][:], in_=smb["lob"][:]),
                              reads=[r_sm["lob"]], writes=[r_sm["lof"]])
                        for ri, nm in enumerate(("hif", "midf", "lof")):
                            em.op("dve", lambda e: e.tensor_copy(out=Av[:, 0, :, ri], in_=sm[nm][:]),
                                  reads=[r_sm[nm]], writes=[r_A] if ri == 0 else [], pwrites=[] if ri == 0 else [r_A])
                            em.op("dve", lambda e: e.tensor_scalar(out=Av[:, 1, :, 3 + ri], in0=sm[nm][:], scalar1=-1.0,
                                                                   scalar2=None, op0=ALU.mult),
                                  reads=[r_sm[nm]], pwrites=[r_A])
                        em.op("pe", lambda e: e.transpose(pst[0:96, 0:128], A[:, :], ident_f[:]),
                              reads=[r_A, r_const], writes=[r_pst])
                        em.op("act", lambda e: e.activation(out=augT[:, i * 128:(i + 1) * 128], in_=pst[0:96, 0:128],
                                                            func=AF.Copy), reads=[r_pst], pwrites=[r_augT])
                    em.dma("sp", aug_d[:, :], augT[:], reads=[r_augT], writes=[R["aug_d"]])
            em.barrier()

    def attn_alloc(ph, pfx):
        o = dict(
            sps=[ps(ph, f"{pfx}_s{i}") for i in range(3)], r_sps=[Res() for _ in range(3)],
            pexp=[sb(ph, f"{pfx}_p{i}", [128, 512], BF16) for i in range(3)], r_pexp=[Res() for _ in range(3)],
            ops=[ps(ph, f"{pfx}_o{i}") for i in range(2)], r_ops=[Res() for _ in range(2)],
            rden=sb(ph, f"{pfx}_rden", [128, 512], F32), r_rden=Res(),
            oT=[sb(ph, f"{pfx}_oT{i}", [128, 512], BF16) for i in range(2)], r_oT=[Res() for _ in range(2)],
            ctr={"o": 0, "s": 0})
        return o

    def attn_block(o, kT, r_kT, qT, r_qT, q0, Kc, prow, V, r_V, odd, n_ts, col_lo_fn, diag_fn, mask_mm_fn,
                   out_ap, r_out):
        ob = o["ctr"]["o"] % 2
        o["ctr"]["o"] += 1
        ops, r_ops = o["ops"][ob], o["r_ops"][ob]
        oT, r_oT = o["oT"][ob], o["r_oT"][ob]
        vc = 64 if odd else 0
        base = o["ctr"]["s"]
        o["ctr"]["s"] += n_ts
        has_mask = mask_mm_fn is not None

        def emit_S(i):
            lo = col_lo_fn(i)
            s = (base + i) % 3
            sps, r_sps = o["sps"][s], o["r_sps"][s]
            em.op("pe", lambda e: e.matmul(sps[:, lo:512], lhsT=kT[prow:prow + Kc, i * 128:(i + 1) * 128],
                                           rhs=qT[prow:prow + Kc, q0 + lo:q0 + 512], start=True, stop=not has_mask),
                  reads=[r_kT, r_qT], writes=[r_sps], inc=not has_mask)
            if has_mask:
                mask_mm_fn(i, lo, sps, r_sps)

        def emit_rest(i):
            lo = col_lo_fn(i)
            s = (base + i) % 3
            sps, r_sps, pexp, r_pexp = o["sps"][s], o["r_sps"][s], o["pexp"][s], o["r_pexp"][s]
            em.op("act", lambda e: e.activation(out=pexp[:, lo:512], in_=sps[:, lo:512], func=AF.Exp),
                  reads=[r_sps], writes=[r_pexp])
            dc = diag_fn(i) if diag_fn is not None else None
            if dc is not None:
                em.op("pool", lambda e: e.tensor_tensor(out=pexp[:, dc:dc + 128], in0=pexp[:, dc:dc + 128],
                                                        in1=tri_b[:, :], op=ALU.mult),
                      reads=[r_pexp, r_const], writes=[r_pexp])
            em.op("pe", lambda e: e.matmul(ops[:, lo:512], lhsT=V[:, i, vc:vc + 128], rhs=pexp[:, lo:512],
                                           start=(i == 0), stop=(i == n_ts - 1)),
                  reads=[r_V, r_pexp], writes=[r_ops] if i == 0 else [], pwrites=[] if i == 0 else [r_ops],
                  inc=(i == n_ts - 1))

        emit_S(0)
        for i in range(n_ts):
            if i + 1 < n_ts:
                emit_S(i + 1)
            emit_rest(i)
        (orow, drow) = (64, 0) if odd else (0, 64)
        em.op("dve", lambda e: e.reciprocal(out=o["rden"][orow:orow + 64, :], in_=ops[drow:drow + 64, :]),
              reads=[r_ops], writes=[o["r_rden"]])
        em.op("dve", lambda e: e.tensor_tensor(out=oT[orow:orow + 64, :], in0=ops[orow:orow + 64, :],
                                               in1=o["rden"][orow:orow + 64, :], op=ALU.mult),
              reads=[r_ops, o["r_rden"]], writes=[r_oT])
        em.dma("sp", out_ap, oT[orow:orow + 64, :], reads=[r_oT], pwrites=[r_out])

    def phase_fox():
        with ExitStack() as ph:
            o = attn_alloc(ph, "fx")
            qa = [sb(ph, f"fx_q{i}", [70, T], BF16) for i in range(2)]
            ka = [sb(ph, f"fx_k{i}", [70, T], BF16) for i in range(2)]
            r_qa = [Res() for _ in range(2)]
            r_ka = [Res() for _ in range(2)]
            V = sb(ph, "fx_V", [128, NTQ, 768], BF16)
            r_V = Res()
            em.dma("sp", V[:], fv_d.rearrange("(i p) c -> p i c", p=128), reads=[R["fv_d"]], writes=[r_V])

            def load_head(h):
                b = h % 2
                em.dma("sp", qa[b][0:64, :], fqT_d[h * 64:(h + 1) * 64, :], reads=[R["fqT_d"]], writes=[r_qa[b]])
                em.dma("sp", qa[b][64:70, :], aug_d[h * 6:h * 6 + 6, :], reads=[R["aug_d"]], pwrites=[r_qa[b]])
                em.dma("sp", ka[b][0:64, :], fkT_d[h * 64:(h + 1) * 64, :], reads=[R["fkT_d"]], writes=[r_ka[b]])
                em.dma("sp", ka[b][64:70, :], aug_d[48 + h * 6:48 + h * 6 + 6, :], reads=[R["aug_d"]],
                       pwrites=[r_ka[b]])

            load_head(0)
            for h in range(8):
                if h + 1 < 8:
                    load_head(h + 1)
                b = h % 2
                pair, odd = h // 2, h % 2
                Vp = V[:, :, pair * 192:(pair + 1) * 192]
                for tb in range(NTB):
                    attn_block(o, ka[b], r_ka[b], qa[b], r_qa[b], tb * 512, 70, 0, Vp, r_V, odd, 4 * (tb + 1),
                               lambda i: max(0, (i - 4 * tb) * 128),
                               lambda i: ((i - 4 * tb) * 128 if i >= 4 * tb else None),
                               None, obT_d[h * 64:(h + 1) * 64, tb * 512:(tb + 1) * 512], R["obT_d"])
            em.barrier()

    def phase_dsa():
        with ExitStack() as ph:
            o = attn_alloc(ph, "ds")
            iq = sb(ph, "ds_iq", [128, 4, T], BF16)
            ik2 = sb(ph, "ds_ik", [128, T], BF16)
            wabs = sb(ph, "ds_wabs", [128, NTQ, 8], F32)
            wsgn = sb(ph, "ds_wsgn", [128, NTQ, 8], F32)
            r_iq, r_ik, r_w = Res(), Res(), Res()
            sc = sb(ph, "ds_sc", [128, T], F32)
            junk = sb(ph, "ds_junk", [128, T], BF16)
            r_sc, r_junk = Res(), Res()
            mbs = [[sb(ph, f"ds_mb{s_}_{i}", [128, T], BF16) for i in range(4)] for s_ in range(2)]
            r_mbs = [[Res() for _ in range(4)] for _ in range(2)]
            rl = [sb(ph, f"ds_rl{i}", [128, 512], F32) for i in range(2)]
            r_rl = [Res() for _ in range(2)]
            scps = [ps(ph, f"ds_sp{i}") for i in range(2)]
            r_scps = [Res() for _ in range(2)]
            sm = {n: sb(ph, "ds_" + n, [128, 1], F32) for n in ("lo", "w0", "wk", "mid", "cnt", "gw", "mx", "thr0")}
            r_sm = {n: Res() for n in sm}
            Vp = [sb(ph, f"ds_V{i}", [128, NTQ, 192], BF16) for i in range(2)]
            r_Vp = [Res() for _ in range(2)]
            qh = [sb(ph, f"ds_q{i}", [128, 512], BF16) for i in range(2)]
            kh = [sb(ph, f"ds_k{i}", [128, T], BF16) for i in range(2)]
            r_qh = [Res() for _ in range(2)]
            r_kh = [Res() for _ in range(2)]

            em.dma("sp", iq[:], iqT_d.rearrange("(k p) t -> p k t", p=128), reads=[R["iqT_d"]], writes=[r_iq])
            em.dma("sp", ik2[0:64, :], ikT_d[:, :], reads=[R["ikT_d"]], pwrites=[r_ik])
            em.dma("sp", ik2[64:128, :], ikT_d[:, :], reads=[R["ikT_d"]], pwrites=[r_ik])
            em.dma("sp", wabs[:].rearrange("p a b -> p (a b)"), wabs_d[:, :], reads=[R["w_d"]], pwrites=[r_w])
            em.dma("sp", wsgn[:].rearrange("p a b -> p (a b)"), wsgn_d[:, :], reads=[R["w_d"]], pwrites=[r_w])
            em.op("dve", lambda e: e.memset(sm["thr0"][:], -1e29), writes=[r_sm["thr0"]])
            ctr = {"p": 0, "v": 0, "h": 0}
            dv_v = dv_d.rearrange("(i p) c -> p i c", p=128)

            def prep_q(tb, qi, mb, r_mb):
                g = tb * 4 + qi
                L2 = 128 * (g + 1)
                L1 = L2 - 64
                for kb in range((L2 + 511) // 512):
                    c0 = kb * 512
                    cw = min(512, L2 - c0)
                    for h in range(8):
                        pair, prow = h // 2, (h % 2) * 64
                        p = ctr["p"] % 2
                        ctr["p"] += 1
                        em.op("pe", lambda e: e.matmul(scps[p][:, 0:cw],
                                                       lhsT=iq[prow:prow + 64, pair, g * 128:(g + 1) * 128],
                                                       rhs=ik2[prow:prow + 64, c0:c0 + cw], start=True, stop=True),
                              reads=[r_iq, r_ik], writes=[r_scps[p]])
                        em.op("act", lambda e: e.activation(out=rl[p][:, 0:cw], in_=scps[p][:, 0:cw], func=AF.Relu,
                                                            scale=wabs[:, g, h:h + 1]),
                              reads=[r_scps[p], r_w], writes=[r_rl[p]])
                        if h == 0:
                            em.op("dve", lambda e: e.tensor_scalar(out=sc[:, c0:c0 + cw], in0=rl[p][:, 0:cw],
                                                                   scalar1=wsgn[:, g, 0:1], scalar2=None,
                                                                   op0=ALU.mult),
                                  reads=[r_rl[p], r_w], writes=[r_sc] if kb == 0 else [],
                                  pwrites=[] if kb == 0 else [r_sc])
                        else:
                            em.op("dve", lambda e: e.scalar_tensor_tensor(out=sc[:, c0:c0 + cw], in0=rl[p][:, 0:cw],
                                                                          scalar=wsgn[:, g, h:h + 1],
                                                                          in1=sc[:, c0:c0 + cw], op0=ALU.mult,
                                                                          op1=ALU.add),
                                  reads=[r_rl[p], r_w, r_sc], pwrites=[r_sc])
                em.op("dve", lambda e: e.memset(sc[0:64, L1:L2], -1e30), reads=[r_sc], pwrites=[r_sc])
                if L2 > TOPK:
                    em.op("dve", lambda e: e.tensor_reduce(out=sm["mx"][:], in_=sc[:, 0:L2], axis=AX.X, op=ALU.max),
                          reads=[r_sc], writes=[r_sm["mx"]])
                    em.op("dve", lambda e: e.tensor_reduce(out=sm["lo"][:], in_=sc[:, 0:L1], axis=AX.X, op=ALU.min),
                          reads=[r_sc], writes=[r_sm["lo"]])
                    em.op("dve", lambda e: e.tensor_tensor(out=sm["w0"][:], in0=sm["mx"][:], in1=sm["lo"][:],
                                                           op=ALU.subtract),
                          reads=[r_sm["mx"], r_sm["lo"]], writes=[r_sm["w0"]])
                    em.op("dve", lambda e: e.tensor_scalar(out=sm["w0"][:], in0=sm["w0"][:], scalar1=1.0001,
                                                           scalar2=1e-12, op0=ALU.mult, op1=ALU.add),
                          reads=[r_sm["w0"]], writes=[r_sm["w0"]])
                    for k in range(bisect_iters):
                        em.op("dve", lambda e: e.scalar_tensor_tensor(out=sm["mid"][:], in0=sm["w0"][:],
                                                                      scalar=2.0 ** -(k + 1), in1=sm["lo"][:],
                                                                      op0=ALU.mult, op1=ALU.add),
                              reads=[r_sm["w0"], r_sm["lo"]], writes=[r_sm["mid"]])
                        em.op("dve", lambda e: e.tensor_scalar(out=junk[:, 0:L2], in0=sc[:, 0:L2],
                                                               scalar1=sm["mid"][:, 0:1], scalar2=None,
                                                               op0=ALU.is_ge, op1=ALU.add,
                                                               accum_out=sm["cnt"][:, 0:1]),
                              reads=[r_sc, r_sm["mid"]], writes=[r_junk, r_sm["cnt"]])
                        em.op("dve", lambda e: e.scalar_tensor_tensor(out=sm["gw"][:], in0=sm["cnt"][:],
                                                                      scalar=float(TOPK), in1=sm["w0"][:],
                                                                      op0=ALU.is_ge, op1=ALU.mult),
                              reads=[r_sm["cnt"], r_sm["w0"]], writes=[r_sm["gw"]])
                        em.op("dve", lambda e: e.scalar_tensor_tensor(out=sm["lo"][:], in0=sm["gw"][:],
                                                                      scalar=2.0 ** -(k + 1), in1=sm["lo"][:],
                                                                      op0=ALU.mult, op1=ALU.add),
                              reads=[r_sm["gw"], r_sm["lo"]], writes=[r_sm["lo"]])
                    thr, r_thr = sm["lo"], r_sm["lo"]
                else:
                    thr, r_thr = sm["thr0"], r_sm["thr0"]
                em.op("dve", lambda e: e.tensor_scalar(out=mb[qi][:, 0:L2], in0=sc[:, 0:L2], scalar1=thr[:, 0:1],
                                                       scalar2=NEG, op0=ALU.is_lt, op1=ALU.mult),
                      reads=[r_sc, r_thr], writes=[r_mb[qi]])

            def attend_pair(tb, pair, mb, r_mb):
                n_ts = 4 * (tb + 1)


                def mask_mm(i, lo, sps, r_sps):
                    q_first = lo // 128
                    for qq in range(q_first, 4):
                        em.op("pe", lambda e: e.matmul(sps[:, qq * 128:(qq + 1) * 128],
                                                       lhsT=mb[qq][:, i * 128:(i + 1) * 128], rhs=ident_b[:, :],
                                                       start=False, stop=(qq == 3)),
                              reads=[r_mb[qq], r_const], pwrites=[r_sps], inc=(qq == 3))

                vb = ctr["v"] % 2
                ctr["v"] += 1
                em.dma("sp", Vp[vb][:, 0:n_ts, :], dv_v[:, 0:n_ts, pair * 192:(pair + 1) * 192],
                       reads=[R["dv_d"]], writes=[r_Vp[vb]])
                for odd in range(2):
                    h = pair * 2 + odd
                    prow = odd * 64
                    hb = ctr["h"] % 2
                    ctr["h"] += 1
                    em.dma("sp", qh[hb][prow:prow + 64, :], dqT_d[h * 64:(h + 1) * 64, tb * 512:(tb + 1) * 512],
                           reads=[R["dqT_d"]], writes=[r_qh[hb]])
                    em.dma("sp", kh[hb][prow:prow + 64, 0:n_ts * 128], dkT_d[h * 64:(h + 1) * 64, 0:n_ts * 128],
                           reads=[R["dkT_d"]], writes=[r_kh[hb]])
                    attn_block(o, kh[hb], r_kh[hb], qh[hb], r_qh[hb], 0, 64, prow, Vp[vb], r_Vp[vb], odd, n_ts,
                               lambda i: max(0, (i - 4 * tb) * 128), None, mask_mm,
                               oaT_d[h * 64:(h + 1) * 64, tb * 512:(tb + 1) * 512], R["oaT_d"])

            order = list(range(NTB - 1, -1, -1))
            for qi in range(4):
                prep_q(order[0], qi, mbs[0], r_mbs[0])
            for n_, tb in enumerate(order):
                for qi in range(4):
                    if n_ + 1 < NTB:
                        prep_q(order[n_ + 1], qi, mbs[(n_ + 1) % 2], r_mbs[(n_ + 1) % 2])
                    attend_pair(tb, qi, mbs[n_ % 2], r_mbs[n_ % 2])
            em.barrier()

    def ln_alloc(ph, pfx, g_d, b_d):
        o = dict(
            y=[sb(ph, f"{pfx}_y{i}", [128, D], F32) for i in range(2)], r_y=[Res() for _ in range(2)],
            xo=[sb(ph, f"{pfx}_xo{i}", [128, D], F32) for i in range(2)], r_xo=[Res() for _ in range(2)],
            xr=[sb(ph, f"{pfx}_xr{i}", [128, D], F32) for i in range(2)], r_xr=[Res() for _ in range(2)],
            st=sb(ph, f"{pfx}_st", [128, 6 * (D // 512)], F32), r_st=Res(),
            mv=sb(ph, f"{pfx}_mv", [128, 2], F32), r_mv=Res(),
            sd=sb(ph, f"{pfx}_sd", [128, 1], F32), r_sd=Res(),
            gb=sb(ph, f"{pfx}_gb", [128, D], F32), bb=sb(ph, f"{pfx}_bb", [128, D], F32), r_gb=Res(),
            xb=sb(ph, f"{pfx}_xb", [128, D], BF16), r_xb=Res(),
            tp=ps(ph, f"{pfx}_tp", [128, D], BF16), r_tp=Res(),
            stage=[sb(ph, f"{pfx}_sg{i}", [128, KT, 512], BF16) for i in range(2)], r_stage=[Res() for _ in range(2)],
            n=0)
        em.dma("sp", o["gb"][:], g_d.partition_broadcast(128), pwrites=[o["r_gb"]])
        em.dma("sp", o["bb"][:], b_d.partition_broadcast(128), pwrites=[o["r_gb"]])
        return o

    def ln_stage(o, i, halves, xres_d, store_d, r_store, xT_dst, r_xT_dst, after_fn=None):
        b = o["n"] % 2
        o["n"] += 1
        y, r_y, xo, r_xo, xr, r_xr = o["y"][b], o["r_y"][b], o["xo"][b], o["r_xo"][b], o["xr"][b], o["r_xr"][b]
        em.dma("sp", xr[:], xres_d[i * 128:(i + 1) * 128, :], reads=[R["x1_d"], R["xcur_d"]], writes=[r_xr])
        for hf, (pa, r_pa) in enumerate(halves):
            sl = slice(hf * 512, (hf + 1) * 512)
            em.op("dve", lambda e: e.scalar_tensor_tensor(out=y[:, sl], in0=xr[:, sl], scalar=alpha, in1=pa,
                                                          op0=ALU.mult, op1=ALU.add),
                  reads=[r_xr, r_pa], writes=[r_y] if hf == 0 else [], pwrites=[] if hf == 0 else [r_y])
        for c in range(D // 512):
            em.op("dve", lambda e: e.bn_stats(out=o["st"][:, c * 6:(c + 1) * 6], in_=y[:, c * 512:(c + 1) * 512]),
                  reads=[r_y], writes=[o["r_st"]] if c == 0 else [], pwrites=[] if c == 0 else [o["r_st"]])
        em.op("dve", lambda e: e.bn_aggr(out=o["mv"][:, 0:2], in_=o["st"][:, :]), reads=[o["r_st"]], writes=[o["r_mv"]])
        em.op("dve", lambda e: e.tensor_scalar(out=o["sd"][:], in0=o["mv"][:, 1:2], scalar1=LN_EPS, scalar2=None,
                                               op0=ALU.add), reads=[o["r_mv"]], writes=[o["r_sd"]])
        em.op("act", lambda e: e.activation(out=o["sd"][:], in_=o["sd"][:], func=AF.Sqrt),
              reads=[o["r_sd"]], writes=[o["r_sd"]])
        em.op("dve", lambda e: e.reciprocal(out=o["sd"][:], in_=o["sd"][:]), reads=[o["r_sd"]], writes=[o["r_sd"]])
        em.op("dve", lambda e: e.tensor_scalar(out=y[:], in0=y[:], scalar1=o["mv"][:, 0:1], scalar2=o["sd"][:, 0:1],
                                               op0=ALU.subtract, op1=ALU.mult),
              reads=[r_y, o["r_mv"], o["r_sd"]], writes=[r_y])
        em.op("pool", lambda e: e.tensor_tensor(out=xo[:], in0=y[:], in1=o["gb"][:], op=ALU.mult),
              reads=[r_y, o["r_gb"]], writes=[r_xo])
        em.op("pool", lambda e: e.tensor_tensor(out=xo[:], in0=xo[:], in1=o["bb"][:], op=ALU.add),
              reads=[r_xo, o["r_gb"]], writes=[r_xo])
        em.dma("sp", store_d[i * 128:(i + 1) * 128, :], xo[:], reads=[r_xo], pwrites=[r_store])
        if xT_dst is not None:
            tbk, qi = i // 4, i % 4
            stg, r_stg = o["stage"][tbk % 2], o["r_stage"][tbk % 2]
            emit_transposes((o["xb"], o["r_xb"], o["tp"], o["r_tp"], stg, r_stg), xo, r_xo, qi)
            if qi == 3:
                em.dma("sp", xT_dst.rearrange("(k p) t -> p k t", p=128)[:, :, tbk * 512:(tbk + 1) * 512], stg[:],
                       reads=[r_stg], pwrites=[r_xT_dst])
        if after_fn is not None:
            after_fn(i, xo, r_xo, o["xb"], o["r_xb"])

    def phase_merge(l, moe_j):
        with ExitStack() as ph:
            lo_ = ln_alloc(ph, "p4", ln_mix_g[l], ln_mix_b[l])
            Wa = sb(ph, "p4_Wa", [128, 4, D], BF16)
            Wb = sb(ph, "p4_Wb", [128, 4, D], BF16)
            Wo = sb(ph, "p4_Wo", [128, KT, D], BF16)
            r_W = Res()
            oa = [sb(ph, f"p4_oa{i}", [128, 4, 512], BF16) for i in range(2)]
            ob = [sb(ph, f"p4_ob{i}", [128, 4, 512], BF16) for i in range(2)]
            sga = [sb(ph, f"p4_sga{i}", [128, KT, 512], BF16) for i in range(2)]
            sgb = [sb(ph, f"p4_sgb{i}", [128, KT, 512], BF16) for i in range(2)]
            r_in = [Res() for _ in range(2)]
            mg = sb(ph, "p4_mg", [128, KT, 512], BF16)
            r_mg = Res()
            t1 = sb(ph, "p4_t1", [128, 512], F32)
            t2 = sb(ph, "p4_t2", [128, 512], F32)
            r_t1, r_t2 = Res(), Res()
            psa = ps(ph, "p4_psa")
            psb = ps(ph, "p4_psb")
            r_psa, r_psb = Res(), Res()
            nbm = 1 if moe_j is not None else 2
            psm = [ps(ph, f"p4_psm{i}") for i in range(nbm * (D // 512))]
            r_psm = [Res() for _ in range(nbm * (D // 512))]
            em.dma("pool", Wa[:], w_ba[l].rearrange("(k p) d -> p k d", p=128), pwrites=[r_W])
            em.dma("pool", Wb[:], w_bb[l].rearrange("(k p) d -> p k d", p=128), pwrites=[r_W])
            for kt in range(KT):
                em.dma("pool", Wo[:, kt, :], w_out[l][kt * 128:(kt + 1) * 128, :], pwrites=[r_W])
            after = None
            if moe_j is not None:
                rt = sb(ph, "p4_rt", [128, KT, NEXP], F32)
                r_rt = Res()
                em.dma("sp", rt[:], moe_router[moe_j].rearrange("(k p) e -> p k e", p=128), writes=[r_rt])
                tpf = ps(ph, "p4_tpf", [128, D], F32)
                r_tpf = Res()
                xTf = sb(ph, "p4_xTf", [128, D], F32)
                r_xTf = Res()
                pcs = ps(ph, "p4_pcs")
                r_pcs = Res()
                zt = sb(ph, "p4_zt", [128, 2 * D], BF16)
                r_zt = Res()
                r_zf = Res()
                em.op("pool", lambda e: e.memset(zt[:], 0.0), writes=[r_zt])
                for r0 in range(0, NEXP * CAP, 256):
                    em.dma("sp", xe_d[r0:r0 + 256, :].rearrange("(p a) d -> p (a d)", a=2), zt[:], reads=[r_zt],
                           pwrites=[R["xe_d"], r_zf])
                mall = sb(ph, "p4_mall", [128, NTQ, NEXP], F32)
                r_mall = Res()
                gi = sb(ph, "p4_gi", [128, NTQ, 2], F32)
                ii = sb(ph, "p4_ii", [128, NTQ, 2], I32)
                r_gi, r_ii = Res(), Res()
                eoff_i = sb(ph, "p4_eoffi", [128, NEXP], I32)
                eoff = sb(ph, "p4_eoff", [128, NEXP], F32)
                r_eoff = Res()
                em.op("pool", lambda e: e.iota(eoff_i[:], pattern=[[CAP, NEXP]], base=0, channel_multiplier=0),
                      writes=[r_eoff])
                em.op("dve", lambda e: e.tensor_copy(out=eoff[:], in_=eoff_i[:]), reads=[r_eoff], writes=[r_eoff])
                s8 = {n: sb(ph, "p4_" + n, [128, NEXP], F32) for n in ("lg", "is1", "l2", "is2", "pos", "v", "ov", "tmp")}
                s1 = {n: sb(ph, "p4_" + n, [128, 1], F32) for n in ("m1", "m2", "d")}
                r_s = {n: Res() for n in list(s8) + list(s1)}

                def after(i, xo, r_xo, xb, r_xb):
                    for kt in range(KT):
                        em.op("pe", lambda e: e.transpose(tpf[:, kt * 128:(kt + 1) * 128],
                                                          xo[:, kt * 128:(kt + 1) * 128], ident_f[:]),
                              reads=[r_xo, r_const], writes=[r_tpf] if kt == 0 else [],
                              pwrites=[] if kt == 0 else [r_tpf], inc=(kt == KT - 1))
                    em.op("act", lambda e: e.activation(out=xTf[:], in_=tpf[:], func=AF.Copy),
                          reads=[r_tpf], writes=[r_xTf])
                    for kt in range(KT):
                        em.op("pe", lambda e: e.matmul(pcs[:, 0:NEXP], lhsT=xTf[:, kt * 128:(kt + 1) * 128],
                                                       rhs=rt[:, kt, :], start=(kt == 0), stop=(kt == KT - 1)),
                              reads=[r_xTf, r_rt], writes=[r_pcs] if kt == 0 else [],
                              pwrites=[] if kt == 0 else [r_pcs], inc=(kt == KT - 1))
                    em.op("dve", lambda e: e.tensor_copy(out=s8["lg"][:], in_=pcs[:, 0:NEXP]),
                          reads=[r_pcs], writes=[r_s["lg"]])
                    em.op("dve", lambda e: e.tensor_reduce(out=s1["m1"][:], in_=s8["lg"][:], axis=AX.X, op=ALU.max),
                          reads=[r_s["lg"]], writes=[r_s["m1"]])
                    em.op("dve", lambda e: e.tensor_scalar(out=s8["is1"][:], in0=s8["lg"][:], scalar1=s1["m1"][:, 0:1],
                                                           scalar2=None, op0=ALU.is_equal),
                          reads=[r_s["lg"], r_s["m1"]], writes=[r_s["is1"]])
                    em.op("dve", lambda e: e.scalar_tensor_tensor(out=s8["l2"][:], in0=s8["is1"][:], scalar=-1e30,
                                                                  in1=s8["lg"][:], op0=ALU.mult, op1=ALU.add),
                          reads=[r_s["is1"], r_s["lg"]], writes=[r_s["l2"]])
                    em.op("dve", lambda e: e.tensor_reduce(out=s1["m2"][:], in_=s8["l2"][:], axis=AX.X, op=ALU.max),
                          reads=[r_s["l2"]], writes=[r_s["m2"]])
                    em.op("dve", lambda e: e.tensor_scalar(out=s8["is2"][:], in0=s8["l2"][:], scalar1=s1["m2"][:, 0:1],
                                                           scalar2=None, op0=ALU.is_equal),
                          reads=[r_s["l2"], r_s["m2"]], writes=[r_s["is2"]])
                    em.op("dve", lambda e: e.tensor_tensor(out=s1["d"][:], in0=s1["m2"][:], in1=s1["m1"][:],
                                                           op=ALU.subtract),
                          reads=[r_s["m1"], r_s["m2"]], writes=[r_s["d"]])
                    em.op("act", lambda e: e.activation(out=gi[:, i, 1:2], in_=s1["d"][:], func=AF.Sigmoid),
                          reads=[r_s["d"]], pwrites=[r_gi])
                    em.op("dve", lambda e: e.tensor_scalar(out=gi[:, i, 0:1], in0=gi[:, i, 1:2], scalar1=-1.0, scalar2=1.0,
                                                           op0=ALU.mult, op1=ALU.add),
                          reads=[r_gi], pwrites=[r_gi])
                    em.op("dve", lambda e: e.tensor_tensor(out=mall[:, i, :], in0=s8["is1"][:], in1=s8["is2"][:],
                                                           op=ALU.add),
                          reads=[r_s["is1"], r_s["is2"]], pwrites=[r_mall])
                    for j in range(i + 1):
                        em.op("pe", lambda e: e.matmul(pcs[:, 8:8 + NEXP], lhsT=(tri_f if j == i else ones_f)[:, :],
                                                       rhs=mall[:, j, :], start=(j == 0), stop=(j == i)),
                              reads=[r_mall, r_const], writes=[r_pcs] if j == 0 else [],
                              pwrites=[] if j == 0 else [r_pcs], inc=(j == i))
                    em.op("dve", lambda e: e.tensor_scalar(out=s8["pos"][:], in0=pcs[:, 8:8 + NEXP], scalar1=-1.0,
                                                           scalar2=None, op0=ALU.add),
                          reads=[r_pcs], writes=[r_s["pos"]])
                    em.op("dve", lambda e: e.tensor_scalar(out=s8["ov"][:], in0=s8["pos"][:], scalar1=float(CAP),
                                                           scalar2=1e6, op0=ALU.is_ge, op1=ALU.mult),
                          reads=[r_s["pos"]], writes=[r_s["ov"]])
                    em.op("dve", lambda e: e.tensor_tensor(out=s8["v"][:], in0=s8["pos"][:], in1=eoff[:], op=ALU.add),
                          reads=[r_s["pos"], r_eoff], writes=[r_s["v"]])
                    em.op("dve", lambda e: e.tensor_tensor(out=s8["v"][:], in0=s8["v"][:], in1=s8["ov"][:], op=ALU.add),
                          reads=[r_s["v"], r_s["ov"]], writes=[r_s["v"]])
                    for w_, nm in enumerate(("is1", "is2")):
                        em.op("dve", lambda e: e.tensor_tensor(out=s8["tmp"][:], in0=s8[nm][:], in1=s8["v"][:],
                                                               op=ALU.mult),
                              reads=[r_s[nm], r_s["v"]], writes=[r_s["tmp"]])
                        em.op("dve", lambda e: e.tensor_reduce(out=s1["m1"][:], in_=s8["tmp"][:], axis=AX.X, op=ALU.add),
                              reads=[r_s["tmp"]], writes=[r_s["m1"]])
                        em.op("dve", lambda e: e.tensor_copy(out=ii[:, i, w_:w_ + 1], in_=s1["m1"][:]),
                              reads=[r_s["m1"]], pwrites=[r_ii])
                        em.dma_fn("pool", lambda g: g.indirect_dma_start(
                            out=xe_d[:, :], out_offset=bass.IndirectOffsetOnAxis(ap=ii[:, i, w_:w_ + 1], axis=0),
                            in_=xb[:, :], in_offset=None, bounds_check=bc_reg[0], oob_is_err=False),
                            reads=[r_ii, r_xb, r_zf], pwrites=[R["xe_d"]])

            xres_d = x_in if l == 0 else xcur_d

            def load_in(tb):
                b = tb % 2
                tsl = slice(tb * 512, (tb + 1) * 512)
                em.dma("sp", oa[b][:], oaT_d.rearrange("(k p) t -> p k t", p=128)[:, :, tsl], reads=[R["oaT_d"]],
                       writes=[r_in[b]])
                em.dma("sp", ob[b][:], obT_d.rearrange("(k p) t -> p k t", p=128)[:, :, tsl], reads=[R["obT_d"]],
                       pwrites=[r_in[b]])
                em.dma("sp", sga[b][:], sgaT_d.rearrange("(k p) t -> p k t", p=128)[:, :, tsl], reads=[R["sgaT_d"]],
                       pwrites=[r_in[b]])
                em.dma("sp", sgb[b][:], sgbT_d.rearrange("(k p) t -> p k t", p=128)[:, :, tsl], reads=[R["sgbT_d"]],
                       pwrites=[r_in[b]])

            load_in(0)
            for tb in range(NTB):
                if tb + 1 < NTB:
                    load_in(tb + 1)
                b = tb % 2
                for dm in range(KT):
                    for k in range(4):
                        em.op("pe", lambda e: e.matmul(psa[:, :], lhsT=Wa[:, k, dm * 128:(dm + 1) * 128],
                                                       rhs=oa[b][:, k, :], start=(k == 0), stop=(k == 3)),
                              reads=[r_W, r_in[b]], writes=[r_psa] if k == 0 else [], pwrites=[] if k == 0 else [r_psa],
                              inc=(k == 3))
                    for k in range(4):
                        em.op("pe", lambda e: e.matmul(psb[:, :], lhsT=Wb[:, k, dm * 128:(dm + 1) * 128],
                                                       rhs=ob[b][:, k, :], start=(k == 0), stop=(k == 3)),
                              reads=[r_W, r_in[b]], writes=[r_psb] if k == 0 else [], pwrites=[] if k == 0 else [r_psb],
                              inc=(k == 3))
                    em.op("dve", lambda e: e.tensor_tensor(out=t1[:], in0=psa[:, :], in1=sga[b][:, dm, :], op=ALU.mult),
                          reads=[r_psa, r_in[b]], writes=[r_t1])
                    em.op("dve", lambda e: e.tensor_tensor(out=t2[:], in0=psb[:, :], in1=sgb[b][:, dm, :], op=ALU.mult),
                          reads=[r_psb, r_in[b]], writes=[r_t2])
                    em.op("pool", lambda e: e.tensor_tensor(out=mg[:, dm, :], in0=t1[:], in1=t2[:], op=ALU.add),
                          reads=[r_t1, r_t2], writes=[r_mg] if dm == 0 else [], pwrites=[] if dm == 0 else [r_mg])
                for qi in range(4):
                    i = tb * 4 + qi
                    halves = []
                    for hf in range(D // 512):
                        pi = (i % nbm) * (D // 512) + hf
                        for k in range(KT):
                            em.op("pe", lambda e: e.matmul(psm[pi][:, :], lhsT=mg[:, k, qi * 128:(qi + 1) * 128],
                                                           rhs=Wo[:, k, hf * 512:(hf + 1) * 512], start=(k == 0),
                                                           stop=(k == KT - 1)),
                                  reads=[r_mg, r_W], writes=[r_psm[pi]] if k == 0 else [],
                                  pwrites=[] if k == 0 else [r_psm[pi]], inc=(k == KT - 1))
                        halves.append((psm[pi][:, :], r_psm[pi]))
                    ln_stage(lo_, i, halves, xres_d, x1_d, R["x1_d"], x1T_d, R["x1T_d"], after)
            if moe_j is not None:
                em.dma("sp", moe_g_d[:, :], gi[:].rearrange("p a b -> p (a b)"), reads=[r_gi], pwrites=[R["moe_d"]])
                em.dma("sp", moe_i_d[:, :], ii[:].rearrange("p a b -> p (a b)"), reads=[r_ii], pwrites=[R["moe_d"]])
            em.barrier()

    def phase_ffn(l, experts, gated, final):
        with ExitStack() as ph:
            lo_ = ln_alloc(ph, "p5", ln_ffn_g[l], ln_ffn_b[l])
            NQ = TBF // 128
            x1T = sb(ph, "p5_xT", [128, KT, TBF], BF16)
            r_x1T = Res()
            yacc = sb(ph, "p5_yacc", [128, NQ, D], F32)
            r_yacc = Res()
            hT = [sb(ph, f"p5_hT{i}", [128, 4, TBF], BF16) for i in range(2)]
            r_hT = [Res() for _ in range(2)]
            Wg = [sb(ph, f"p5_Wg{i}", [128, KT, 512], BF16) for i in range(2)]
            Wu = [sb(ph, f"p5_Wu{i}", [128, KT, 512], BF16) for i in range(2)]
            Wd = [sb(ph, f"p5_Wd{i}", [128, 4, D], BF16) for i in range(2)]
            r_Wc = [Res() for _ in range(2)]
            gbc = sb(ph, "p5_gbc", [128, TBF], F32)
            r_gbc = Res()
            sg = [sb(ph, f"p5_sg{i}", [128, 512], F32) for i in range(2)]
            r_sg = [Res() for _ in range(2)]
            tm = [sb(ph, f"p5_tm{i}", [128, 512], F32) for i in range(2)]
            r_tm = [Res() for _ in range(2)]
            psg = [ps(ph, f"p5_pg{i}") for i in range(2)]
            psu = [ps(ph, f"p5_pu{i}") for i in range(2)]
            psd = [ps(ph, f"p5_pd{i}") for i in range(2)]
            r_psg = [Res() for _ in range(2)]
            r_psu = [Res() for _ in range(2)]
            r_psd = [Res() for _ in range(2)]
            NCH = FT // 4
            work = [(tbf, ei, c) for tbf in range(NTBF) for ei in range(len(experts)) for c in range(NCH)]
            ctr = {"p": 0, "d": 0}

            def load_chunk(n):
                tbf, ei, c = work[n]
                wg_d, wu_d, wd_d = experts[ei]
                b = n % 2
                em.dma("pool", Wg[b][:], wg_d[:, c * 512:(c + 1) * 512].rearrange("(k p) f -> p k f", p=128),
                       writes=[r_Wc[b]])
                em.dma("pool", Wu[b][:], wu_d[:, c * 512:(c + 1) * 512].rearrange("(k p) f -> p k f", p=128),
                       pwrites=[r_Wc[b]])
                em.dma("pool", Wd[b][:], wd_d[c * 512:(c + 1) * 512, :].rearrange("(k p) d -> p k d", p=128),
                       pwrites=[r_Wc[b]])

            load_chunk(0)
            for n, (tbf, ei, c) in enumerate(work):
                if n + 1 < len(work):
                    load_chunk(n + 1)
                b = n % 2
                t0 = tbf * TBF
                if ei == 0 and c == 0:
                    em.dma("sp", x1T[:], x1T_d.rearrange("(k p) t -> p k t", p=128)[:, :, t0:t0 + TBF],
                           reads=[R["x1T_d"]], writes=[r_x1T])
                if gated and c == 0:
                    em.dma("sp", gbc[:], gateT_d[ei, t0:t0 + TBF].partition_broadcast(128), reads=[R["gateT_d"]],
                           writes=[r_gbc])
                for f4 in range(4):
                    for tsub in range(TBF // 512):
                        p = ctr["p"] % 2
                        ctr["p"] += 1
                        tsl = slice(tsub * 512, (tsub + 1) * 512)
                        for kt in range(KT):
                            em.op("pe", lambda e: e.matmul(psg[p][:, :], lhsT=Wg[b][:, kt, f4 * 128:(f4 + 1) * 128],
                                                           rhs=x1T[:, kt, tsl], start=(kt == 0), stop=(kt == KT - 1)),
                                  reads=[r_Wc[b], r_x1T], writes=[r_psg[p]] if kt == 0 else [],
                                  pwrites=[] if kt == 0 else [r_psg[p]], inc=(kt == KT - 1))
                        for kt in range(KT):
                            em.op("pe", lambda e: e.matmul(psu[p][:, :], lhsT=Wu[b][:, kt, f4 * 128:(f4 + 1) * 128],
                                                           rhs=x1T[:, kt, tsl], start=(kt == 0), stop=(kt == KT - 1)),
                                  reads=[r_Wc[b], r_x1T], writes=[r_psu[p]] if kt == 0 else [],
                                  pwrites=[] if kt == 0 else [r_psu[p]], inc=(kt == KT - 1))
                        em.op("act", lambda e: e.activation(out=sg[p][:], in_=psg[p][:, :], func=AF.Silu),
                              reads=[r_psg[p]], writes=[r_sg[p]])
                        first_h = (f4 == 0 and tsub == 0)
                        if gated:
                            em.op("dve", lambda e: e.tensor_tensor(out=tm[p][:], in0=psu[p][:, :], in1=sg[p][:],
                                                                   op=ALU.mult),
                                  reads=[r_psu[p], r_sg[p]], writes=[r_tm[p]])
                            em.op("pool", lambda e: e.tensor_tensor(out=hT[b][:, f4, tsl], in0=tm[p][:],
                                                                    in1=gbc[:, tsl], op=ALU.mult),
                                  reads=[r_tm[p], r_gbc], writes=[r_hT[b]] if first_h else [],
                                  pwrites=[] if first_h else [r_hT[b]])
                        else:
                            em.op("dve", lambda e: e.tensor_tensor(out=hT[b][:, f4, tsl], in0=psu[p][:, :], in1=sg[p][:],
                                                                   op=ALU.mult),
                                  reads=[r_psu[p], r_sg[p]], writes=[r_hT[b]] if first_h else [],
                                  pwrites=[] if first_h else [r_hT[b]])
                first_acc = (ei == 0 and c == 0)
                for q in range(NQ):
                    for hf in range(D // 512):
                        d = ctr["d"] % 2
                        ctr["d"] += 1
                        for f4 in range(4):
                            em.op("pe", lambda e: e.matmul(psd[d][:, :], lhsT=hT[b][:, f4, q * 128:(q + 1) * 128],
                                                           rhs=Wd[b][:, f4, hf * 512:(hf + 1) * 512], start=(f4 == 0),
                                                           stop=(f4 == 3)),
                                  reads=[r_hT[b], r_Wc[b]], writes=[r_psd[d]] if f4 == 0 else [],
                                  pwrites=[] if f4 == 0 else [r_psd[d]], inc=(f4 == 3))
                        ysl = yacc[:, q, hf * 512:(hf + 1) * 512]
                        if first_acc:
                            em.op("dve", lambda e: e.tensor_copy(out=ysl, in_=psd[d][:, :]),
                                  reads=[r_psd[d]], writes=[r_yacc] if (q == 0 and hf == 0) else [],
                                  pwrites=[] if (q == 0 and hf == 0) else [r_yacc])
                        else:
                            em.op("dve", lambda e: e.tensor_tensor(out=ysl, in0=ysl, in1=psd[d][:, :], op=ALU.add),
                                  reads=[r_psd[d], r_yacc], pwrites=[r_yacc])
                if ei == len(experts) - 1 and c == NCH - 1:
                    for q in range(NQ):
                        i = tbf * NQ + q
                        halves = [(yacc[:, q, hf * 512:(hf + 1) * 512], r_yacc) for hf in range(D // 512)]
                        if final:
                            ln_stage(lo_, i, halves, x1_d, out_d, R["out_d"], None, None)
                        else:
                            ln_stage(lo_, i, halves, x1_d, xcur_d, R["xcur_d"], xT_d, R["xT_d"])
            em.barrier()

    def phase_moe(l, j, final):
        NQ = CAP // 128
        NSUB = CAP // 512
        NCH = FT // 4
        with ExitStack() as ph:
            xeT = sb(ph, "pm_xeT", [128, KT, CAP], BF16)
            r_xeT = Res()
            yacc = sb(ph, "pm_yacc", [128, NQ, D], F32)
            r_yacc = Res()
            hT = [sb(ph, f"pm_hT{i}", [128, 4, CAP], BF16) for i in range(2)]
            r_hT = [Res() for _ in range(2)]
            Wg = [sb(ph, f"pm_Wg{i}", [128, KT, 512], BF16) for i in range(2)]
            Wu = [sb(ph, f"pm_Wu{i}", [128, KT, 512], BF16) for i in range(2)]
            Wd = [sb(ph, f"pm_Wd{i}", [128, 4, D], BF16) for i in range(2)]
            r_Wc = [Res() for _ in range(2)]
            sg = [sb(ph, f"pm_sg{i}", [128, 512], F32) for i in range(2)]
            r_sg = [Res() for _ in range(2)]
            xr = [sb(ph, f"pm_xr{i}", [128, D], BF16) for i in range(2)]
            r_xr = [Res() for _ in range(2)]
            tp = ps(ph, "pm_tp", [128, D], BF16)
            r_tp = Res()
            psg = [ps(ph, f"pm_pg{i}") for i in range(2)]
            psu = [ps(ph, f"pm_pu{i}") for i in range(2)]
            psd = [ps(ph, f"pm_pd{i}") for i in range(2)]
            r_psg = [Res() for _ in range(2)]
            r_psu = [Res() for _ in range(2)]
            r_psd = [Res() for _ in range(2)]
            work = [(e, c) for e in range(NEXP) for c in range(NCH)]
            ctr = {"p": 0, "d": 0, "x": 0}

            def load_chunk(n):
                e, c = work[n]
                b = n % 2
                em.dma("pool", Wg[b][:], moe_wg[j][e][:, c * 512:(c + 1) * 512].rearrange("(k p) f -> p k f", p=128),
                       writes=[r_Wc[b]])
                em.dma("pool", Wu[b][:], moe_wu[j][e][:, c * 512:(c + 1) * 512].rearrange("(k p) f -> p k f", p=128),
                       pwrites=[r_Wc[b]])
                em.dma("pool", Wd[b][:], moe_wd[j][e][c * 512:(c + 1) * 512, :].rearrange("(k p) d -> p k d", p=128),
                       pwrites=[r_Wc[b]])

            load_chunk(0)
            for n, (e_, c) in enumerate(work):
                if n + 1 < len(work):
                    load_chunk(n + 1)
                b = n % 2
                if c == 0:
                    for q in range(NQ):
                        xb_ = ctr["x"] % 2
                        ctr["x"] += 1
                        r0 = e_ * CAP + q * 128
                        em.dma("sp", xr[xb_][:], xe_d[r0:r0 + 128, :], reads=[R["xe_d"]], writes=[r_xr[xb_]])
                        for kt in range(KT):
                            em.op("pe", lambda e: e.transpose(tp[:, kt * 128:(kt + 1) * 128],
                                                              xr[xb_][:, kt * 128:(kt + 1) * 128], ident_b[:]),
                                  reads=[r_xr[xb_], r_const], writes=[r_tp] if kt == 0 else [],
                                  pwrites=[] if kt == 0 else [r_tp], inc=(kt == KT - 1))
                        em.op("dve", lambda e: e.tensor_copy(out=xeT[:, :, q * 128:(q + 1) * 128],
                                                             in_=tp[:].rearrange("p (k c) -> p k c", c=128)),
                              reads=[r_tp], writes=[r_xeT] if q == 0 else [], pwrites=[] if q == 0 else [r_xeT])
                for f4 in range(4):
                    for tsub in range(NSUB):
                        p = ctr["p"] % 2
                        ctr["p"] += 1
                        tsl = slice(tsub * 512, (tsub + 1) * 512)
                        for kt in range(KT):
                            em.op("pe", lambda e: e.matmul(psg[p][:, :], lhsT=Wg[b][:, kt, f4 * 128:(f4 + 1) * 128],
                                                           rhs=xeT[:, kt, tsl], start=(kt == 0), stop=(kt == KT - 1)),
                                  reads=[r_Wc[b], r_xeT], writes=[r_psg[p]] if kt == 0 else [],
                                  pwrites=[] if kt == 0 else [r_psg[p]], inc=(kt == KT - 1))
                        for kt in range(KT):
                            em.op("pe", lambda e: e.matmul(psu[p][:, :], lhsT=Wu[b][:, kt, f4 * 128:(f4 + 1) * 128],
                                                           rhs=xeT[:, kt, tsl], start=(kt == 0), stop=(kt == KT - 1)),
                                  reads=[r_Wc[b], r_xeT], writes=[r_psu[p]] if kt == 0 else [],
                                  pwrites=[] if kt == 0 else [r_psu[p]], inc=(kt == KT - 1))
                        em.op("act", lambda e: e.activation(out=sg[p][:], in_=psg[p][:, :], func=AF.Silu),
                              reads=[r_psg[p]], writes=[r_sg[p]])
                        first_h = (f4 == 0 and tsub == 0)
                        em.op("dve", lambda e: e.tensor_tensor(out=hT[b][:, f4, tsl], in0=psu[p][:, :], in1=sg[p][:],
                                                               op=ALU.mult),
                              reads=[r_psu[p], r_sg[p]], writes=[r_hT[b]] if first_h else [],
                              pwrites=[] if first_h else [r_hT[b]])
                for q in range(NQ):
                    for hf in range(D // 512):
                        d = ctr["d"] % 2
                        ctr["d"] += 1
                        for f4 in range(4):
                            em.op("pe", lambda e: e.matmul(psd[d][:, :], lhsT=hT[b][:, f4, q * 128:(q + 1) * 128],
                                                           rhs=Wd[b][:, f4, hf * 512:(hf + 1) * 512], start=(f4 == 0),
                                                           stop=(f4 == 3)),
                                  reads=[r_hT[b], r_Wc[b]], writes=[r_psd[d]] if f4 == 0 else [],
                                  pwrites=[] if f4 == 0 else [r_psd[d]], inc=(f4 == 3))
                        ysl = yacc[:, q, hf * 512:(hf + 1) * 512]
                        if c == 0:
                            em.op("act", lambda e: e.activation(out=ysl, in_=psd[d][:, :], func=AF.Copy),
                                  reads=[r_psd[d]], writes=[r_yacc] if (q == 0 and hf == 0) else [],
                                  pwrites=[] if (q == 0 and hf == 0) else [r_yacc])
                        else:
                            em.op("dve", lambda e: e.tensor_tensor(out=ysl, in0=ysl, in1=psd[d][:, :], op=ALU.add),
                                  reads=[r_psd[d], r_yacc], pwrites=[r_yacc])
                if c == NCH - 1:
                    em.dma("sp", ye_d[e_ * CAP:(e_ + 1) * CAP, :].rearrange("(q p) d -> p q d", p=128), yacc[:],
                           reads=[r_yacc], pwrites=[R["ye_d"]])
            em.barrier()
        with ExitStack() as ph:
            lo_ = ln_alloc(ph, "pc", ln_ffn_g[l], ln_ffn_b[l])
            gi = sb(ph, "pc_gi", [128, NTQ, 2], F32)
            ii = sb(ph, "pc_ii", [128, NTQ, 2], I32)
            r_gi = Res()
            em.dma("sp", gi[:].rearrange("p a b -> p (a b)"), moe_g_d[:, :], reads=[R["moe_d"]], pwrites=[r_gi])
            em.dma("sp", ii[:].rearrange("p a b -> p (a b)"), moe_i_d[:, :], reads=[R["moe_d"]], pwrites=[r_gi])
            y1 = [sb(ph, f"pc_y1{i}", [128, D], F32) for i in range(2)]
            y2 = [sb(ph, f"pc_y2{i}", [128, D], F32) for i in range(2)]
            r_y1 = [Res() for _ in range(2)]
            r_y2 = [Res() for _ in range(2)]
            for i in range(2):
                em.op("pool", lambda e: e.memset(y1[i][:], 0.0), writes=[r_y1[i]])
                em.op("pool", lambda e: e.memset(y2[i][:], 0.0), writes=[r_y2[i]])
            for i in range(NTQ):
                b = i % 2
                em.dma_fn("pool", lambda g: g.indirect_dma_start(
                    out=y1[b][:, :], out_offset=None, in_=ye_d[:, :],
                    in_offset=bass.IndirectOffsetOnAxis(ap=ii[:, i, 0:1], axis=0),
                    bounds_check=bc_reg[0], oob_is_err=False),
                    reads=[R["ye_d"], r_gi], writes=[r_y1[b]])
                em.dma_fn("pool", lambda g: g.indirect_dma_start(
                    out=y2[b][:, :], out_offset=None, in_=ye_d[:, :],
                    in_offset=bass.IndirectOffsetOnAxis(ap=ii[:, i, 1:2], axis=0),
                    bounds_check=bc_reg[0], oob_is_err=False),
                    reads=[R["ye_d"], r_gi], writes=[r_y2[b]])
                em.op("dve", lambda e: e.tensor_scalar(out=y1[b][:], in0=y1[b][:], scalar1=gi[:, i, 0:1], scalar2=None,
                                                       op0=ALU.mult), reads=[r_y1[b], r_gi], writes=[r_y1[b]])
                em.op("dve", lambda e: e.scalar_tensor_tensor(out=y1[b][:], in0=y2[b][:], scalar=gi[:, i, 1:2],
                                                              in1=y1[b][:], op0=ALU.mult, op1=ALU.add),
                      reads=[r_y1[b], r_y2[b], r_gi], writes=[r_y1[b]])
                halves = [(y1[b][:, hf * 512:(hf + 1) * 512], r_y1[b]) for hf in range(D // 512)]
                if final:
                    ln_stage(lo_, i, halves, x1_d, out_d, R["out_d"], None, None)
                else:
                    ln_stage(lo_, i, halves, x1_d, xcur_d, R["xcur_d"], xT_d, R["xT_d"])
            em.barrier()

    bc_reg[0] = nc.gpsimd.to_reg(NEXP * CAP - 1)
    phase_tables()
    phase_x0()
    for l in range(DEPTH):
        j = l // 2
        is_moe = (l % 2 == 1)
        phase_proj(l)
        phase_fox()
        phase_dsa()
        phase_merge(l, j if is_moe else None)
        if is_moe:
            phase_moe(l, j, l == DEPTH - 1)
        else:
            phase_ffn(l, [(ffn_wg[j], ffn_wu[j], ffn_wd[j])], False, l == DEPTH - 1)
    top.close()
    return nc, em


_INPUT_ORDER = ["x", "positions", "w_in", "b_forget", "w_branch_a", "w_branch_b", "w_out", "ln_mix_g", "ln_mix_b",
                "ln_ffn_g", "ln_ffn_b", "ffn_w_gate", "ffn_w_up", "ffn_w_down", "moe_router", "moe_w_gate",
                "moe_w_up", "moe_w_down"]


def kernel(**inputs):
    x = np.asarray(inputs["x"])
    B, T, D = x.shape
    depth = int(np.asarray(inputs["w_in"]).shape[0])
    DFF = int(np.asarray(inputs["ffn_w_gate"]).shape[-1])
    nc, em = build_program(T, D, DFF, depth)
    shared = {k: np.ascontiguousarray(np.asarray(inputs[k])) for k in _INPUT_ORDER if k not in ("x", "positions")}
    in_maps = []
    for b in range(B):
        m = dict(shared)
        m["x"] = np.ascontiguousarray(x[b])
        m["positions"] = np.ascontiguousarray(np.asarray(inputs["positions"])[b:b + 1]).astype(np.int32)
        in_maps.append(m)
    res = run_bass_kernel_spmd(nc, in_maps, core_ids=list(range(B)))
    return np.stack([np.asarray(r["out"]) for r in res.results], axis=0).astype(np.float32)
```

```python
import math
from contextlib import ExitStack

import numpy as np
import concourse.bass as bass
import concourse.mybir as mybir
from concourse.bass_utils import run_bass_kernel_spmd

F32 = mybir.dt.float32
BF16 = mybir.dt.bfloat16
I32 = mybir.dt.int32
AF = mybir.ActivationFunctionType
ALU = mybir.AluOpType
AX = mybir.AxisListType

N_CORES = 8
PROJ_SIZES = (512, 512, 512, 512, 64, 8, 512, 512, 512, 8, 1024, 1024)
PROJ_TOTAL = sum(PROJ_SIZES)
OFF = dict(dq=0, dk=512, dv=1024, iq=1536, ik=2048, iw=2112, fq=2120, fk=2632, fv=3144,
           fl=3656, ga=3664, gb=4688)
ROPE_THETA = 10000.0
LN_EPS = 1e-5
NEG = -30000.0


class Res:
    __slots__ = ("name", "w", "r", "x")

    def __init__(self, name=""):
        self.name = name
        self.w = {}
        self.r = {}
        self.x = None


def _key(tok):
    return tok[:2] if tok[0] == "c" else tok[:3]


class Emitter:
    COMPUTE = ("pe", "act", "dve", "pool")
    QUEUES = ("sp", "act", "pool")

    def __init__(self, nc, stack, n_dma_sems=10):
        self.nc = nc
        self.engs = {"pe": nc.tensor, "act": nc.scalar, "dve": nc.vector,
                     "pool": nc.gpsimd, "sp": nc.sync}
        self.sem = {e: stack.enter_context(nc.semaphore("c_" + e)) for e in self.COMPUTE}
        self.cnt = {e: 0 for e in self.COMPUTE}
        self.dsem = {q: [stack.enter_context(nc.semaphore(f"d_{q}{i}")) for i in range(n_dma_sems)]
                     for q in self.QUEUES}
        self.dcnt = {q: 0 for q in self.QUEUES}
        self.nd = n_dma_sems
        self.seen = {e: {} for e in self.engs}
        self.pend = []
        self.n_ins = 0
        self.n_wait = 0

    def _wait(self, e, tok):
        if tok[0] == "c":
            _, f, n = tok
            if f == "pe" and e == "pe":
                return
            key = ("c", f)
            s = self.sem[f]
        else:
            _, q, idx, n = tok
            key = ("d", q, idx)
            s = self.dsem[q][idx]
        if self.seen[e].get(key, 0) >= n:
            return
        self.seen[e][key] = n
        self.pend.append((s, n))

    def _deps(self, e, reads, writes, pwrites):
        for r in reads:
            for t in r.w.values():
                self._wait(e, t)
        for w in writes:
            for t in w.w.values():
                self._wait(e, t)
            for t in w.r.values():
                self._wait(e, t)
        for w in pwrites:
            for t in w.r.values():
                self._wait(e, t)
            if w.x is not None:
                self._wait(e, w.x)

    def _record(self, tok, reads, writes, pwrites):
        k = _key(tok)
        for r in reads:
            r.r[k] = tok
        for w in writes:
            w.w = {k: tok}
            w.r = {}
            w.x = tok
        for w in pwrites:
            w.w[k] = tok

    def _flush(self, e, ins_fn):
        pend, self.pend = self.pend, []
        if pend:
            for (s, n) in pend[:-1]:
                self.engs[e].wait_ge(s, n)
                self.n_wait += 1
            ins = ins_fn()
            ins._wait_ge(pend[-1][0], pend[-1][1])
            return ins
        return ins_fn()

    def op(self, e, fn, reads=(), writes=(), pwrites=(), inc=True):
        self._deps(e, reads, writes, pwrites)
        ins = self._flush(e, lambda: fn(self.engs[e]))
        if inc:
            self.cnt[e] += 1
            ins.then_inc(self.sem[e], 1)
            tok = ("c", e, self.cnt[e])
        else:
            tok = ("c", e, self.cnt[e] + 1)
        self._record(tok, reads, writes, pwrites)
        self.n_ins += 1
        return tok

    def dma(self, q, out, in_, reads=(), writes=(), pwrites=(), **kw):
        i = self.dcnt[q]
        idx = i % self.nd
        prev = 16 * (i // self.nd)
        if prev > 0:
            self._wait(q, ("d", q, idx, prev))
        self._deps(q, reads, writes, pwrites)
        ins = self._flush(q, lambda: self.engs[q].dma_start(out=out, in_=in_, **kw))
        ins.then_inc(self.dsem[q][idx], 16)
        self.dcnt[q] += 1
        tok = ("d", q, idx, prev + 16)
        self._record(tok, reads, writes, pwrites)
        self.n_ins += 1
        return tok

    def dma_fn(self, q, fn, reads=(), writes=(), pwrites=()):
        i = self.dcnt[q]
        idx = i % self.nd
        prev = 16 * (i // self.nd)
        if prev > 0:
            self._wait(q, ("d", q, idx, prev))
        self._deps(q, reads, writes, pwrites)
        ins = self._flush(q, lambda: fn(self.engs[q]))
        ins.then_inc(self.dsem[q][idx], 16)
        self.dcnt[q] += 1
        tok = ("d", q, idx, prev + 16)
        self._record(tok, reads, writes, pwrites)
        self.n_ins += 1
        return tok

    def barrier(self, engines=None):
        for e in (engines or list(self.engs)):
            for f in self.COMPUTE:
                if self.cnt[f] > 0:
                    self._wait(e, ("c", f, self.cnt[f])) if not (e == "pe" and f == "pe") else None
            if e == "pe" and self.cnt["pe"] > 0 and self.seen["pe"].get(("c", "pe"), 0) < self.cnt["pe"]:
                self.seen["pe"][("c", "pe")] = self.cnt["pe"]
                self.pend.append((self.sem["pe"], self.cnt["pe"]))
            for q in self.QUEUES:
                for idx in range(self.nd):
                    n_used = (self.dcnt[q] - idx + self.nd - 1) // self.nd if self.dcnt[q] > idx else 0
                    if n_used > 0:
                        self._wait(e, ("d", q, idx, 16 * n_used))
            pend, self.pend = self.pend, []
            for (s, n) in pend:
                self.engs[e].wait_ge(s, n)
                self.n_wait += 1


def build_program(T, D, DFF, DEPTH, NEXP=8, bisect_iters=12):
    assert T % 512 == 0 and D % 512 == 0 and DFF % 512 == 0
    KT = D // 128
    NTQ = T // 128
    NTB = T // 512
    FT = DFF // 128
    TOPK = min(256, T // 4)
    n_dense = (DEPTH + 1) // 2
    n_moe = DEPTH // 2
    alpha = (2.0 * DEPTH) ** 0.25
    TBF = 1024 if T % 1024 == 0 else 512
    NTBF = T // TBF

    nc = bass.Bass("TRN2", target_bir_lowering=False)

    def din(name, shape, dt=F32):
        return nc.dram_tensor(name, list(shape), dt, kind="ExternalInput").ap()

    def dscr(name, shape, dt):
        return nc.dram_tensor(name, list(shape), dt).ap()

    x_in = din("x", [T, D])
    pos_in = din("positions", [1, T], I32)
    w_in = din("w_in", [DEPTH, D, PROJ_TOTAL])
    b_forget = din("b_forget", [DEPTH, 8])
    w_ba = din("w_branch_a", [DEPTH, 512, D])
    w_bb = din("w_branch_b", [DEPTH, 512, D])
    w_out = din("w_out", [DEPTH, D, D])
    ln_mix_g = din("ln_mix_g", [DEPTH, D])
    ln_mix_b = din("ln_mix_b", [DEPTH, D])
    ln_ffn_g = din("ln_ffn_g", [DEPTH, D])
    ln_ffn_b = din("ln_ffn_b", [DEPTH, D])
    ffn_wg = din("ffn_w_gate", [n_dense, D, DFF])
    ffn_wu = din("ffn_w_up", [n_dense, D, DFF])
    ffn_wd = din("ffn_w_down", [n_dense, DFF, D])
    moe_router = din("moe_router", [max(n_moe, 1), D, NEXP])
    moe_wg = din("moe_w_gate", [max(n_moe, 1), NEXP, D, DFF])
    moe_wu = din("moe_w_up", [max(n_moe, 1), NEXP, D, DFF])
    moe_wd = din("moe_w_down", [max(n_moe, 1), NEXP, DFF, D])
    out_d = nc.dram_tensor("out", [T, D], F32, kind="ExternalOutput").ap()

    xT_d = dscr("xT_d", [D, T], BF16)
    x1T_d = dscr("x1T_d", [D, T], BF16)
    xcur_d = dscr("xcur_d", [T, D], F32)
    x1_d = dscr("x1_d", [T, D], F32)
    cosT_d = dscr("cosT_d", [128, T], F32)
    sinT_d = dscr("sinT_d", [128, T], F32)
    dqT_d = dscr("dqT_d", [512, T], BF16)
    dkT_d = dscr("dkT_d", [512, T], BF16)
    iqT_d = dscr("iqT_d", [512, T], BF16)
    ikT_d = dscr("ikT_d", [64, T], BF16)
    fqT_d = dscr("fqT_d", [512, T], BF16)
    fkT_d = dscr("fkT_d", [512, T], BF16)
    dv_d = dscr("dv_d", [T, 768], BF16)
    fv_d = dscr("fv_d", [T, 768], BF16)
    wabs_d = dscr("wabs_d", [128, NTQ * 8], F32)
    wsgn_d = dscr("wsgn_d", [128, NTQ * 8], F32)
    aug_d = dscr("aug_d", [96, T], BF16)
    sgaT_d = dscr("sgaT_d", [D, T], BF16)
    sgbT_d = dscr("sgbT_d", [D, T], BF16)
    oaT_d = dscr("oaT_d", [512, T], BF16)
    obT_d = dscr("obT_d", [512, T], BF16)
    gateT_d = dscr("gateT_d", [NEXP, T], F32)
    CAP = max(512, ((T * 3 // 8) + 511) // 512 * 512)
    xe_d = dscr("xe_d", [NEXP * CAP, D], BF16)
    ye_d = dscr("ye_d", [NEXP * CAP, D], F32)
    moe_g_d = dscr("moe_g_d", [128, NTQ * 2], F32)
    moe_i_d = dscr("moe_i_d", [128, NTQ * 2], I32)

    R = {n: Res(n) for n in ("xT_d", "x1T_d", "xcur_d", "x1_d", "tab_d", "dqT_d", "dkT_d", "iqT_d",
                             "ikT_d", "fqT_d", "fkT_d", "dv_d", "fv_d", "w_d", "aug_d", "sgaT_d",
                             "sgbT_d", "oaT_d", "obT_d", "gateT_d", "out_d", "xe_d", "ye_d", "moe_d")}

    top = ExitStack()
    em = Emitter(nc, top)

    uid = [0]
    bc_reg = [None]

    def sb(st, name, shape, dt):
        uid[0] += 1
        return st.enter_context(nc.sbuf_tensor(f"{name}_{uid[0]}", list(shape), dt))

    def ps(st, name, shape=(128, 512), dt=F32):
        uid[0] += 1
        return st.enter_context(nc.psum_tensor(f"{name}_{uid[0]}", list(shape), dt))

    ident_b = sb(top, "ident_b", [128, 128], BF16)
    ident_f = sb(top, "ident_f", [128, 128], F32)
    tri_b = sb(top, "tri_b", [128, 128], BF16)
    tri_f = sb(top, "tri_f", [128, 128], F32)
    ones_f = sb(top, "ones_f", [128, 128], F32)
    r_const = Res("const")

    with ExitStack() as ph:
        io_i = sb(ph, "io_i", [128, 128], I32)
        r_io = Res()
        em.op("pool", lambda e: e.iota(io_i[:], pattern=[[1, 128]], base=0, channel_multiplier=-1),
              writes=[r_io])
        em.op("dve", lambda e: e.tensor_scalar(out=ident_f[:], in0=io_i[:], scalar1=0.0, scalar2=None,
                                               op0=ALU.is_equal), reads=[r_io], pwrites=[r_const])
        em.op("dve", lambda e: e.tensor_scalar(out=ident_b[:], in0=io_i[:], scalar1=0.0, scalar2=None,
                                               op0=ALU.is_equal), reads=[r_io], pwrites=[r_const])
        em.op("dve", lambda e: e.tensor_scalar(out=tri_f[:], in0=io_i[:], scalar1=0.0, scalar2=None,
                                               op0=ALU.is_ge), reads=[r_io], pwrites=[r_const])
        em.op("dve", lambda e: e.tensor_scalar(out=tri_b[:], in0=io_i[:], scalar1=0.0, scalar2=None,
                                               op0=ALU.is_ge), reads=[r_io], pwrites=[r_const])
        em.op("dve", lambda e: e.memset(ones_f[:], 1.0), pwrites=[r_const])
        em.barrier()

    def phase_tables():
        with ExitStack() as ph:
            posi = sb(ph, "posi", [128, T], I32)
            ang = sb(ph, "ang", [128, T], F32)
            a2 = sb(ph, "a2", [128, T], F32)
            u = sb(ph, "u", [128, T], F32)
            ni = sb(ph, "ni", [128, T], I32)
            nf = sb(ph, "nf", [128, T], F32)
            ji = sb(ph, "ji", [128, 1], I32)
            jf = sb(ph, "jf", [128, 1], F32)
            invf = sb(ph, "invf", [128, 1], F32)
            r_pos, r_ang, r_a2, r_u, r_ni, r_nf, r_j, r_jf, r_inv = (Res() for _ in range(9))
            em.dma("sp", posi[:], pos_in.to_broadcast([128, T]), writes=[r_pos])
            for g in range(4):
                em.op("pool", lambda e: e.iota(ji[32 * g:32 * g + 32, :], pattern=[[0, 1]], base=0,
                                               channel_multiplier=1), pwrites=[r_j])
            em.op("dve", lambda e: e.tensor_copy(out=jf[:], in_=ji[:]), reads=[r_j], writes=[r_jf])
            em.op("act", lambda e: e.activation(out=invf[:], in_=jf[:], func=AF.Exp,
                                                scale=-math.log(ROPE_THETA) / 32.0),
                  reads=[r_jf], writes=[r_inv])
            em.op("dve", lambda e: e.tensor_copy(out=ang[:], in_=posi[:]), reads=[r_pos], writes=[r_ang])
            em.op("dve", lambda e: e.tensor_scalar(out=ang[:], in0=ang[:], scalar1=invf[:, 0:1], scalar2=None,
                                                   op0=ALU.mult), reads=[r_ang, r_inv], writes=[r_ang])
            C1 = 6.28125
            C2 = 2.0 * math.pi - C1
            PI_LO = 3.1415925
            for (dst, shift) in ((sinT_d, 0.0), (cosT_d, math.pi / 2)):
                em.op("dve", lambda e: e.tensor_scalar(out=a2[:], in0=ang[:], scalar1=shift, scalar2=None,
                                                       op0=ALU.add), reads=[r_ang], writes=[r_a2])
                em.op("dve", lambda e: e.tensor_scalar(out=u[:], in0=a2[:], scalar1=1.0 / (2 * math.pi),
                                                       scalar2=0.5, op0=ALU.mult, op1=ALU.add),
                      reads=[r_a2], writes=[r_u])
                em.op("dve", lambda e: e.tensor_copy(out=ni[:], in_=u[:]), reads=[r_u], writes=[r_ni])
                em.op("dve", lambda e: e.tensor_copy(out=nf[:], in_=ni[:]), reads=[r_ni], writes=[r_nf])
                em.op("dve", lambda e: e.scalar_tensor_tensor(out=a2[:], in0=nf[:], scalar=-C1, in1=a2[:],
                                                              op0=ALU.mult, op1=ALU.add),
                      reads=[r_nf, r_a2], writes=[r_a2])
                em.op("dve", lambda e: e.scalar_tensor_tensor(out=a2[:], in0=nf[:], scalar=-C2, in1=a2[:],
                                                              op0=ALU.mult, op1=ALU.add),
                      reads=[r_nf, r_a2], writes=[r_a2])
                em.op("dve", lambda e: e.tensor_scalar(out=u[:], in0=a2[:], scalar1=-math.pi,
                                                       scalar2=2 * math.pi, op0=ALU.is_lt, op1=ALU.mult),
                      reads=[r_a2], writes=[r_u])
                em.op("dve", lambda e: e.tensor_tensor(out=a2[:], in0=a2[:], in1=u[:], op=ALU.add),
                      reads=[r_a2, r_u], writes=[r_a2])
                em.op("dve", lambda e: e.tensor_scalar(out=u[:], in0=a2[:], scalar1=math.pi,
                                                       scalar2=-2 * math.pi, op0=ALU.is_gt, op1=ALU.mult),
                      reads=[r_a2], writes=[r_u])
                em.op("dve", lambda e: e.tensor_tensor(out=a2[:], in0=a2[:], in1=u[:], op=ALU.add),
                      reads=[r_a2, r_u], writes=[r_a2])
                em.op("dve", lambda e: e.tensor_scalar(out=a2[:], in0=a2[:], scalar1=-PI_LO, scalar2=PI_LO,
                                                       op0=ALU.max, op1=ALU.min), reads=[r_a2], writes=[r_a2])
                em.op("act", lambda e: e.activation(out=u[:], in_=a2[:], func=AF.Sin),
                      reads=[r_a2], writes=[r_u])
                em.dma("sp", dst[:, :], u[:], reads=[r_u], pwrites=[R["tab_d"]])
            em.barrier()

    def emit_transposes(ph_objs, src_tile, r_src, qi):
        xb, r_xb, tp, r_tp, stage, r_stage = ph_objs
        em.op("act", lambda e: e.activation(out=xb[:], in_=src_tile[:], func=AF.Copy),
              reads=[r_src], writes=[r_xb])
        for kt in range(KT):
            em.op("pe", lambda e: e.transpose(tp[:, kt * 128:(kt + 1) * 128], xb[:, kt * 128:(kt + 1) * 128],
                                              ident_b[:]),
                  reads=[r_xb, r_const], writes=[r_tp] if kt == 0 else [], pwrites=[] if kt == 0 else [r_tp],
                  inc=(kt == KT - 1))
        em.op("dve", lambda e: e.tensor_copy(out=stage[:, :, qi * 128:(qi + 1) * 128],
                                             in_=tp[:].rearrange("p (k c) -> p k c", c=128)),
              reads=[r_tp], pwrites=[r_stage])

    def phase_x0():
        with ExitStack() as ph:
            xt = [sb(ph, f"x0_{i}", [128, D], F32) for i in range(2)]
            r_xt = [Res() for _ in range(2)]
            xb = sb(ph, "x0b", [128, D], BF16)
            tp = ps(ph, "x0tp", [128, D], BF16)
            stage = [sb(ph, f"x0s{i}", [128, KT, 512], BF16) for i in range(2)]
            r_stage = [Res() for _ in range(2)]
            r_xb, r_tp = Res(), Res()
            for i in range(NTQ):
                b = i % 2
                tb, qi = i // 4, i % 4
                em.dma("sp", xt[b][:], x_in[i * 128:(i + 1) * 128, :], writes=[r_xt[b]])
                emit_transposes((xb, r_xb, tp, r_tp, stage[tb % 2], r_stage[tb % 2]), xt[b], r_xt[b], qi)
                if qi == 3:
                    em.dma("sp", xT_d.rearrange("(k p) t -> p k t", p=128)[:, :, tb * 512:(tb + 1) * 512],
                           stage[tb % 2][:], reads=[r_stage[tb % 2]], pwrites=[R["xT_d"]])
            em.barrier()

    def phase_proj(l):
        with ExitStack() as ph:
            xT = sb(ph, "p1_xT", [128, KT, T], BF16)
            r_xT = Res()
            cosT = sb(ph, "p1_cos", [128, T], F32)
            sinT = sb(ph, "p1_sin", [128, T], F32)
            r_tab = Res()
            wb = [sb(ph, f"p1_w{i}", [128, KT, 512], BF16) for i in range(2)]
            r_wb = [Res() for _ in range(2)]
            wsw = sb(ph, "p1_wsw", [128, KT, 512], BF16)
            r_wsw = Res()
            stage = [sb(ph, f"p1_st{i}", [128, 512], BF16) for i in range(3)]
            r_st = [Res() for _ in range(3)]
            t1 = sb(ph, "p1_t1", [128, 512], F32)
            t2 = sb(ph, "p1_t2", [128, 512], F32)
            r_t1, r_t2 = Res(), Res()
            stv = [sb(ph, f"p1_sv{i}", [128, 4, 192], BF16) for i in range(2)]
            r_stv = [Res() for _ in range(2)]
            psA = [ps(ph, f"p1_pA{i}") for i in range(2)]
            psB = [ps(ph, f"p1_pB{i}") for i in range(2)]
            r_pA = [Res() for _ in range(2)]
            r_pB = [Res() for _ in range(2)]
            pss = ps(ph, "p1_pss")
            r_pss = Res()
            pst = ps(ph, "p1_pst")
            r_pst = Res()
            bfb = sb(ph, "p1_bf", [128, 8], F32)
            r_bfb = Res()
            logf = sb(ph, "p1_logf", [128, NTQ, 8], F32)
            r_logf = Res()
            wabs = sb(ph, "p1_wabs", [128, NTQ, 8], F32)
            wsgn = sb(ph, "p1_wsgn", [128, NTQ, 8], F32)
            r_wab, r_wsg = Res(), Res()
            augT = sb(ph, "p1_augT", [96, T], BF16)
            r_augT = Res()
            A = sb(ph, "p1_A", [128, 96], F32)
            r_A = Res()
            sm = {n: sb(ph, "p1_" + n, [128, 8], F32) for n in ("z", "e", "c", "hif", "d1", "midf", "d2", "lof")}
            smb = {n: sb(ph, "p1_" + n, [128, 8], BF16) for n in ("hib", "midb", "lob")}
            r_sm = {n: Res() for n in list(sm) + list(smb)}

            em.dma("sp", xT[:], xT_d.rearrange("(k p) t -> p k t", p=128), reads=[R["xT_d"]], writes=[r_xT])
            em.dma("sp", cosT[:], cosT_d[:, :], reads=[R["tab_d"]], pwrites=[r_tab])
            em.dma("sp", sinT[:], sinT_d[:, :], reads=[R["tab_d"]], pwrites=[r_tab])
            em.dma("sp", bfb[:], b_forget[l].partition_broadcast(128), writes=[r_bfb])
            em.op("dve", lambda e: e.memset(A[:], 1.0), writes=[r_A])
            for i in range(2):
                em.op("pool", lambda e: e.memset(stv[i][:], 1.0), writes=[r_stv[i]])

            wl = w_in[l]
            groups = [("dq", 512, "rot", dqT_d, 0.125), ("dk", 512, "rot", dkT_d, 1.0),
                      ("dv", 512, "tok", dv_d, 1.0), ("iq", 512, "rot", iqT_d, 0.125),
                      ("ik", 72, "ikiw", ikT_d, 1.0),
                      ("fq", 512, "plain", fqT_d, 0.125), ("fk", 512, "plain", fkT_d, 1.0),
                      ("fv", 512, "tok", fv_d, 1.0), ("fl", 8, "fl", None, 1.0),
                      ("ga", 512, "sig", sgaT_d, 0), ("ga2", 512, "sig", sgaT_d, 512),
                      ("gb", 512, "sig", sgbT_d, 0), ("gb2", 512, "sig", sgbT_d, 512)]
            if D != 1024:
                raise NotImplementedError

            def colstart(name):
                if name == "ga2":
                    return OFF["ga"] + 512
                if name == "gb2":
                    return OFF["gb"] + 512
                return OFF[name]

            def load_w(gi):
                name, ncol = groups[gi][0], groups[gi][1]
                c0 = colstart(name)
                b = gi % 2
                em.dma("pool", wb[b][:, :, 0:ncol],
                       wl[:, c0:c0 + ncol].rearrange("(k p) c -> p k c", p=128), writes=[r_wb[b]])

            cnt = {"st": 0, "pp": 0, "sv": 0}

            def fm_tile(W, r_W, Wsw, col0, M, tb, kind, scale, dst, drow0):
                p = cnt["pp"] % 2
                cnt["pp"] += 1
                tsl = slice(tb * 512, (tb + 1) * 512)
                for kt in range(KT):
                    em.op("pe", lambda e: e.matmul(psA[p][0:M, :], lhsT=W[:, kt, col0:col0 + M],
                                                   rhs=xT[:, kt, tsl], start=(kt == 0), stop=(kt == KT - 1)),
                          reads=[r_W, r_xT], writes=[r_pA[p]] if kt == 0 else [], pwrites=[] if kt == 0 else [r_pA[p]],
                          inc=(kt == KT - 1))
                if kind == "rot":
                    for kt in range(KT):
                        em.op("pe", lambda e: e.matmul(psB[p][0:M, :], lhsT=Wsw[:, kt, col0:col0 + M],
                                                       rhs=xT[:, kt, tsl], start=(kt == 0), stop=(kt == KT - 1)),
                              reads=[r_wsw, r_xT], writes=[r_pB[p]] if kt == 0 else [],
                              pwrites=[] if kt == 0 else [r_pB[p]], inc=(kt == KT - 1))
                s = cnt["st"] % 3
                cnt["st"] += 1
                if kind == "rot":
                    em.op("dve", lambda e: e.scalar_tensor_tensor(out=t1[0:M, :], in0=psA[p][0:M, :], scalar=scale,
                                                                  in1=cosT[0:M, tsl], op0=ALU.mult, op1=ALU.mult),
                          reads=[r_pA[p], r_tab], writes=[r_t1])
                    em.op("dve", lambda e: e.scalar_tensor_tensor(out=t2[0:M, :], in0=psB[p][0:M, :], scalar=scale,
                                                                  in1=sinT[0:M, tsl], op0=ALU.mult, op1=ALU.mult),
                          reads=[r_pB[p], r_tab], writes=[r_t2])
                    em.op("pool", lambda e: e.tensor_tensor(out=stage[s][0:M, :], in0=t1[0:M, :], in1=t2[0:M, :],
                                                            op=ALU.add),
                          reads=[r_t1, r_t2], writes=[r_st[s]])
                elif kind == "plain":
                    em.op("act", lambda e: e.activation(out=stage[s][0:M, :], in_=psA[p][0:M, :], func=AF.Copy,
                                                        scale=scale), reads=[r_pA[p]], writes=[r_st[s]])
                else:
                    em.op("act", lambda e: e.activation(out=stage[s][0:M, :], in_=psA[p][0:M, :], func=AF.Sigmoid),
                          reads=[r_pA[p]], writes=[r_st[s]])
                em.dma("sp", dst[drow0:drow0 + M, tsl], stage[s][0:M, :], reads=[r_st[s]], pwrites=[R[_rn(dst)]])

            names = {id(dqT_d): "dqT_d", id(dkT_d): "dkT_d", id(iqT_d): "iqT_d", id(ikT_d): "ikT_d",
                     id(fqT_d): "fqT_d", id(fkT_d): "fkT_d", id(sgaT_d): "sgaT_d", id(sgbT_d): "sgbT_d",
                     id(dv_d): "dv_d", id(fv_d): "fv_d"}

            def _rn(d):
                return names[id(d)]

            def make_swapped(W, r_W, ncol):
                nh = ncol // 64
                Wv = W[:, :, 0:ncol].rearrange("p k (h two j) -> p k h two j", two=2, j=32)
                Sv = wsw[:, :, 0:ncol].rearrange("p k (h two j) -> p k h two j", two=2, j=32)
                for kt in range(KT):
                    em.op("act", lambda e: e.activation(out=Sv[:, kt, :, 0, :], in_=Wv[:, kt, :, 1, :], func=AF.Copy,
                                                        scale=-1.0),
                          reads=[r_W], writes=[r_wsw] if kt == 0 else [], pwrites=[] if kt == 0 else [r_wsw])
                    em.op("pool", lambda e: e.tensor_copy(out=Sv[:, kt, :, 1, :], in_=Wv[:, kt, :, 0, :]),
                          reads=[r_W], pwrites=[r_wsw])

            load_w(0)
            for gi, (name, ncol, kind, dst, extra) in enumerate(groups):
                if gi + 1 < len(groups):
                    load_w(gi + 1)
                W, r_W = wb[gi % 2], r_wb[gi % 2]
                if kind in ("rot",):
                    make_swapped(W, r_W, ncol)
                    for ct in range(ncol // 128):
                        for tb in range(NTB):
                            fm_tile(W, r_W, wsw, ct * 128, 128, tb, "rot", extra, dst, ct * 128)
                elif kind == "plain":
                    for ct in range(ncol // 128):
                        for tb in range(NTB):
                            fm_tile(W, r_W, None, ct * 128, 128, tb, "plain", extra, dst, ct * 128)
                elif kind == "sig":
                    for ct in range(ncol // 128):
                        for tb in range(NTB):
                            fm_tile(W, r_W, None, ct * 128, 128, tb, "sig", 1.0, dst, extra + ct * 128)
                elif kind == "ikiw":
                    make_swapped(W, r_W, 64)
                    for tb in range(NTB):
                        fm_tile(W, r_W, wsw, 0, 64, tb, "rot", 1.0, dst, 0)
                    for i in range(NTQ):
                        for kt in range(KT):
                            em.op("pe", lambda e: e.matmul(pss[:, 0:8], lhsT=xT[:, kt, i * 128:(i + 1) * 128],
                                                           rhs=W[:, kt, 64:72], start=(kt == 0), stop=(kt == KT - 1)),
                                  reads=[r_W, r_xT], writes=[r_pss] if kt == 0 else [],
                                  pwrites=[] if kt == 0 else [r_pss], inc=(kt == KT - 1))
                        em.op("act", lambda e: e.activation(out=wabs[:, i, :], in_=pss[:, 0:8], func=AF.Abs,
                                                            scale=8.0 ** -0.5),
                              reads=[r_pss], pwrites=[r_wab])
                        em.op("act", lambda e: e.activation(out=wsgn[:, i, :], in_=pss[:, 0:8], func=AF.Sign),
                              reads=[r_pss], pwrites=[r_wsg])
                    em.dma("sp", wabs_d[:, :], wabs[:].rearrange("p a b -> p (a b)"), reads=[r_wab], pwrites=[R["w_d"]])
                    em.dma("sp", wsgn_d[:, :], wsgn[:].rearrange("p a b -> p (a b)"), reads=[r_wsg], pwrites=[R["w_d"]])
                elif kind == "tok":
                    for i in range(NTQ):
                        p = cnt["pp"] % 2
                        cnt["pp"] += 1
                        for kt in range(KT):
                            em.op("pe", lambda e: e.matmul(psA[p][:, :], lhsT=xT[:, kt, i * 128:(i + 1) * 128],
                                                           rhs=W[:, kt, 0:512], start=(kt == 0), stop=(kt == KT - 1)),
                                  reads=[r_W, r_xT], writes=[r_pA[p]] if kt == 0 else [],
                                  pwrites=[] if kt == 0 else [r_pA[p]], inc=(kt == KT - 1))
                        s = cnt["sv"] % 2
                        cnt["sv"] += 1
                        sv = stv[s][:].rearrange("p a (three c) -> p a three c", c=64)
                        pv = psA[p][:, :].rearrange("p (a two c) -> p a two c", two=2, c=64)
                        em.op("act", lambda e: e.activation(out=sv[:, :, 0, :], in_=pv[:, :, 0, :], func=AF.Copy),
                              reads=[r_pA[p]], writes=[r_stv[s]])
                        em.op("dve", lambda e: e.tensor_copy(out=sv[:, :, 2, :], in_=pv[:, :, 1, :]),
                              reads=[r_pA[p]], pwrites=[r_stv[s]])
                        em.dma("sp", dst[i * 128:(i + 1) * 128, :], stv[s][:].rearrange("p a c -> p (a c)"),
                               reads=[r_stv[s]], pwrites=[R[_rn(dst)]])
                elif kind == "fl":
                    for i in range(NTQ):
                        for kt in range(KT):
                            em.op("pe", lambda e: e.matmul(pss[:, 0:8], lhsT=xT[:, kt, i * 128:(i + 1) * 128],
                                                           rhs=W[:, kt, 0:8], start=(kt == 0), stop=(kt == KT - 1)),
                                  reads=[r_W, r_xT], writes=[r_pss] if kt == 0 else [],
                                  pwrites=[] if kt == 0 else [r_pss], inc=(kt == KT - 1))
                        em.op("dve", lambda e: e.tensor_tensor(out=sm["z"][:], in0=pss[:, 0:8], in1=bfb[:], op=ALU.add),
                              reads=[r_pss, r_bfb], writes=[r_sm["z"]])
                        em.op("act", lambda e: e.activation(out=sm["e"][:], in_=sm["z"][:], func=AF.Exp, scale=-1.0),
                              reads=[r_sm["z"]], writes=[r_sm["e"]])
                        em.op("act", lambda e: e.activation(out=sm["z"][:], in_=sm["e"][:], func=AF.Ln, bias=1.0),
                              reads=[r_sm["e"]], writes=[r_sm["z"]])
                        em.op("dve", lambda e: e.tensor_scalar(out=logf[:, i, :], in0=sm["z"][:], scalar1=-1.0,
                                                               scalar2=None, op0=ALU.mult),
                              reads=[r_sm["z"]], pwrites=[r_logf])
                        for j in range(i + 1):
                            em.op("pe", lambda e: e.matmul(pss[:, 8:16], lhsT=(tri_f if j == i else ones_f)[:, :],
                                                           rhs=logf[:, j, :], start=(j == 0), stop=(j == i)),
                                  reads=[r_logf, r_const], writes=[r_pss] if j == 0 else [],
                                  pwrites=[] if j == 0 else [r_pss], inc=(j == i))
                        em.op("dve", lambda e: e.tensor_copy(out=sm["c"][:], in_=pss[:, 8:16]),
                              reads=[r_pss], writes=[r_sm["c"]])
                        Av = A[:].rearrange("p (s h r) -> p s h r", s=2, r=6)
                        em.op("dve", lambda e: e.tensor_copy(out=smb["hib"][:], in_=sm["c"][:]),
                              reads=[r_sm["c"]], writes=[r_sm["hib"]])
                        em.op("dve", lambda e: e.tensor_copy(out=sm["hif"][:], in_=smb["hib"][:]),
                              reads=[r_sm["hib"]], writes=[r_sm["hif"]])
                        em.op("dve", lambda e: e.tensor_tensor(out=sm["d1"][:], in0=sm["c"][:], in1=sm["hif"][:],
                                                               op=ALU.subtract),
                              reads=[r_sm["c"], r_sm["hif"]], writes=[r_sm["d1"]])
                        em.op("dve", lambda e: e.tensor_copy(out=smb["midb"][:], in_=sm["d1"][:]),
                              reads=[r_sm["d1"]], writes=[r_sm["midb"]])
                        em.op("dve", lambda e: e.tensor_copy(out=sm["midf"][:], in_=smb["midb"][:]),
                              reads=[r_sm["midb"]], writes=[r_sm["midf"]])
                        em.op("dve", lambda e: e.tensor_tensor(out=sm["d2"][:], in0=sm["d1"][:], in1=sm["midf"][:],
                                                               op=ALU.subtract),
                              reads=[r_sm["d1"], r_sm["midf"]], writes=[r_sm["d2"]])
                        em.op("dve", lambda e: e.tensor_copy(out=smb["lob"][:], in_=sm["d2"][:]),
                              reads=[r_sm["d2"]], writes=[r_sm["lob"]])
                        em.op("dve", lambda e: e.tensor_copy(out=sm["lof"][:], in_=smb["lob"][:]),
                              reads=[r_sm["lob"]], writes=[r_sm["lof"]])
                        for ri, nm in enumerate(("hif", "midf", "lof")):
                            em.op("dve", lambda e: e.tensor_copy(out=Av[:, 0, :, ri], in_=sm[nm][:]),
                                  reads=[r_sm[nm]], writes=[r_A] if ri == 0 else [], pwrites=[] if ri == 0 else [r_A])
                            em.op("dve", lambda e: e.tensor_scalar(out=Av[:, 1, :, 3 + ri], in0=sm[nm][:], scalar1=-1.0,
                                                                   scalar2=None, op0=ALU.mult),
                                  reads=[r_sm[nm]], pwrites=[r_A])
                        em.op("pe", lambda e: e.transpose(pst[0:96, 0:128], A[:, :], ident_f[:]),
                              reads=[r_A, r_const], writes=[r_pst])
                        em.op("act", lambda e: e.activation(out=augT[:, i * 128:(i + 1) * 128], in_=pst[0:96, 0:128],
                                                            func=AF.Copy), reads=[r_pst], pwrites=[r_augT])
                    em.dma("sp", aug_d[:, :], augT[:], reads=[r_augT], writes=[R["aug_d"]])
            em.barrier()

    def attn_alloc(ph, pfx):
        o = dict(
            sps=[ps(ph, f"{pfx}_s{i}") for i in range(3)], r_sps=[Res() for _ in range(3)],
            pexp=[sb(ph, f"{pfx}_p{i}", [128, 512], BF16) for i in range(3)], r_pexp=[Res() for _ in range(3)],
            ops=[ps(ph, f"{pfx}_o{i}") for i in range(2)], r_ops=[Res() for _ in range(2)],
            rden=sb(ph, f"{pfx}_rden", [128, 512], F32), r_rden=Res(),
            oT=[sb(ph, f"{pfx}_oT{i}", [128, 512], BF16) for i in range(2)], r_oT=[Res() for _ in range(2)],
            ctr={"o": 0, "s": 0})
        return o

    def attn_block(o, kT, r_kT, qT, r_qT, q0, Kc, prow, V, r_V, odd, n_ts, col_lo_fn, diag_fn, mask_mm_fn,
                   out_ap, r_out):
        ob = o["ctr"]["o"] % 2
        o["ctr"]["o"] += 1
        ops, r_ops = o["ops"][ob], o["r_ops"][ob]
        oT, r_oT = o["oT"][ob], o["r_oT"][ob]
        vc = 64 if odd else 0
        base = o["ctr"]["s"]
        o["ctr"]["s"] += n_ts
        has_mask = mask_mm_fn is not None

        def emit_S(i):
            lo = col_lo_fn(i)
            s = (base + i) % 3
            sps, r_sps = o["sps"][s], o["r_sps"][s]
            em.op("pe", lambda e: e.matmul(sps[:, lo:512], lhsT=kT[prow:prow + Kc, i * 128:(i + 1) * 128],
                                           rhs=qT[prow:prow + Kc, q0 + lo:q0 + 512], start=True, stop=not has_mask),
                  reads=[r_kT, r_qT], writes=[r_sps], inc=not has_mask)
            if has_mask:
                mask_mm_fn(i, lo, sps, r_sps)

        def emit_rest(i):
            lo = col_lo_fn(i)
            s = (base + i) % 3
            sps, r_sps, pexp, r_pexp = o["sps"][s], o["r_sps"][s], o["pexp"][s], o["r_pexp"][s]
            em.op("act", lambda e: e.activation(out=pexp[:, lo:512], in_=sps[:, lo:512], func=AF.Exp),
                  reads=[r_sps], writes=[r_pexp])
            dc = diag_fn(i) if diag_fn is not None else None
            if dc is not None:
                em.op("pool", lambda e: e.tensor_tensor(out=pexp[:, dc:dc + 128], in0=pexp[:, dc:dc + 128],
                                                        in1=tri_b[:, :], op=ALU.mult),
                      reads=[r_pexp, r_const], writes=[r_pexp])
            em.op("pe", lambda e: e.matmul(ops[:, lo:512], lhsT=V[:, i, vc:vc + 128], rhs=pexp[:, lo:512],
                                           start=(i == 0), stop=(i == n_ts - 1)),
                  reads=[r_V, r_pexp], writes=[r_ops] if i == 0 else [], pwrites=[] if i == 0 else [r_ops],
                  inc=(i == n_ts - 1))

        emit_S(0)
        for i in range(n_ts):
            if i + 1 < n_ts:
                emit_S(i + 1)
            emit_rest(i)
        (orow, drow) = (64, 0) if odd else (0, 64)
        em.op("dve", lambda e: e.reciprocal(out=o["rden"][orow:orow + 64, :], in_=ops[drow:drow + 64, :]),
              reads=[r_ops], writes=[o["r_rden"]])
        em.op("dve", lambda e: e.tensor_tensor(out=oT[orow:orow + 64, :], in0=ops[orow:orow + 64, :],
                                               in1=o["rden"][orow:orow + 64, :], op=ALU.mult),
              reads=[r_ops, o["r_rden"]], writes=[r_oT])
        em.dma("sp", out_ap, oT[orow:orow + 64, :], reads=[r_oT], pwrites=[r_out])

    def phase_fox():
        with ExitStack() as ph:
            o = attn_alloc(ph, "fx")
            qa = [sb(ph, f"fx_q{i}", [70, T], BF16) for i in range(2)]
            ka = [sb(ph, f"fx_k{i}", [70, T], BF16) for i in range(2)]
            r_qa = [Res() for _ in range(2)]
            r_ka = [Res() for _ in range(2)]
            V = sb(ph, "fx_V", [128, NTQ, 768], BF16)
            r_V = Res()
            em.dma("sp", V[:], fv_d.rearrange("(i p) c -> p i c", p=128), reads=[R["fv_d"]], writes=[r_V])

            def load_head(h):
                b = h % 2
                em.dma("sp", qa[b][0:64, :], fqT_d[h * 64:(h + 1) * 64, :], reads=[R["fqT_d"]], writes=[r_qa[b]])
                em.dma("sp", qa[b][64:70, :], aug_d[h * 6:h * 6 + 6, :], reads=[R["aug_d"]], pwrites=[r_qa[b]])
                em.dma("sp", ka[b][0:64, :], fkT_d[h * 64:(h + 1) * 64, :], reads=[R["fkT_d"]], writes=[r_ka[b]])
                em.dma("sp", ka[b][64:70, :], aug_d[48 + h * 6:48 + h * 6 + 6, :], reads=[R["aug_d"]],
                       pwrites=[r_ka[b]])

            load_head(0)
            for h in range(8):
                if h + 1 < 8:
                    load_head(h + 1)
                b = h % 2
                pair, odd = h // 2, h % 2
                Vp = V[:, :, pair * 192:(pair + 1) * 192]
                for tb in range(NTB):
                    attn_block(o, ka[b], r_ka[b], qa[b], r_qa[b], tb * 512, 70, 0, Vp, r_V, odd, 4 * (tb + 1),
                               lambda i: max(0, (i - 4 * tb) * 128),
                               lambda i: ((i - 4 * tb) * 128 if i >= 4 * tb else None),
                               None, obT_d[h * 64:(h + 1) * 64, tb * 512:(tb + 1) * 512], R["obT_d"])
            em.barrier()

    def phase_dsa():
        with ExitStack() as ph:
            o = attn_alloc(ph, "ds")
            iq = sb(ph, "ds_iq", [128, 4, T], BF16)
            ik2 = sb(ph, "ds_ik", [128, T], BF16)
            wabs = sb(ph, "ds_wabs", [128, NTQ, 8], F32)
            wsgn = sb(ph, "ds_wsgn", [128, NTQ, 8], F32)
            r_iq, r_ik, r_w = Res(), Res(), Res()
            sc = sb(ph, "ds_sc", [128, T], F32)
            junk = sb(ph, "ds_junk", [128, T], BF16)
            r_sc, r_junk = Res(), Res()
            mbs = [[sb(ph, f"ds_mb{s_}_{i}", [128, T], BF16) for i in range(4)] for s_ in range(2)]
            r_mbs = [[Res() for _ in range(4)] for _ in range(2)]
            rl = [sb(ph, f"ds_rl{i}", [128, 512], F32) for i in range(2)]
            r_rl = [Res() for _ in range(2)]
            scps = [ps(ph, f"ds_sp{i}") for i in range(2)]
            r_scps = [Res() for _ in range(2)]
            sm = {n: sb(ph, "ds_" + n, [128, 1], F32) for n in ("lo", "w0", "wk", "mid", "cnt", "gw", "mx", "thr0")}
            r_sm = {n: Res() for n in sm}
            Vp = [sb(ph, f"ds_V{i}", [128, NTQ, 192], BF16) for i in range(2)]
            r_Vp = [Res() for _ in range(2)]
            qh = [sb(ph, f"ds_q{i}", [128, 512], BF16) for i in range(2)]
            kh = [sb(ph, f"ds_k{i}", [128, T], BF16) for i in range(2)]
            r_qh = [Res() for _ in range(2)]
            r_kh = [Res() for _ in range(2)]

            em.dma("sp", iq[:], iqT_d.rearrange("(k p) t -> p k t", p=128), reads=[R["iqT_d"]], writes=[r_iq])
            em.dma("sp", ik2[0:64, :], ikT_d[:, :], reads=[R["ikT_d"]], pwrites=[r_ik])
            em.dma("sp", ik2[64:128, :], ikT_d[:, :], reads=[R["ikT_d"]], pwrites=[r_ik])
            em.dma("sp", wabs[:].rearrange("p a b -> p (a b)"), wabs_d[:, :], reads=[R["w_d"]], pwrites=[r_w])
            em.dma("sp", wsgn[:].rearrange("p a b -> p (a b)"), wsgn_d[:, :], reads=[R["w_d"]], pwrites=[r_w])
            em.op("dve", lambda e: e.memset(sm["thr0"][:], -1e29), writes=[r_sm["thr0"]])
            ctr = {"p": 0, "v": 0, "h": 0}
            dv_v = dv_d.rearrange("(i p) c -> p i c", p=128)

            def prep_q(tb, qi, mb, r_mb):
                g = tb * 4 + qi
                L2 = 128 * (g + 1)
                L1 = L2 - 64
                for kb in range((L2 + 511) // 512):
                    c0 = kb * 512
                    cw = min(512, L2 - c0)
                    for h in range(8):
                        pair, prow = h // 2, (h % 2) * 64
                        p = ctr["p"] % 2
                        ctr["p"] += 1
                        em.op("pe", lambda e: e.matmul(scps[p][:, 0:cw],
                                                       lhsT=iq[prow:prow + 64, pair, g * 128:(g + 1) * 128],
                                                       rhs=ik2[prow:prow + 64, c0:c0 + cw], start=True, stop=True),
                              reads=[r_iq, r_ik], writes=[r_scps[p]])
                        em.op("act", lambda e: e.activation(out=rl[p][:, 0:cw], in_=scps[p][:, 0:cw], func=AF.Relu,
                                                            scale=wabs[:, g, h:h + 1]),
                              reads=[r_scps[p], r_w], writes=[r_rl[p]])
                        if h == 0:
                            em.op("dve", lambda e: e.tensor_scalar(out=sc[:, c0:c0 + cw], in0=rl[p][:, 0:cw],
                                                                   scalar1=wsgn[:, g, 0:1], scalar2=None,
                                                                   op0=ALU.mult),
                                  reads=[r_rl[p], r_w], writes=[r_sc] if kb == 0 else [],
                                  pwrites=[] if kb == 0 else [r_sc])
                        else:
                            em.op("dve", lambda e: e.scalar_tensor_tensor(out=sc[:, c0:c0 + cw], in0=rl[p][:, 0:cw],
                                                                          scalar=wsgn[:, g, h:h + 1],
                                                                          in1=sc[:, c0:c0 + cw], op0=ALU.mult,
                                                                          op1=ALU.add),
                                  reads=[r_rl[p], r_w, r_sc], pwrites=[r_sc])
                em.op("dve", lambda e: e.memset(sc[0:64, L1:L2], -1e30), reads=[r_sc], pwrites=[r_sc])
                if L2 > TOPK:
                    em.op("dve", lambda e: e.tensor_reduce(out=sm["mx"][:], in_=sc[:, 0:L2], axis=AX.X, op=ALU.max),
                          reads=[r_sc], writes=[r_sm["mx"]])
                    em.op("dve", lambda e: e.tensor_reduce(out=sm["lo"][:], in_=sc[:, 0:L1], axis=AX.X, op=ALU.min),
                          reads=[r_sc], writes=[r_sm["lo"]])
                    em.op("dve", lambda e: e.tensor_tensor(out=sm["w0"][:], in0=sm["mx"][:], in1=sm["lo"][:],
                                                           op=ALU.subtract),
                          reads=[r_sm["mx"], r_sm["lo"]], writes=[r_sm["w0"]])
                    em.op("dve", lambda e: e.tensor_scalar(out=sm["w0"][:], in0=sm["w0"][:], scalar1=1.0001,
                                                           scalar2=1e-12, op0=ALU.mult, op1=ALU.add),
                          reads=[r_sm["w0"]], writes=[r_sm["w0"]])
                    for k in range(bisect_iters):
                        em.op("dve", lambda e: e.scalar_tensor_tensor(out=sm["mid"][:], in0=sm["w0"][:],
                                                                      scalar=2.0 ** -(k + 1), in1=sm["lo"][:],
                                                                      op0=ALU.mult, op1=ALU.add),
                              reads=[r_sm["w0"], r_sm["lo"]], writes=[r_sm["mid"]])
                        em.op("dve", lambda e: e.tensor_scalar(out=junk[:, 0:L2], in0=sc[:, 0:L2],
                                                               scalar1=sm["mid"][:, 0:1], scalar2=None,
                                                               op0=ALU.is_ge, op1=ALU.add,
                                                               accum_out=sm["cnt"][:, 0:1]),
                              reads=[r_sc, r_sm["mid"]], writes=[r_junk, r_sm["cnt"]])
                        em.op("dve", lambda e: e.scalar_tensor_tensor(out=sm["gw"][:], in0=sm["cnt"][:],
                                                                      scalar=float(TOPK), in1=sm["w0"][:],
                                                                      op0=ALU.is_ge, op1=ALU.mult),
                              reads=[r_sm["cnt"], r_sm["w0"]], writes=[r_sm["gw"]])
                        em.op("dve", lambda e: e.scalar_tensor_tensor(out=sm["lo"][:], in0=sm["gw"][:],
                                                                      scalar=2.0 ** -(k + 1), in1=sm["lo"][:],
                                                                      op0=ALU.mult, op1=ALU.add),
                              reads=[r_sm["gw"], r_sm["lo"]], writes=[r_sm["lo"]])
                    thr, r_thr = sm["lo"], r_sm["lo"]
                else:
                    thr, r_thr = sm["thr0"], r_sm["thr0"]
                em.op("dve", lambda e: e.tensor_scalar(out=mb[qi][:, 0:L2], in0=sc[:, 0:L2], scalar1=thr[:, 0:1],
                                                       scalar2=NEG, op0=ALU.is_lt, op1=ALU.mult),
                      reads=[r_sc, r_thr], writes=[r_mb[qi]])

            def attend_pair(tb, pair, mb, r_mb):
                n_ts = 4 * (tb + 1)


                def mask_mm(i, lo, sps, r_sps):
                    q_first = lo // 128
                    for qq in range(q_first, 4):
                        em.op("pe", lambda e: e.matmul(sps[:, qq * 128:(qq + 1) * 128],
                                                       lhsT=mb[qq][:, i * 128:(i + 1) * 128], rhs=ident_b[:, :],
                                                       start=False, stop=(qq == 3)),
                              reads=[r_mb[qq], r_const], pwrites=[r_sps], inc=(qq == 3))

                vb = ctr["v"] % 2
                ctr["v"] += 1
                em.dma("sp", Vp[vb][:, 0:n_ts, :], dv_v[:, 0:n_ts, pair * 192:(pair + 1) * 192],
                       reads=[R["dv_d"]], writes=[r_Vp[vb]])
                for odd in range(2):
                    h = pair * 2 + odd
                    prow = odd * 64
                    hb = ctr["h"] % 2
                    ctr["h"] += 1
                    em.dma("sp", qh[hb][prow:prow + 64, :], dqT_d[h * 64:(h + 1) * 64, tb * 512:(tb + 1) * 512],
                           reads=[R["dqT_d"]], writes=[r_qh[hb]])
                    em.dma("sp", kh[hb][prow:prow + 64, 0:n_ts * 128], dkT_d[h * 64:(h + 1) * 64, 0:n_ts * 128],
                           reads=[R["dkT_d"]], writes=[r_kh[hb]])
                    attn_block(o, kh[hb], r_kh[hb], qh[hb], r_qh[hb], 0, 64, prow, Vp[vb], r_Vp[vb], odd, n_ts,
                               lambda i: max(0, (i - 4 * tb) * 128), None, mask_mm,
                               oaT_d[h * 64:(h + 1) * 64, tb * 512:(tb + 1) * 512], R["oaT_d"])

            order = list(range(NTB - 1, -1, -1))
            for qi in range(4):
                prep_q(order[0], qi, mbs[0], r_mbs[0])
            for n_, tb in enumerate(order):
                for qi in range(4):
                    if n_ + 1 < NTB:
                        prep_q(order[n_ + 1], qi, mbs[(n_ + 1) % 2], r_mbs[(n_ + 1) % 2])
                    attend_pair(tb, qi, mbs[n_ % 2], r_mbs[n_ % 2])
            em.barrier()

    def ln_alloc(ph, pfx, g_d, b_d):
        o = dict(
            y=[sb(ph, f"{pfx}_y{i}", [128, D], F32) for i in range(2)], r_y=[Res() for _ in range(2)],
            xo=[sb(ph, f"{pfx}_xo{i}", [128, D], F32) for i in range(2)], r_xo=[Res() for _ in range(2)],
            xr=[sb(ph, f"{pfx}_xr{i}", [128, D], F32) for i in range(2)], r_xr=[Res() for _ in range(2)],
            st=sb(ph, f"{pfx}_st", [128, 6 * (D // 512)], F32), r_st=Res(),
            mv=sb(ph, f"{pfx}_mv", [128, 2], F32), r_mv=Res(),
            sd=sb(ph, f"{pfx}_sd", [128, 1], F32), r_sd=Res(),
            gb=sb(ph, f"{pfx}_gb", [128, D], F32), bb=sb(ph, f"{pfx}_bb", [128, D], F32), r_gb=Res(),
            xb=sb(ph, f"{pfx}_xb", [128, D], BF16), r_xb=Res(),
            tp=ps(ph, f"{pfx}_tp", [128, D], BF16), r_tp=Res(),
            stage=[sb(ph, f"{pfx}_sg{i}", [128, KT, 512], BF16) for i in range(2)], r_stage=[Res() for _ in range(2)],
            n=0)
        em.dma("sp", o["gb"][:], g_d.partition_broadcast(128), pwrites=[o["r_gb"]])
        em.dma("sp", o["bb"][:], b_d.partition_broadcast(128), pwrites=[o["r_gb"]])
        return o

    def ln_stage(o, i, halves, xres_d, store_d, r_store, xT_dst, r_xT_dst, after_fn=None):
        b = o["n"] % 2
        o["n"] += 1
        y, r_y, xo, r_xo, xr, r_xr = o["y"][b], o["r_y"][b], o["xo"][b], o["r_xo"][b], o["xr"][b], o["r_xr"][b]
        em.dma("sp", xr[:], xres_d[i * 128:(i + 1) * 128, :], reads=[R["x1_d"], R["xcur_d"]], writes=[r_xr])
        for hf, (pa, r_pa) in enumerate(halves):
            sl = slice(hf * 512, (hf + 1) * 512)
            em.op("dve", lambda e: e.scalar_tensor_tensor(out=y[:, sl], in0=xr[:, sl], scalar=alpha, in1=pa,
                                                          op0=ALU.mult, op1=ALU.add),
                  reads=[r_xr, r_pa], writes=[r_y] if hf == 0 else [], pwrites=[] if hf == 0 else [r_y])
        for c in range(D // 512):
            em.op("dve", lambda e: e.bn_stats(out=o["st"][:, c * 6:(c + 1) * 6], in_=y[:, c * 512:(c + 1) * 512]),
                  reads=[r_y], writes=[o["r_st"]] if c == 0 else [], pwrites=[] if c == 0 else [o["r_st"]])
        em.op("dve", lambda e: e.bn_aggr(out=o["mv"][:, 0:2], in_=o["st"][:, :]), reads=[o["r_st"]], writes=[o["r_mv"]])
        em.op("dve", lambda e: e.tensor_scalar(out=o["sd"][:], in0=o["mv"][:, 1:2], scalar1=LN_EPS, scalar2=None,
                                               op0=ALU.add), reads=[o["r_mv"]], writes=[o["r_sd"]])
        em.op("act", lambda e: e.activation(out=o["sd"][:], in_=o["sd"][:], func=AF.Sqrt),
              reads=[o["r_sd"]], writes=[o["r_sd"]])
        em.op("dve", lambda e: e.reciprocal(out=o["sd"][:], in_=o["sd"][:]), reads=[o["r_sd"]], writes=[o["r_sd"]])
        em.op("dve", lambda e: e.tensor_scalar(out=y[:], in0=y[:], scalar1=o["mv"][:, 0:1], scalar2=o["sd"][:, 0:1],
                                               op0=ALU.subtract, op1=ALU.mult),
              reads=[r_y, o["r_mv"], o["r_sd"]], writes=[r_y])
        em.op("pool", lambda e: e.tensor_tensor(out=xo[:], in0=y[:], in1=o["gb"][:], op=ALU.mult),
              reads=[r_y, o["r_gb"]], writes=[r_xo])
        em.op("pool", lambda e: e.tensor_tensor(out=xo[:], in0=xo[:], in1=o["bb"][:], op=ALU.add),
              reads=[r_xo, o["r_gb"]], writes=[r_xo])
        em.dma("sp", store_d[i * 128:(i + 1) * 128, :], xo[:], reads=[r_xo], pwrites=[r_store])
        if xT_dst is not None:
            tbk, qi = i // 4, i % 4
            stg, r_stg = o["stage"][tbk % 2], o["r_stage"][tbk % 2]
            emit_transposes((o["xb"], o["r_xb"], o["tp"], o["r_tp"], stg, r_stg), xo, r_xo, qi)
            if qi == 3:
                em.dma("sp", xT_dst.rearrange("(k p) t -> p k t", p=128)[:, :, tbk * 512:(tbk + 1) * 512], stg[:],
                       reads=[r_stg], pwrites=[r_xT_dst])
        if after_fn is not None:
            after_fn(i, xo, r_xo, o["xb"], o["r_xb"])

    def phase_merge(l, moe_j):
        with ExitStack() as ph:
            lo_ = ln_alloc(ph, "p4", ln_mix_g[l], ln_mix_b[l])
            Wa = sb(ph, "p4_Wa", [128, 4, D], BF16)
            Wb = sb(ph, "p4_Wb", [128, 4, D], BF16)
            Wo = sb(ph, "p4_Wo", [128, KT, D], BF16)
            r_W = Res()
            oa = [sb(ph, f"p4_oa{i}", [128, 4, 512], BF16) for i in range(2)]
            ob = [sb(ph, f"p4_ob{i}", [128, 4, 512], BF16) for i in range(2)]
            sga = [sb(ph, f"p4_sga{i}", [128, KT, 512], BF16) for i in range(2)]
            sgb = [sb(ph, f"p4_sgb{i}", [128, KT, 512], BF16) for i in range(2)]
            r_in = [Res() for _ in range(2)]
            mg = sb(ph, "p4_mg", [128, KT, 512], BF16)
            r_mg = Res()
            t1 = sb(ph, "p4_t1", [128, 512], F32)
            t2 = sb(ph, "p4_t2", [128, 512], F32)
            r_t1, r_t2 = Res(), Res()
            psa = ps(ph, "p4_psa")
            psb = ps(ph, "p4_psb")
            r_psa, r_psb = Res(), Res()
            nbm = 1 if moe_j is not None else 2
            psm = [ps(ph, f"p4_psm{i}") for i in range(nbm * (D // 512))]
            r_psm = [Res() for _ in range(nbm * (D // 512))]
            em.dma("pool", Wa[:], w_ba[l].rearrange("(k p) d -> p k d", p=128), pwrites=[r_W])
            em.dma("pool", Wb[:], w_bb[l].rearrange("(k p) d -> p k d", p=128), pwrites=[r_W])
            for kt in range(KT):
                em.dma("pool", Wo[:, kt, :], w_out[l][kt * 128:(kt + 1) * 128, :], pwrites=[r_W])
            after = None
            if moe_j is not None:
                rt = sb(ph, "p4_rt", [128, KT, NEXP], F32)
                r_rt = Res()
                em.dma("sp", rt[:], moe_router[moe_j].rearrange("(k p) e -> p k e", p=128), writes=[r_rt])
                tpf = ps(ph, "p4_tpf", [128, D], F32)
                r_tpf = Res()
                xTf = sb(ph, "p4_xTf", [128, D], F32)
                r_xTf = Res()
                pcs = ps(ph, "p4_pcs")
                r_pcs = Res()
                zt = sb(ph, "p4_zt", [128, 2 * D], BF16)
                r_zt = Res()
                r_zf = Res()
                em.op("pool", lambda e: e.memset(zt[:], 0.0), writes=[r_zt])
                for r0 in range(0, NEXP * CAP, 256):
                    em.dma("sp", xe_d[r0:r0 + 256, :].rearrange("(p a) d -> p (a d)", a=2), zt[:], reads=[r_zt],
                           pwrites=[R["xe_d"], r_zf])
                mall = sb(ph, "p4_mall", [128, NTQ, NEXP], F32)
                r_mall = Res()
                gi = sb(ph, "p4_gi", [128, NTQ, 2], F32)
                ii = sb(ph, "p4_ii", [128, NTQ, 2], I32)
                r_gi, r_ii = Res(), Res()
                eoff_i = sb(ph, "p4_eoffi", [128, NEXP], I32)
                eoff = sb(ph, "p4_eoff", [128, NEXP], F32)
                r_eoff = Res()
                em.op("pool", lambda e: e.iota(eoff_i[:], pattern=[[CAP, NEXP]], base=0, channel_multiplier=0),
                      writes=[r_eoff])
                em.op("dve", lambda e: e.tensor_copy(out=eoff[:], in_=eoff_i[:]), reads=[r_eoff], writes=[r_eoff])
                s8 = {n: sb(ph, "p4_" + n, [128, NEXP], F32) for n in ("lg", "is1", "l2", "is2", "pos", "v", "ov", "tmp")}
                s1 = {n: sb(ph, "p4_" + n, [128, 1], F32) for n in ("m1", "m2", "d")}
                r_s = {n: Res() for n in list(s8) + list(s1)}

                def after(i, xo, r_xo, xb, r_xb):
                    for kt in range(KT):
                        em.op("pe", lambda e: e.transpose(tpf[:, kt * 128:(kt + 1) * 128],
                                                          xo[:, kt * 128:(kt + 1) * 128], ident_f[:]),
                              reads=[r_xo, r_const], writes=[r_tpf] if kt == 0 else [],
                              pwrites=[] if kt == 0 else [r_tpf], inc=(kt == KT - 1))
                    em.op("act", lambda e: e.activation(out=xTf[:], in_=tpf[:], func=AF.Copy),
                          reads=[r_tpf], writes=[r_xTf])
                    for kt in range(KT):
                        em.op("pe", lambda e: e.matmul(pcs[:, 0:NEXP], lhsT=xTf[:, kt * 128:(kt + 1) * 128],
                                                       rhs=rt[:, kt, :], start=(kt == 0), stop=(kt == KT - 1)),
                              reads=[r_xTf, r_rt], writes=[r_pcs] if kt == 0 else [],
                              pwrites=[] if kt == 0 else [r_pcs], inc=(kt == KT - 1))
                    em.op("dve", lambda e: e.tensor_copy(out=s8["lg"][:], in_=pcs[:, 0:NEXP]),
                          reads=[r_pcs], writes=[r_s["lg"]])
                    em.op("dve", lambda e: e.tensor_reduce(out=s1["m1"][:], in_=s8["lg"][:], axis=AX.X, op=ALU.max),
                          reads=[r_s["lg"]], writes=[r_s["m1"]])
                    em.op("dve", lambda e: e.tensor_scalar(out=s8["is1"][:], in0=s8["lg"][:], scalar1=s1["m1"][:, 0:1],
                                                           scalar2=None, op0=ALU.is_equal),
                          reads=[r_s["lg"], r_s["m1"]], writes=[r_s["is1"]])
                    em.op("dve", lambda e: e.scalar_tensor_tensor(out=s8["l2"][:], in0=s8["is1"][:], scalar=-1e30,
                                                                  in1=s8["lg"][:], op0=ALU.mult, op1=ALU.add),
                          reads=[r_s["is1"], r_s["lg"]], writes=[r_s["l2"]])
                    em.op("dve", lambda e: e.tensor_reduce(out=s1["m2"][:], in_=s8["l2"][:], axis=AX.X, op=ALU.max),
                          reads=[r_s["l2"]], writes=[r_s["m2"]])
                    em.op("dve", lambda e: e.tensor_scalar(out=s8["is2"][:], in0=s8["l2"][:], scalar1=s1["m2"][:, 0:1],
                                                           scalar2=None, op0=ALU.is_equal),
                          reads=[r_s["l2"], r_s["m2"]], writes=[r_s["is2"]])
                    em.op("dve", lambda e: e.tensor_tensor(out=s1["d"][:], in0=s1["m2"][:], in1=s1["m1"][:],
                                                           op=ALU.subtract),
                          reads=[r_s["m1"], r_s["m2"]], writes=[r_s["d"]])
                    em.op("act", lambda e: e.activation(out=gi[:, i, 1:2], in_=s1["d"][:], func=AF.Sigmoid),
                          reads=[r_s["d"]], pwrites=[r_gi])
                    em.op("dve", lambda e: e.tensor_scalar(out=gi[:, i, 0:1], in0=gi[:, i, 1:2], scalar1=-1.0, scalar2=1.0,
                                                           op0=ALU.mult, op1=ALU.add),
                          reads=[r_gi], pwrites=[r_gi])
                    em.op("dve", lambda e: e.tensor_tensor(out=mall[:, i, :], in0=s8["is1"][:], in1=s8["is2"][:],
                                                           op=ALU.add),
                          reads=[r_s["is1"], r_s["is2"]], pwrites=[r_mall])
                    for j in range(i + 1):
                        em.op("pe", lambda e: e.matmul(pcs[:, 8:8 + NEXP], lhsT=(tri_f if j == i else ones_f)[:, :],
                                                       rhs=mall[:, j, :], start=(j == 0), stop=(j == i)),
                              reads=[r_mall, r_const], writes=[r_pcs] if j == 0 else [],
                              pwrites=[] if j == 0 else [r_pcs], inc=(j == i))
                    em.op("dve", lambda e: e.tensor_scalar(out=s8["pos"][:], in0=pcs[:, 8:8 + NEXP], scalar1=-1.0,
                                                           scalar2=None, op0=ALU.add),
                          reads=[r_pcs], writes=[r_s["pos"]])
                    em.op("dve", lambda e: e.tensor_scalar(out=s8["ov"][:], in0=s8["pos"][:], scalar1=float(CAP),
                                                           scalar2=1e6, op0=ALU.is_ge, op1=ALU.mult),
                          reads=[r_s["pos"]], writes=[r_s["ov"]])
                    em.op("dve", lambda e: e.tensor_tensor(out=s8["v"][:], in0=s8["pos"][:], in1=eoff[:], op=ALU.add),
                          reads=[r_s["pos"], r_eoff], writes=[r_s["v"]])
                    em.op("dve", lambda e: e.tensor_tensor(out=s8["v"][:], in0=s8["v"][:], in1=s8["ov"][:], op=ALU.add),
                          reads=[r_s["v"], r_s["ov"]], writes=[r_s["v"]])
                    for w_, nm in enumerate(("is1", "is2")):
                        em.op("dve", lambda e: e.tensor_tensor(out=s8["tmp"][:], in0=s8[nm][:], in1=s8["v"][:],
                                                               op=ALU.mult),
                              reads=[r_s[nm], r_s["v"]], writes=[r_s["tmp"]])
                        em.op("dve", lambda e: e.tensor_reduce(out=s1["m1"][:], in_=s8["tmp"][:], axis=AX.X, op=ALU.add),
                              reads=[r_s["tmp"]], writes=[r_s["m1"]])
                        em.op("dve", lambda e: e.tensor_copy(out=ii[:, i, w_:w_ + 1], in_=s1["m1"][:]),
                              reads=[r_s["m1"]], pwrites=[r_ii])
                        em.dma_fn("pool", lambda g: g.indirect_dma_start(
                            out=xe_d[:, :], out_offset=bass.IndirectOffsetOnAxis(ap=ii[:, i, w_:w_ + 1], axis=0),
                            in_=xb[:, :], in_offset=None, bounds_check=bc_reg[0], oob_is_err=False),
                            reads=[r_ii, r_xb, r_zf], pwrites=[R["xe_d"]])

            xres_d = x_in if l == 0 else xcur_d

            def load_in(tb):
                b = tb % 2
                tsl = slice(tb * 512, (tb + 1) * 512)
                em.dma("sp", oa[b][:], oaT_d.rearrange("(k p) t -> p k t", p=128)[:, :, tsl], reads=[R["oaT_d"]],
                       writes=[r_in[b]])
                em.dma("sp", ob[b][:], obT_d.rearrange("(k p) t -> p k t", p=128)[:, :, tsl], reads=[R["obT_d"]],
                       pwrites=[r_in[b]])
                em.dma("sp", sga[b][:], sgaT_d.rearrange("(k p) t -> p k t", p=128)[:, :, tsl], reads=[R["sgaT_d"]],
                       pwrites=[r_in[b]])
                em.dma("sp", sgb[b][:], sgbT_d.rearrange("(k p) t -> p k t", p=128)[:, :, tsl], reads=[R["sgbT_d"]],
                       pwrites=[r_in[b]])

            load_in(0)
            for tb in range(NTB):
                if tb + 1 < NTB:
                    load_in(tb + 1)
                b = tb % 2
                for dm in range(KT):
                    for k in range(4):
                        em.op("pe", lambda e: e.matmul(psa[:, :], lhsT=Wa[:, k, dm * 128:(dm + 1) * 128],
                                                       rhs=oa[b][:, k, :], start=(k == 0), stop=(k == 3)),
                              reads=[r_W, r_in[b]], writes=[r_psa] if k == 0 else [], pwrites=[] if k == 0 else [r_psa],
                              inc=(k == 3))
                    for k in range(4):
                        em.op("pe", lambda e: e.matmul(psb[:, :], lhsT=Wb[:, k, dm * 128:(dm + 1) * 128],
                                                       rhs=ob[b][:, k, :], start=(k == 0), stop=(k == 3)),
                              reads=[r_W, r_in[b]], writes=[r_psb] if k == 0 else [], pwrites=[] if k == 0 else [r_psb],
                              inc=(k == 3))
                    em.op("dve", lambda e: e.tensor_tensor(out=t1[:], in0=psa[:, :], in1=sga[b][:, dm, :], op=ALU.mult),
                          reads=[r_psa, r_in[b]], writes=[r_t1])
                    em.op("dve", lambda e: e.tensor_tensor(out=t2[:], in0=psb[:, :], in1=sgb[b][:, dm, :], op=ALU.mult),
                          reads=[r_psb, r_in[b]], writes=[r_t2])
                    em.op("pool", lambda e: e.tensor_tensor(out=mg[:, dm, :], in0=t1[:], in1=t2[:], op=ALU.add),
                          reads=[r_t1, r_t2], writes=[r_mg] if dm == 0 else [], pwrites=[] if dm == 0 else [r_mg])
                for qi in range(4):
                    i = tb * 4 + qi
                    halves = []
                    for hf in range(D // 512):
                        pi = (i % nbm) * (D // 512) + hf
                        for k in range(KT):
                            em.op("pe", lambda e: e.matmul(psm[pi][:, :], lhsT=mg[:, k, qi * 128:(qi + 1) * 128],
                                                           rhs=Wo[:, k, hf * 512:(hf + 1) * 512], start=(k == 0),
                                                           stop=(k == KT - 1)),
                                  reads=[r_mg, r_W], writes=[r_psm[pi]] if k == 0 else [],
                                  pwrites=[] if k == 0 else [r_psm[pi]], inc=(k == KT - 1))
                        halves.append((psm[pi][:, :], r_psm[pi]))
                    ln_stage(lo_, i, halves, xres_d, x1_d, R["x1_d"], x1T_d, R["x1T_d"], after)
            if moe_j is not None:
                em.dma("sp", moe_g_d[:, :], gi[:].rearrange("p a b -> p (a b)"), reads=[r_gi], pwrites=[R["moe_d"]])
                em.dma("sp", moe_i_d[:, :], ii[:].rearrange("p a b -> p (a b)"), reads=[r_ii], pwrites=[R["moe_d"]])
            em.barrier()

    def phase_ffn(l, experts, gated, final):
        with ExitStack() as ph:
            lo_ = ln_alloc(ph, "p5", ln_ffn_g[l], ln_ffn_b[l])
            NQ = TBF // 128
            x1T = sb(ph, "p5_xT", [128, KT, TBF], BF16)
            r_x1T = Res()
            yacc = sb(ph, "p5_yacc", [128, NQ, D], F32)
            r_yacc = Res()
            hT = [sb(ph, f"p5_hT{i}", [128, 4, TBF], BF16) for i in range(2)]
            r_hT = [Res() for _ in range(2)]
            Wg = [sb(ph, f"p5_Wg{i}", [128, KT, 512], BF16) for i in range(2)]
            Wu = [sb(ph, f"p5_Wu{i}", [128, KT, 512], BF16) for i in range(2)]
            Wd = [sb(ph, f"p5_Wd{i}", [128, 4, D], BF16) for i in range(2)]
            r_Wc = [Res() for _ in range(2)]
            gbc = sb(ph, "p5_gbc", [128, TBF], F32)
            r_gbc = Res()
            sg = [sb(ph, f"p5_sg{i}", [128, 512], F32) for i in range(2)]
            r_sg = [Res() for _ in range(2)]
            tm = [sb(ph, f"p5_tm{i}", [128, 512], F32) for i in range(2)]
            r_tm = [Res() for _ in range(2)]
            psg = [ps(ph, f"p5_pg{i}") for i in range(2)]
            psu = [ps(ph, f"p5_pu{i}") for i in range(2)]
            psd = [ps(ph, f"p5_pd{i}") for i in range(2)]
            r_psg = [Res() for _ in range(2)]
            r_psu = [Res() for _ in range(2)]
            r_psd = [Res() for _ in range(2)]
            NCH = FT // 4
            work = [(tbf, ei, c) for tbf in range(NTBF) for ei in range(len(experts)) for c in range(NCH)]
            ctr = {"p": 0, "d": 0}

            def load_chunk(n):
                tbf, ei, c = work[n]
                wg_d, wu_d, wd_d = experts[ei]
                b = n % 2
                em.dma("pool", Wg[b][:], wg_d[:, c * 512:(c + 1) * 512].rearrange("(k p) f -> p k f", p=128),
                       writes=[r_Wc[b]])
                em.dma("pool", Wu[b][:], wu_d[:, c * 512:(c + 1) * 512].rearrange("(k p) f -> p k f", p=128),
                       pwrites=[r_Wc[b]])
                em.dma("pool", Wd[b][:], wd_d[c * 512:(c + 1) * 512, :].rearrange("(k p) d -> p k d", p=128),
                       pwrites=[r_Wc[b]])

            load_chunk(0)
            for n, (tbf, ei, c) in enumerate(work):
                if n + 1 < len(work):
                    load_chunk(n + 1)
                b = n % 2
                t0 = tbf * TBF
                if ei == 0 and c == 0:
                    em.dma("sp", x1T[:], x1T_d.rearrange("(k p) t -> p k t", p=128)[:, :, t0:t0 + TBF],
                           reads=[R["x1T_d"]], writes=[r_x1T])
                if gated and c == 0:
                    em.dma("sp", gbc[:], gateT_d[ei, t0:t0 + TBF].partition_broadcast(128), reads=[R["gateT_d"]],
                           writes=[r_gbc])
                for f4 in range(4):
                    for tsub in range(TBF // 512):
                        p = ctr["p"] % 2
                        ctr["p"] += 1
                        tsl = slice(tsub * 512, (tsub + 1) * 512)
                        for kt in range(KT):
                            em.op("pe", lambda e: e.matmul(psg[p][:, :], lhsT=Wg[b][:, kt, f4 * 128:(f4 + 1) * 128],
                                                           rhs=x1T[:, kt, tsl], start=(kt == 0), stop=(kt == KT - 1)),
                                  reads=[r_Wc[b], r_x1T], writes=[r_psg[p]] if kt == 0 else [],
                                  pwrites=[] if kt == 0 else [r_psg[p]], inc=(kt == KT - 1))
                        for kt in range(KT):
                            em.op("pe", lambda e: e.matmul(psu[p][:, :], lhsT=Wu[b][:, kt, f4 * 128:(f4 + 1) * 128],
                                                           rhs=x1T[:, kt, tsl], start=(kt == 0), stop=(kt == KT - 1)),
                                  reads=[r_Wc[b], r_x1T], writes=[r_psu[p]] if kt == 0 else [],
                                  pwrites=[] if kt == 0 else [r_psu[p]], inc=(kt == KT - 1))
                        em.op("act", lambda e: e.activation(out=sg[p][:], in_=psg[p][:, :], func=AF.Silu),
                              reads=[r_psg[p]], writes=[r_sg[p]])
                        first_h = (f4 == 0 and tsub == 0)
                        if gated:
                            em.op("dve", lambda e: e.tensor_tensor(out=tm[p][:], in0=psu[p][:, :], in1=sg[p][:],
                                                                   op=ALU.mult),
                                  reads=[r_psu[p], r_sg[p]], writes=[r_tm[p]])
                            em.op("pool", lambda e: e.tensor_tensor(out=hT[b][:, f4, tsl], in0=tm[p][:],
                                                                    in1=gbc[:, tsl], op=ALU.mult),
                                  reads=[r_tm[p], r_gbc], writes=[r_hT[b]] if first_h else [],
                                  pwrites=[] if first_h else [r_hT[b]])
                        else:
                            em.op("dve", lambda e: e.tensor_tensor(out=hT[b][:, f4, tsl], in0=psu[p][:, :], in1=sg[p][:],
                                                                   op=ALU.mult),
                                  reads=[r_psu[p], r_sg[p]], writes=[r_hT[b]] if first_h else [],
                                  pwrites=[] if first_h else [r_hT[b]])
                first_acc = (ei == 0 and c == 0)
                for q in range(NQ):
                    for hf in range(D // 512):
                        d = ctr["d"] % 2
                        ctr["d"] += 1
                        for f4 in range(4):
                            em.op("pe", lambda e: e.matmul(psd[d][:, :], lhsT=hT[b][:, f4, q * 128:(q + 1) * 128],
                                                           rhs=Wd[b][:, f4, hf * 512:(hf + 1) * 512], start=(f4 == 0),
                                                           stop=(f4 == 3)),
                                  reads=[r_hT[b], r_Wc[b]], writes=[r_psd[d]] if f4 == 0 else [],
                                  pwrites=[] if f4 == 0 else [r_psd[d]], inc=(f4 == 3))
                        ysl = yacc[:, q, hf * 512:(hf + 1) * 512]
                        if first_acc:
                            em.op("dve", lambda e: e.tensor_copy(out=ysl, in_=psd[d][:, :]),
                                  reads=[r_psd[d]], writes=[r_yacc] if (q == 0 and hf == 0) else [],
                                  pwrites=[] if (q == 0 and hf == 0) else [r_yacc])
                        else:
                            em.op("dve", lambda e: e.tensor_tensor(out=ysl, in0=ysl, in1=psd[d][:, :], op=ALU.add),
                                  reads=[r_psd[d], r_yacc], pwrites=[r_yacc])
                if ei == len(experts) - 1 and c == NCH - 1:
                    for q in range(NQ):
                        i = tbf * NQ + q
                        halves = [(yacc[:, q, hf * 512:(hf + 1) * 512], r_yacc) for hf in range(D // 512)]
                        if final:
                            ln_stage(lo_, i, halves, x1_d, out_d, R["out_d"], None, None)
                        else:
                            ln_stage(lo_, i, halves, x1_d, xcur_d, R["xcur_d"], xT_d, R["xT_d"])
            em.barrier()

    def phase_moe(l, j, final):
        NQ = CAP // 128
        NSUB = CAP // 512
        NCH = FT // 4
        with ExitStack() as ph:
            xeT = sb(ph, "pm_xeT", [128, KT, CAP], BF16)
            r_xeT = Res()
            yacc = sb(ph, "pm_yacc", [128, NQ, D], F32)
            r_yacc = Res()
            hT = [sb(ph, f"pm_hT{i}", [128, 4, CAP], BF16) for i in range(2)]
            r_hT = [Res() for _ in range(2)]
            Wg = [sb(ph, f"pm_Wg{i}", [128, KT, 512], BF16) for i in range(2)]
            Wu = [sb(ph, f"pm_Wu{i}", [128, KT, 512], BF16) for i in range(2)]
            Wd = [sb(ph, f"pm_Wd{i}", [128, 4, D], BF16) for i in range(2)]
            r_Wc = [Res() for _ in range(2)]
            sg = [sb(ph, f"pm_sg{i}", [128, 512], F32) for i in range(2)]
            r_sg = [Res() for _ in range(2)]
            xr = [sb(ph, f"pm_xr{i}", [128, D], BF16) for i in range(2)]
            r_xr = [Res() for _ in range(2)]
            tp = ps(ph, "pm_tp", [128, D], BF16)
            r_tp = Res()
            psg = [ps(ph, f"pm_pg{i}") for i in range(2)]
            psu = [ps(ph, f"pm_pu{i}") for i in range(2)]
            psd = [ps(ph, f"pm_pd{i}") for i in range(2)]
            r_psg = [Res() for _ in range(2)]
            r_psu = [Res() for _ in range(2)]
            r_psd = [Res() for _ in range(2)]
            work = [(e, c) for e in range(NEXP) for c in range(NCH)]
            ctr = {"p": 0, "d": 0, "x": 0}

            def load_chunk(n):
                e, c = work[n]
                b = n % 2
                em.dma("pool", Wg[b][:], moe_wg[j][e][:, c * 512:(c + 1) * 512].rearrange("(k p) f -> p k f", p=128),
                       writes=[r_Wc[b]])
                em.dma("pool", Wu[b][:], moe_wu[j][e][:, c * 512:(c + 1) * 512].rearrange("(k p) f -> p k f", p=128),
                       pwrites=[r_Wc[b]])
                em.dma("pool", Wd[b][:], moe_wd[j][e][c * 512:(c + 1) * 512, :].rearrange("(k p) d -> p k d", p=128),
                       pwrites=[r_Wc[b]])

            load_chunk(0)
            for n, (e_, c) in enumerate(work):
                if n + 1 < len(work):
                    load_chunk(n + 1)
                b = n % 2
                if c == 0:
                    for q in range(NQ):
                        xb_ = ctr["x"] % 2
                        ctr["x"] += 1
                        r0 = e_ * CAP + q * 128
                        em.dma("sp", xr[xb_][:], xe_d[r0:r0 + 128, :], reads=[R["xe_d"]], writes=[r_xr[xb_]])
                        for kt in range(KT):
                            em.op("pe", lambda e: e.transpose(tp[:, kt * 128:(kt + 1) * 128],
                                                              xr[xb_][:, kt * 128:(kt + 1) * 128], ident_b[:]),
                                  reads=[r_xr[xb_], r_const], writes=[r_tp] if kt == 0 else [],
                                  pwrites=[] if kt == 0 else [r_tp], inc=(kt == KT - 1))
                        em.op("dve", lambda e: e.tensor_copy(out=xeT[:, :, q * 128:(q + 1) * 128],
                                                             in_=tp[:].rearrange("p (k c) -> p k c", c=128)),
                              reads=[r_tp], writes=[r_xeT] if q == 0 else [], pwrites=[] if q == 0 else [r_xeT])
                for f4 in range(4):
                    for tsub in range(NSUB):
                        p = ctr["p"] % 2
                        ctr["p"] += 1
                        tsl = slice(tsub * 512, (tsub + 1) * 512)
                        for kt in range(KT):
                            em.op("pe", lambda e: e.matmul(psg[p][:, :], lhsT=Wg[b][:, kt, f4 * 128:(f4 + 1) * 128],
                                                           rhs=xeT[:, kt, tsl], start=(kt == 0), stop=(kt == KT - 1)),
                                  reads=[r_Wc[b], r_xeT], writes=[r_psg[p]] if kt == 0 else [],
                                  pwrites=[] if kt == 0 else [r_psg[p]], inc=(kt == KT - 1))
                        for kt in range(KT):
                            em.op("pe", lambda e: e.matmul(psu[p][:, :], lhsT=Wu[b][:, kt, f4 * 128:(f4 + 1) * 128],
                                                           rhs=xeT[:, kt, tsl], start=(kt == 0), stop=(kt == KT - 1)),
                                  reads=[r_Wc[b], r_xeT], writes=[r_psu[p]] if kt == 0 else [],
                                  pwrites=[] if kt == 0 else [r_psu[p]], inc=(kt == KT - 1))
                        em.op("act", lambda e: e.activation(out=sg[p][:], in_=psg[p][:, :], func=AF.Silu),
                              reads=[r_psg[p]], writes=[r_sg[p]])
                        first_h = (f4 == 0 and tsub == 0)
                        em.op("dve", lambda e: e.tensor_tensor(out=hT[b][:, f4, tsl], in0=psu[p][:, :], in1=sg[p][:],
                                                               op=ALU.mult),
                              reads=[r_psu[p], r_sg[p]], writes=[r_hT[b]] if first_h else [],
                              pwrites=[] if first_h else [r_hT[b]])
                for q in range(NQ):
                    for hf in range(D // 512):
                        d = ctr["d"] % 2
                        ctr["d"] += 1
                        for f4 in range(4):
                            em.op("pe", lambda e: e.matmul(psd[d][:, :], lhsT=hT[b][:, f4, q * 128:(q + 1) * 128],
                                                           rhs=Wd[b][:, f4, hf * 512:(hf + 1) * 512], start=(f4 == 0),
                                                           stop=(f4 == 3)),
                                  reads=[r_hT[b], r_Wc[b]], writes=[r_psd[d]] if f4 == 0 else [],
                                  pwrites=[] if f4 == 0 else [r_psd[d]], inc=(f4 == 3))
                        ysl = yacc[:, q, hf * 512:(hf + 1) * 512]
                        if c == 0:
                            em.op("act", lambda e: e.activation(out=ysl, in_=psd[d][:, :], func=AF.Copy),
                                  reads=[r_psd[d]], writes=[r_yacc] if (q == 0 and hf == 0) else [],
                                  pwrites=[] if (q == 0 and hf == 0) else [r_yacc])
                        else:
                            em.op("dve", lambda e: e.tensor_tensor(out=ysl, in0=ysl, in1=psd[d][:, :], op=ALU.add),
                                  reads=[r_psd[d], r_yacc], pwrites=[r_yacc])
                if c == NCH - 1:
                    em.dma("sp", ye_d[e_ * CAP:(e_ + 1) * CAP, :].rearrange("(q p) d -> p q d", p=128), yacc[:],
                           reads=[r_yacc], pwrites=[R["ye_d"]])
            em.barrier()
        with ExitStack() as ph:
            lo_ = ln_alloc(ph, "pc", ln_ffn_g[l], ln_ffn_b[l])
            gi = sb(ph, "pc_gi", [128, NTQ, 2], F32)
            ii = sb(ph, "pc_ii", [128, NTQ, 2], I32)
            r_gi = Res()
            em.dma("sp", gi[:].rearrange("p a b -> p (a b)"), moe_g_d[:, :], reads=[R["moe_d"]], pwrites=[r_gi])
            em.dma("sp", ii[:].rearrange("p a b -> p (a b)"), moe_i_d[:, :], reads=[R["moe_d"]], pwrites=[r_gi])
            y1 = [sb(ph, f"pc_y1{i}", [128, D], F32) for i in range(2)]
            y2 = [sb(ph, f"pc_y2{i}", [128, D], F32) for i in range(2)]
            r_y1 = [Res() for _ in range(2)]
            r_y2 = [Res() for _ in range(2)]
            for i in range(2):
                em.op("pool", lambda e: e.memset(y1[i][:], 0.0), writes=[r_y1[i]])
                em.op("pool", lambda e: e.memset(y2[i][:], 0.0), writes=[r_y2[i]])
            def gather(i):
                b = i % 2
                em.dma_fn("pool", lambda g: g.indirect_dma_start(
                    out=y1[b][:, :], out_offset=None, in_=ye_d[:, :],
                    in_offset=bass.IndirectOffsetOnAxis(ap=ii[:, i, 0:1], axis=0),
                    bounds_check=bc_reg[0], oob_is_err=False),
                    reads=[R["ye_d"], r_gi], writes=[r_y1[b]])
                em.dma_fn("pool", lambda g: g.indirect_dma_start(
                    out=y2[b][:, :], out_offset=None, in_=ye_d[:, :],
                    in_offset=bass.IndirectOffsetOnAxis(ap=ii[:, i, 1:2], axis=0),
                    bounds_check=bc_reg[0], oob_is_err=False),
                    reads=[R["ye_d"], r_gi], writes=[r_y2[b]])

            gather(0)
            for i in range(NTQ):
                b = i % 2
                if i + 1 < NTQ:
                    gather(i + 1)
                em.op("dve", lambda e: e.tensor_scalar(out=y1[b][:], in0=y1[b][:], scalar1=gi[:, i, 0:1], scalar2=None,
                                                       op0=ALU.mult), reads=[r_y1[b], r_gi], writes=[r_y1[b]])
                em.op("dve", lambda e: e.scalar_tensor_tensor(out=y1[b][:], in0=y2[b][:], scalar=gi[:, i, 1:2],
                                                              in1=y1[b][:], op0=ALU.mult, op1=ALU.add),
                      reads=[r_y1[b], r_y2[b], r_gi], writes=[r_y1[b]])
                halves = [(y1[b][:, hf * 512:(hf + 1) * 512], r_y1[b]) for hf in range(D // 512)]
                if final:
                    ln_stage(lo_, i, halves, x1_d, out_d, R["out_d"], None, None)
                else:
                    ln_stage(lo_, i, halves, x1_d, xcur_d, R["xcur_d"], xT_d, R["xT_d"])
            em.barrier()

    bc_reg[0] = nc.gpsimd.to_reg(NEXP * CAP - 1)
    phase_tables()
    phase_x0()
    for l in range(DEPTH):
        j = l // 2
        is_moe = (l % 2 == 1)
        phase_proj(l)
        phase_fox()
        phase_dsa()
        phase_merge(l, j if is_moe else None)
        if is_moe:
            phase_moe(l, j, l == DEPTH - 1)
        else:
            phase_ffn(l, [(ffn_wg[j], ffn_wu[j], ffn_wd[j])], False, l == DEPTH - 1)
    top.close()
    return nc, em


_INPUT_ORDER = ["x", "positions", "w_in", "b_forget", "w_branch_a", "w_branch_b", "w_out", "ln_mix_g", "ln_mix_b",
                "ln_ffn_g", "ln_ffn_b", "ffn_w_gate", "ffn_w_up", "ffn_w_down", "moe_router", "moe_w_gate",
                "moe_w_up", "moe_w_down"]


def kernel(**inputs):
    x = np.asarray(inputs["x"])
    B, T, D = x.shape
    depth = int(np.asarray(inputs["w_in"]).shape[0])
    DFF = int(np.asarray(inputs["ffn_w_gate"]).shape[-1])
    nc, em = build_program(T, D, DFF, depth)
    shared = {k: np.ascontiguousarray(np.asarray(inputs[k])) for k in _INPUT_ORDER if k not in ("x", "positions")}
    in_maps = []
    for b in range(B):
        m = dict(shared)
        m["x"] = np.ascontiguousarray(x[b])
        m["positions"] = np.ascontiguousarray(np.asarray(inputs["positions"])[b:b + 1]).astype(np.int32)
        in_maps.append(m)
    res = run_bass_kernel_spmd(nc, in_maps, core_ids=list(range(B)))
    return np.stack([np.asarray(r["out"]) for r in res.results], axis=0).astype(np.float32)
```

```python
import math
from contextlib import ExitStack

import numpy as np
import concourse.bass as bass
import concourse.mybir as mybir
from concourse.bass_utils import run_bass_kernel_spmd

F32 = mybir.dt.float32
BF16 = mybir.dt.bfloat16
I32 = mybir.dt.int32
AF = mybir.ActivationFunctionType
ALU = mybir.AluOpType
AX = mybir.AxisListType

N_CORES = 8
PROJ_SIZES = (512, 512, 512, 512, 64, 8, 512, 512, 512, 8, 1024, 1024)
PROJ_TOTAL = sum(PROJ_SIZES)
OFF = dict(dq=0, dk=512, dv=1024, iq=1536, ik=2048, iw=2112, fq=2120, fk=2632, fv=3144,
           fl=3656, ga=3664, gb=4688)
ROPE_THETA = 10000.0
LN_EPS = 1e-5
NEG = -30000.0


class Res:
    __slots__ = ("name", "w", "r", "x")

    def __init__(self, name=""):
        self.name = name
        self.w = {}
        self.r = {}
        self.x = None


def _key(tok):
    return tok[:2] if tok[0] == "c" else tok[:3]


class Emitter:
    COMPUTE = ("pe", "act", "dve", "pool")
    QUEUES = ("sp", "act", "pool")

    def __init__(self, nc, stack, n_dma_sems=10):
        self.nc = nc
        self.engs = {"pe": nc.tensor, "act": nc.scalar, "dve": nc.vector,
                     "pool": nc.gpsimd, "sp": nc.sync}
        self.sem = {e: stack.enter_context(nc.semaphore("c_" + e)) for e in self.COMPUTE}
        self.cnt = {e: 0 for e in self.COMPUTE}
        self.dsem = {q: [stack.enter_context(nc.semaphore(f"d_{q}{i}")) for i in range(n_dma_sems)]
                     for q in self.QUEUES}
        self.dcnt = {q: 0 for q in self.QUEUES}
        self.nd = n_dma_sems
        self.seen = {e: {} for e in self.engs}
        self.pend = []
        self.n_ins = 0
        self.n_wait = 0

    def _wait(self, e, tok):
        if tok[0] == "c":
            _, f, n = tok
            if f == "pe" and e == "pe":
                return
            key = ("c", f)
            s = self.sem[f]
        else:
            _, q, idx, n = tok
            key = ("d", q, idx)
            s = self.dsem[q][idx]
        if self.seen[e].get(key, 0) >= n:
            return
        self.seen[e][key] = n
        self.pend.append((s, n))

    def _deps(self, e, reads, writes, pwrites):
        for r in reads:
            for t in r.w.values():
                self._wait(e, t)
        for w in writes:
            for t in w.w.values():
                self._wait(e, t)
            for t in w.r.values():
                self._wait(e, t)
        for w in pwrites:
            for t in w.r.values():
                self._wait(e, t)
            if w.x is not None:
                self._wait(e, w.x)

    def _record(self, tok, reads, writes, pwrites):
        k = _key(tok)
        for r in reads:
            r.r[k] = tok
        for w in writes:
            w.w = {k: tok}
            w.r = {}
            w.x = tok
        for w in pwrites:
            w.w[k] = tok

    def _flush(self, e, ins_fn):
        pend, self.pend = self.pend, []
        if pend:
            for (s, n) in pend[:-1]:
                self.engs[e].wait_ge(s, n)
                self.n_wait += 1
            ins = ins_fn()
            ins._wait_ge(pend[-1][0], pend[-1][1])
            return ins
        return ins_fn()

    def op(self, e, fn, reads=(), writes=(), pwrites=(), inc=True):
        self._deps(e, reads, writes, pwrites)
        ins = self._flush(e, lambda: fn(self.engs[e]))
        if inc:
            self.cnt[e] += 1
            ins.then_inc(self.sem[e], 1)
            tok = ("c", e, self.cnt[e])
        else:
            tok = ("c", e, self.cnt[e] + 1)
        self._record(tok, reads, writes, pwrites)
        self.n_ins += 1
        return tok

    def dma(self, q, out, in_, reads=(), writes=(), pwrites=(), **kw):
        i = self.dcnt[q]
        idx = i % self.nd
        prev = 16 * (i // self.nd)
        if prev > 0:
            self._wait(q, ("d", q, idx, prev))
        self._deps(q, reads, writes, pwrites)
        ins = self._flush(q, lambda: self.engs[q].dma_start(out=out, in_=in_, **kw))
        ins.then_inc(self.dsem[q][idx], 16)
        self.dcnt[q] += 1
        tok = ("d", q, idx, prev + 16)
        self._record(tok, reads, writes, pwrites)
        self.n_ins += 1
        return tok

    def dma_fn(self, q, fn, reads=(), writes=(), pwrites=()):
        i = self.dcnt[q]
        idx = i % self.nd
        prev = 16 * (i // self.nd)
        if prev > 0:
            self._wait(q, ("d", q, idx, prev))
        self._deps(q, reads, writes, pwrites)
        ins = self._flush(q, lambda: fn(self.engs[q]))
        ins.then_inc(self.dsem[q][idx], 16)
        self.dcnt[q] += 1
        tok = ("d", q, idx, prev + 16)
        self._record(tok, reads, writes, pwrites)
        self.n_ins += 1
        return tok

    def barrier(self, engines=None):
        for e in (engines or list(self.engs)):
            for f in self.COMPUTE:
                if self.cnt[f] > 0:
                    self._wait(e, ("c", f, self.cnt[f])) if not (e == "pe" and f == "pe") else None
            if e == "pe" and self.cnt["pe"] > 0 and self.seen["pe"].get(("c", "pe"), 0) < self.cnt["pe"]:
                self.seen["pe"][("c", "pe")] = self.cnt["pe"]
                self.pend.append((self.sem["pe"], self.cnt["pe"]))
            for q in self.QUEUES:
                for idx in range(self.nd):
                    n_used = (self.dcnt[q] - idx + self.nd - 1) // self.nd if self.dcnt[q] > idx else 0
                    if n_used > 0:
                        self._wait(e, ("d", q, idx, 16 * n_used))
            pend, self.pend = self.pend, []
            for (s, n) in pend:
                self.engs[e].wait_ge(s, n)
                self.n_wait += 1


def build_program(T, D, DFF, DEPTH, NEXP=8, bisect_iters=12):
    assert T % 512 == 0 and D % 512 == 0 and DFF % 512 == 0
    KT = D // 128
    NTQ = T // 128
    NTB = T // 512
    FT = DFF // 128
    TOPK = min(256, T // 4)
    n_dense = (DEPTH + 1) // 2
    n_moe = DEPTH // 2
    alpha = (2.0 * DEPTH) ** 0.25
    TBF = 1024 if T % 1024 == 0 else 512
    NTBF = T // TBF

    nc = bass.Bass("TRN2", target_bir_lowering=False)

    def din(name, shape, dt=F32):
        return nc.dram_tensor(name, list(shape), dt, kind="ExternalInput").ap()

    def dscr(name, shape, dt):
        return nc.dram_tensor(name, list(shape), dt).ap()

    x_in = din("x", [T, D])
    pos_in = din("positions", [1, T], I32)
    w_in = din("w_in", [DEPTH, D, PROJ_TOTAL])
    b_forget = din("b_forget", [DEPTH, 8])
    w_ba = din("w_branch_a", [DEPTH, 512, D])
    w_bb = din("w_branch_b", [DEPTH, 512, D])
    w_out = din("w_out", [DEPTH, D, D])
    ln_mix_g = din("ln_mix_g", [DEPTH, D])
    ln_mix_b = din("ln_mix_b", [DEPTH, D])
    ln_ffn_g = din("ln_ffn_g", [DEPTH, D])
    ln_ffn_b = din("ln_ffn_b", [DEPTH, D])
    ffn_wg = din("ffn_w_gate", [n_dense, D, DFF])
    ffn_wu = din("ffn_w_up", [n_dense, D, DFF])
    ffn_wd = din("ffn_w_down", [n_dense, DFF, D])
    moe_router = din("moe_router", [max(n_moe, 1), D, NEXP])
    moe_wg = din("moe_w_gate", [max(n_moe, 1), NEXP, D, DFF])
    moe_wu = din("moe_w_up", [max(n_moe, 1), NEXP, D, DFF])
    moe_wd = din("moe_w_down", [max(n_moe, 1), NEXP, DFF, D])
    out_d = nc.dram_tensor("out", [T, D], F32, kind="ExternalOutput").ap()

    xT_d = dscr("xT_d", [D, T], BF16)
    x1T_d = dscr("x1T_d", [D, T], BF16)
    xcur_d = dscr("xcur_d", [T, D], F32)
    x1_d = dscr("x1_d", [T, D], F32)
    cosT_d = dscr("cosT_d", [128, T], F32)
    sinT_d = dscr("sinT_d", [128, T], F32)
    dqT_d = dscr("dqT_d", [512, T], BF16)
    dkT_d = dscr("dkT_d", [512, T], BF16)
    iqT_d = dscr("iqT_d", [512, T], BF16)
    ikT_d = dscr("ikT_d", [64, T], BF16)
    fqT_d = dscr("fqT_d", [512, T], BF16)
    fkT_d = dscr("fkT_d", [512, T], BF16)
    dv_d = dscr("dv_d", [T, 768], BF16)
    fv_d = dscr("fv_d", [T, 768], BF16)
    wabs_d = dscr("wabs_d", [128, NTQ * 8], F32)
    wsgn_d = dscr("wsgn_d", [128, NTQ * 8], F32)
    aug_d = dscr("aug_d", [96, T], BF16)
    sgaT_d = dscr("sgaT_d", [D, T], BF16)
    sgbT_d = dscr("sgbT_d", [D, T], BF16)
    oaT_d = dscr("oaT_d", [512, T], BF16)
    obT_d = dscr("obT_d", [512, T], BF16)
    gateT_d = dscr("gateT_d", [NEXP, T], F32)
    CAP = max(512, ((T * 3 // 8) + 511) // 512 * 512)
    xe_d = dscr("xe_d", [NEXP * CAP, D], BF16)
    ye_d = dscr("ye_d", [NEXP * CAP, D], F32)
    moe_g_d = dscr("moe_g_d", [128, NTQ * 2], F32)
    moe_i_d = dscr("moe_i_d", [128, NTQ * 2], I32)

    R = {n: Res(n) for n in ("xT_d", "x1T_d", "xcur_d", "x1_d", "tab_d", "dqT_d", "dkT_d", "iqT_d",
                             "ikT_d", "fqT_d", "fkT_d", "dv_d", "fv_d", "w_d", "aug_d", "sgaT_d",
                             "sgbT_d", "oaT_d", "obT_d", "gateT_d", "out_d", "xe_d", "ye_d", "moe_d")}

    top = ExitStack()
    em = Emitter(nc, top)

    uid = [0]
    bc_reg = [None]

    def sb(st, name, shape, dt):
        uid[0] += 1
        return st.enter_context(nc.sbuf_tensor(f"{name}_{uid[0]}", list(shape), dt))

    def ps(st, name, shape=(128, 512), dt=F32):
        uid[0] += 1
        return st.enter_context(nc.psum_tensor(f"{name}_{uid[0]}", list(shape), dt))

    ident_b = sb(top, "ident_b", [128, 128], BF16)
    ident_f = sb(top, "ident_f", [128, 128], F32)
    tri_b = sb(top, "tri_b", [128, 128], BF16)
    tri_f = sb(top, "tri_f", [128, 128], F32)
    ones_f = sb(top, "ones_f", [128, 128], F32)
    r_const = Res("const")

    with ExitStack() as ph:
        io_i = sb(ph, "io_i", [128, 128], I32)
        r_io = Res()
        em.op("pool", lambda e: e.iota(io_i[:], pattern=[[1, 128]], base=0, channel_multiplier=-1),
              writes=[r_io])
        em.op("dve", lambda e: e.tensor_scalar(out=ident_f[:], in0=io_i[:], scalar1=0.0, scalar2=None,
                                               op0=ALU.is_equal), reads=[r_io], pwrites=[r_const])
        em.op("dve", lambda e: e.tensor_scalar(out=ident_b[:], in0=io_i[:], scalar1=0.0, scalar2=None,
                                               op0=ALU.is_equal), reads=[r_io], pwrites=[r_const])
        em.op("dve", lambda e: e.tensor_scalar(out=tri_f[:], in0=io_i[:], scalar1=0.0, scalar2=None,
                                               op0=ALU.is_ge), reads=[r_io], pwrites=[r_const])
        em.op("dve", lambda e: e.tensor_scalar(out=tri_b[:], in0=io_i[:], scalar1=0.0, scalar2=None,
                                               op0=ALU.is_ge), reads=[r_io], pwrites=[r_const])
        em.op("dve", lambda e: e.memset(ones_f[:], 1.0), pwrites=[r_const])
        em.barrier()

    def phase_tables():
        with ExitStack() as ph:
            posi = sb(ph, "posi", [128, T], I32)
            ang = sb(ph, "ang", [128, T], F32)
            a2 = sb(ph, "a2", [128, T], F32)
            u = sb(ph, "u", [128, T], F32)
            ni = sb(ph, "ni", [128, T], I32)
            nf = sb(ph, "nf", [128, T], F32)
            ji = sb(ph, "ji", [128, 1], I32)
            jf = sb(ph, "jf", [128, 1], F32)
            invf = sb(ph, "invf", [128, 1], F32)
            r_pos, r_ang, r_a2, r_u, r_ni, r_nf, r_j, r_jf, r_inv = (Res() for _ in range(9))
            em.dma("sp", posi[:], pos_in.to_broadcast([128, T]), writes=[r_pos])
            for g in range(4):
                em.op("pool", lambda e: e.iota(ji[32 * g:32 * g + 32, :], pattern=[[0, 1]], base=0,
                                               channel_multiplier=1), pwrites=[r_j])
            em.op("dve", lambda e: e.tensor_copy(out=jf[:], in_=ji[:]), reads=[r_j], writes=[r_jf])
            em.op("act", lambda e: e.activation(out=invf[:], in_=jf[:], func=AF.Exp,
                                                scale=-math.log(ROPE_THETA) / 32.0),
                  reads=[r_jf], writes=[r_inv])
            em.op("dve", lambda e: e.tensor_copy(out=ang[:], in_=posi[:]), reads=[r_pos], writes=[r_ang])
            em.op("dve", lambda e: e.tensor_scalar(out=ang[:], in0=ang[:], scalar1=invf[:, 0:1], scalar2=None,
                                                   op0=ALU.mult), reads=[r_ang, r_inv], writes=[r_ang])
            C1 = 6.28125
            C2 = 2.0 * math.pi - C1
            PI_LO = 3.1415925
            for (dst, shift) in ((sinT_d, 0.0), (cosT_d, math.pi / 2)):
                em.op("dve", lambda e: e.tensor_scalar(out=a2[:], in0=ang[:], scalar1=shift, scalar2=None,
                                                       op0=ALU.add), reads=[r_ang], writes=[r_a2])
                em.op("dve", lambda e: e.tensor_scalar(out=u[:], in0=a2[:], scalar1=1.0 / (2 * math.pi),
                                                       scalar2=0.5, op0=ALU.mult, op1=ALU.add),
                      reads=[r_a2], writes=[r_u])
                em.op("dve", lambda e: e.tensor_copy(out=ni[:], in_=u[:]), reads=[r_u], writes=[r_ni])
                em.op("dve", lambda e: e.tensor_copy(out=nf[:], in_=ni[:]), reads=[r_ni], writes=[r_nf])
                em.op("dve", lambda e: e.scalar_tensor_tensor(out=a2[:], in0=nf[:], scalar=-C1, in1=a2[:],
                                                              op0=ALU.mult, op1=ALU.add),
                      reads=[r_nf, r_a2], writes=[r_a2])
                em.op("dve", lambda e: e.scalar_tensor_tensor(out=a2[:], in0=nf[:], scalar=-C2, in1=a2[:],
                                                              op0=ALU.mult, op1=ALU.add),
                      reads=[r_nf, r_a2], writes=[r_a2])
                em.op("dve", lambda e: e.tensor_scalar(out=u[:], in0=a2[:], scalar1=-math.pi,
                                                       scalar2=2 * math.pi, op0=ALU.is_lt, op1=ALU.mult),
                      reads=[r_a2], writes=[r_u])
                em.op("dve", lambda e: e.tensor_tensor(out=a2[:], in0=a2[:], in1=u[:], op=ALU.add),
                      reads=[r_a2, r_u], writes=[r_a2])
                em.op("dve", lambda e: e.tensor_scalar(out=u[:], in0=a2[:], scalar1=math.pi,
                                                       scalar2=-2 * math.pi, op0=ALU.is_gt, op1=ALU.mult),
                      reads=[r_a2], writes=[r_u])
                em.op("dve", lambda e: e.tensor_tensor(out=a2[:], in0=a2[:], in1=u[:], op=ALU.add),
                      reads=[r_a2, r_u], writes=[r_a2])
                em.op("dve", lambda e: e.tensor_scalar(out=a2[:], in0=a2[:], scalar1=-PI_LO, scalar2=PI_LO,
                                                       op0=ALU.max, op1=ALU.min), reads=[r_a2], writes=[r_a2])
                em.op("act", lambda e: e.activation(out=u[:], in_=a2[:], func=AF.Sin),
                      reads=[r_a2], writes=[r_u])
                em.dma("sp", dst[:, :], u[:], reads=[r_u], pwrites=[R["tab_d"]])
            em.barrier()

    def emit_transposes(ph_objs, src_tile, r_src, qi):
        xb, r_xb, tp, r_tp, stage, r_stage = ph_objs
        em.op("act", lambda e: e.activation(out=xb[:], in_=src_tile[:], func=AF.Copy),
              reads=[r_src], writes=[r_xb])
        for kt in range(KT):
            em.op("pe", lambda e: e.transpose(tp[:, kt * 128:(kt + 1) * 128], xb[:, kt * 128:(kt + 1) * 128],
                                              ident_b[:]),
                  reads=[r_xb, r_const], writes=[r_tp] if kt == 0 else [], pwrites=[] if kt == 0 else [r_tp],
                  inc=(kt == KT - 1))
        em.op("dve", lambda e: e.tensor_copy(out=stage[:, :, qi * 128:(qi + 1) * 128],
                                             in_=tp[:].rearrange("p (k c) -> p k c", c=128)),
              reads=[r_tp], pwrites=[r_stage])

    def phase_x0():
        with ExitStack() as ph:
            xt = [sb(ph, f"x0_{i}", [128, D], F32) for i in range(2)]
            r_xt = [Res() for _ in range(2)]
            xb = sb(ph, "x0b", [128, D], BF16)
            tp = ps(ph, "x0tp", [128, D], BF16)
            stage = [sb(ph, f"x0s{i}", [128, KT, 512], BF16) for i in range(2)]
            r_stage = [Res() for _ in range(2)]
            r_xb, r_tp = Res(), Res()
            for i in range(NTQ):
                b = i % 2
                tb, qi = i // 4, i % 4
                em.dma("sp", xt[b][:], x_in[i * 128:(i + 1) * 128, :], writes=[r_xt[b]])
                emit_transposes((xb, r_xb, tp, r_tp, stage[tb % 2], r_stage[tb % 2]), xt[b], r_xt[b], qi)
                if qi == 3:
                    em.dma("sp", xT_d.rearrange("(k p) t -> p k t", p=128)[:, :, tb * 512:(tb + 1) * 512],
                           stage[tb % 2][:], reads=[r_stage[tb % 2]], pwrites=[R["xT_d"]])
            em.barrier()

    def phase_proj(l):
        with ExitStack() as ph:
            xT = sb(ph, "p1_xT", [128, KT, T], BF16)
            r_xT = Res()
            cosT = sb(ph, "p1_cos", [128, T], F32)
            sinT = sb(ph, "p1_sin", [128, T], F32)
            r_tab = Res()
            wb = [sb(ph, f"p1_w{i}", [128, KT, 512], BF16) for i in range(2)]
            r_wb = [Res() for _ in range(2)]
            wsw = sb(ph, "p1_wsw", [128, KT, 512], BF16)
            r_wsw = Res()
            stage = [sb(ph, f"p1_st{i}", [128, 512], BF16) for i in range(3)]
            r_st = [Res() for _ in range(3)]
            t1 = sb(ph, "p1_t1", [128, 512], F32)
            t2 = sb(ph, "p1_t2", [128, 512], F32)
            r_t1, r_t2 = Res(), Res()
            stv = [sb(ph, f"p1_sv{i}", [128, 4, 192], BF16) for i in range(2)]
            r_stv = [Res() for _ in range(2)]
            psA = [ps(ph, f"p1_pA{i}") for i in range(2)]
            psB = [ps(ph, f"p1_pB{i}") for i in range(2)]
            r_pA = [Res() for _ in range(2)]
            r_pB = [Res() for _ in range(2)]
            pss = ps(ph, "p1_pss")
            r_pss = Res()
            pst = ps(ph, "p1_pst")
            r_pst = Res()
            bfb = sb(ph, "p1_bf", [128, 8], F32)
            r_bfb = Res()
            logf = sb(ph, "p1_logf", [128, NTQ, 8], F32)
            r_logf = Res()
            wabs = sb(ph, "p1_wabs", [128, NTQ, 8], F32)
            wsgn = sb(ph, "p1_wsgn", [128, NTQ, 8], F32)
            r_wab, r_wsg = Res(), Res()
            augT = sb(ph, "p1_augT", [96, T], BF16)
            r_augT = Res()
            A = sb(ph, "p1_A", [128, 96], F32)
            r_A = Res()
            sm = {n: sb(ph, "p1_" + n, [128, 8], F32) for n in ("z", "e", "c", "hif", "d1", "midf", "d2", "lof")}
            smb = {n: sb(ph, "p1_" + n, [128, 8], BF16) for n in ("hib", "midb", "lob")}
            r_sm = {n: Res() for n in list(sm) + list(smb)}

            em.dma("sp", xT[:], xT_d.rearrange("(k p) t -> p k t", p=128), reads=[R["xT_d"]], writes=[r_xT])
            em.dma("sp", cosT[:], cosT_d[:, :], reads=[R["tab_d"]], pwrites=[r_tab])
            em.dma("sp", sinT[:], sinT_d[:, :], reads=[R["tab_d"]], pwrites=[r_tab])
            em.dma("sp", bfb[:], b_forget[l].partition_broadcast(128), writes=[r_bfb])
            em.op("dve", lambda e: e.memset(A[:], 1.0), writes=[r_A])
            for i in range(2):
                em.op("pool", lambda e: e.memset(stv[i][:], 1.0), writes=[r_stv[i]])

            wl = w_in[l]
            groups = [("dq", 512, "rot", dqT_d, 0.125), ("dk", 512, "rot", dkT_d, 1.0),
                      ("dv", 512, "tok", dv_d, 1.0), ("iq", 512, "rot", iqT_d, 0.125),
                      ("ik", 72, "ikiw", ikT_d, 1.0),
                      ("fq", 512, "plain", fqT_d, 0.125), ("fk", 512, "plain", fkT_d, 1.0),
                      ("fv", 512, "tok", fv_d, 1.0), ("fl", 8, "fl", None, 1.0),
                      ("ga", 512, "sig", sgaT_d, 0), ("ga2", 512, "sig", sgaT_d, 512),
                      ("gb", 512, "sig", sgbT_d, 0), ("gb2", 512, "sig", sgbT_d, 512)]
            if D != 1024:
                raise NotImplementedError

            def colstart(name):
                if name == "ga2":
                    return OFF["ga"] + 512
                if name == "gb2":
                    return OFF["gb"] + 512
                return OFF[name]

            def load_w(gi):
                name, ncol = groups[gi][0], groups[gi][1]
                c0 = colstart(name)
                b = gi % 2
                em.dma("pool", wb[b][:, :, 0:ncol],
                       wl[:, c0:c0 + ncol].rearrange("(k p) c -> p k c", p=128), writes=[r_wb[b]])

            cnt = {"st": 0, "pp": 0, "sv": 0}

            def fm_tile(W, r_W, Wsw, col0, M, tb, kind, scale, dst, drow0):
                p = cnt["pp"] % 2
                cnt["pp"] += 1
                tsl = slice(tb * 512, (tb + 1) * 512)
                for kt in range(KT):
                    em.op("pe", lambda e: e.matmul(psA[p][0:M, :], lhsT=W[:, kt, col0:col0 + M],
                                                   rhs=xT[:, kt, tsl], start=(kt == 0), stop=(kt == KT - 1)),
                          reads=[r_W, r_xT], writes=[r_pA[p]] if kt == 0 else [], pwrites=[] if kt == 0 else [r_pA[p]],
                          inc=(kt == KT - 1))
                if kind == "rot":
                    for kt in range(KT):
                        em.op("pe", lambda e: e.matmul(psB[p][0:M, :], lhsT=Wsw[:, kt, col0:col0 + M],
                                                       rhs=xT[:, kt, tsl], start=(kt == 0), stop=(kt == KT - 1)),
                              reads=[r_wsw, r_xT], writes=[r_pB[p]] if kt == 0 else [],
                              pwrites=[] if kt == 0 else [r_pB[p]], inc=(kt == KT - 1))
                s = cnt["st"] % 3
                cnt["st"] += 1
                if kind == "rot":
                    em.op("dve", lambda e: e.scalar_tensor_tensor(out=t1[0:M, :], in0=psA[p][0:M, :], scalar=scale,
                                                                  in1=cosT[0:M, tsl], op0=ALU.mult, op1=ALU.mult),
                          reads=[r_pA[p], r_tab], writes=[r_t1])
                    em.op("dve", lambda e: e.scalar_tensor_tensor(out=t2[0:M, :], in0=psB[p][0:M, :], scalar=scale,
                                                                  in1=sinT[0:M, tsl], op0=ALU.mult, op1=ALU.mult),
                          reads=[r_pB[p], r_tab], writes=[r_t2])
                    em.op("pool", lambda e: e.tensor_tensor(out=stage[s][0:M, :], in0=t1[0:M, :], in1=t2[0:M, :],
                                                            op=ALU.add),
                          reads=[r_t1, r_t2], writes=[r_st[s]])
                elif kind == "plain":
                    em.op("act", lambda e: e.activation(out=stage[s][0:M, :], in_=psA[p][0:M, :], func=AF.Copy,
                                                        scale=scale), reads=[r_pA[p]], writes=[r_st[s]])
                else:
                    em.op("act", lambda e: e.activation(out=stage[s][0:M, :], in_=psA[p][0:M, :], func=AF.Sigmoid),
                          reads=[r_pA[p]], writes=[r_st[s]])
                em.dma("sp", dst[drow0:drow0 + M, tsl], stage[s][0:M, :], reads=[r_st[s]], pwrites=[R[_rn(dst)]])

            names = {id(dqT_d): "dqT_d", id(dkT_d): "dkT_d", id(iqT_d): "iqT_d", id(ikT_d): "ikT_d",
                     id(fqT_d): "fqT_d", id(fkT_d): "fkT_d", id(sgaT_d): "sgaT_d", id(sgbT_d): "sgbT_d",
                     id(dv_d): "dv_d", id(fv_d): "fv_d"}

            def _rn(d):
                return names[id(d)]

            def make_swapped(W, r_W, ncol):
                nh = ncol // 64
                Wv = W[:, :, 0:ncol].rearrange("p k (h two j) -> p k h two j", two=2, j=32)
                Sv = wsw[:, :, 0:ncol].rearrange("p k (h two j) -> p k h two j", two=2, j=32)
                for kt in range(KT):
                    em.op("act", lambda e: e.activation(out=Sv[:, kt, :, 0, :], in_=Wv[:, kt, :, 1, :], func=AF.Copy,
                                                        scale=-1.0),
                          reads=[r_W], writes=[r_wsw] if kt == 0 else [], pwrites=[] if kt == 0 else [r_wsw])
                    em.op("pool", lambda e: e.tensor_copy(out=Sv[:, kt, :, 1, :], in_=Wv[:, kt, :, 0, :]),
                          reads=[r_W], pwrites=[r_wsw])

            load_w(0)
            for gi, (name, ncol, kind, dst, extra) in enumerate(groups):
                if gi + 1 < len(groups):
                    load_w(gi + 1)
                W, r_W = wb[gi % 2], r_wb[gi % 2]
                if kind in ("rot",):
                    make_swapped(W, r_W, ncol)
                    for ct in range(ncol // 128):
                        for tb in range(NTB):
                            fm_tile(W, r_W, wsw, ct * 128, 128, tb, "rot", extra, dst, ct * 128)
                elif kind == "plain":
                    for ct in range(ncol // 128):
                        for tb in range(NTB):
                            fm_tile(W, r_W, None, ct * 128, 128, tb, "plain", extra, dst, ct * 128)
                elif kind == "sig":
                    for ct in range(ncol // 128):
                        for tb in range(NTB):
                            fm_tile(W, r_W, None, ct * 128, 128, tb, "sig", 1.0, dst, extra + ct * 128)
                elif kind == "ikiw":
                    make_swapped(W, r_W, 64)
                    for tb in range(NTB):
                        fm_tile(W, r_W, wsw, 0, 64, tb, "rot", 1.0, dst, 0)
                    for i in range(NTQ):
                        for kt in range(KT):
                            em.op("pe", lambda e: e.matmul(pss[:, 0:8], lhsT=xT[:, kt, i * 128:(i + 1) * 128],
                                                           rhs=W[:, kt, 64:72], start=(kt == 0), stop=(kt == KT - 1)),
                                  reads=[r_W, r_xT], writes=[r_pss] if kt == 0 else [],
                                  pwrites=[] if kt == 0 else [r_pss], inc=(kt == KT - 1))
                        em.op("act", lambda e: e.activation(out=wabs[:, i, :], in_=pss[:, 0:8], func=AF.Abs,
                                                            scale=8.0 ** -0.5),
                              reads=[r_pss], pwrites=[r_wab])
                        em.op("act", lambda e: e.activation(out=wsgn[:, i, :], in_=pss[:, 0:8], func=AF.Sign),
                              reads=[r_pss], pwrites=[r_wsg])
                    em.dma("sp", wabs_d[:, :], wabs[:].rearrange("p a b -> p (a b)"), reads=[r_wab], pwrites=[R["w_d"]])
                    em.dma("sp", wsgn_d[:, :], wsgn[:].rearrange("p a b -> p (a b)"), reads=[r_wsg], pwrites=[R["w_d"]])
                elif kind == "tok":
                    for i in range(NTQ):
                        p = cnt["pp"] % 2
                        cnt["pp"] += 1
                        for kt in range(KT):
                            em.op("pe", lambda e: e.matmul(psA[p][:, :], lhsT=xT[:, kt, i * 128:(i + 1) * 128],
                                                           rhs=W[:, kt, 0:512], start=(kt == 0), stop=(kt == KT - 1)),
                                  reads=[r_W, r_xT], writes=[r_pA[p]] if kt == 0 else [],
                                  pwrites=[] if kt == 0 else [r_pA[p]], inc=(kt == KT - 1))
                        s = cnt["sv"] % 2
                        cnt["sv"] += 1
                        sv = stv[s][:].rearrange("p a (three c) -> p a three c", c=64)
                        pv = psA[p][:, :].rearrange("p (a two c) -> p a two c", two=2, c=64)
                        em.op("act", lambda e: e.activation(out=sv[:, :, 0, :], in_=pv[:, :, 0, :], func=AF.Copy),
                              reads=[r_pA[p]], writes=[r_stv[s]])
                        em.op("dve", lambda e: e.tensor_copy(out=sv[:, :, 2, :], in_=pv[:, :, 1, :]),
                              reads=[r_pA[p]], pwrites=[r_stv[s]])
                        em.dma("sp", dst[i * 128:(i + 1) * 128, :], stv[s][:].rearrange("p a c -> p (a c)"),
                               reads=[r_stv[s]], pwrites=[R[_rn(dst)]])
                elif kind == "fl":
                    for i in range(NTQ):
                        for kt in range(KT):
                            em.op("pe", lambda e: e.matmul(pss[:, 0:8], lhsT=xT[:, kt, i * 128:(i + 1) * 128],
                                                           rhs=W[:, kt, 0:8], start=(kt == 0), stop=(kt == KT - 1)),
                                  reads=[r_W, r_xT], writes=[r_pss] if kt == 0 else [],
                                  pwrites=[] if kt == 0 else [r_pss], inc=(kt == KT - 1))
                        em.op("dve", lambda e: e.tensor_tensor(out=sm["z"][:], in0=pss[:, 0:8], in1=bfb[:], op=ALU.add),
                              reads=[r_pss, r_bfb], writes=[r_sm["z"]])
                        em.op("act", lambda e: e.activation(out=sm["e"][:], in_=sm["z"][:], func=AF.Exp, scale=-1.0),
                              reads=[r_sm["z"]], writes=[r_sm["e"]])
                        em.op("act", lambda e: e.activation(out=sm["z"][:], in_=sm["e"][:], func=AF.Ln, bias=1.0),
                              reads=[r_sm["e"]], writes=[r_sm["z"]])
                        em.op("dve", lambda e: e.tensor_scalar(out=logf[:, i, :], in0=sm["z"][:], scalar1=-1.0,
                                                               scalar2=None, op0=ALU.mult),
                              reads=[r_sm["z"]], pwrites=[r_logf])
                        for j in range(i + 1):
                            em.op("pe", lambda e: e.matmul(pss[:, 8:16], lhsT=(tri_f if j == i else ones_f)[:, :],
                                                           rhs=logf[:, j, :], start=(j == 0), stop=(j == i)),
                                  reads=[r_logf, r_const], writes=[r_pss] if j == 0 else [],
                                  pwrites=[] if j == 0 else [r_pss], inc=(j == i))
                        em.op("dve", lambda e: e.tensor_copy(out=sm["c"][:], in_=pss[:, 8:16]),
                              reads=[r_pss], writes=[r_sm["c"]])
                        Av = A[:].rearrange("p (s h r) -> p s h r", s=2, r=6)
                        em.op("dve", lambda e: e.tensor_copy(out=smb["hib"][:], in_=sm["c"][:]),
                              reads=[r_sm["c"]], writes=[r_sm["hib"]])
                        em.op("dve", lambda e: e.tensor_copy(out=sm["hif"][:], in_=smb["hib"][:]),
                              reads=[r_sm["hib"]], writes=[r_sm["hif"]])
                        em.op("dve", lambda e: e.tensor_tensor(out=sm["d1"][:], in0=sm["c"][:], in1=sm["hif"][:],
                                                               op=ALU.subtract),
                              reads=[r_sm["c"], r_sm["hif"]], writes=[r_sm["d1"]])
                        em.op("dve", lambda e: e.tensor_copy(out=smb["midb"][:], in_=sm["d1"][:]),
                              reads=[r_sm["d1"]], writes=[r_sm["midb"]])
                        em.op("dve", lambda e: e.tensor_copy(out=sm["midf"][:], in_=smb["midb"][:]),
                              reads=[r_sm["midb"]], writes=[r_sm["midf"]])
                        em.op("dve", lambda e: e.tensor_tensor(out=sm["d2"][:], in0=sm["d1"][:], in1=sm["midf"][:],
                                                               op=ALU.subtract),
                              reads=[r_sm["d1"], r_sm["midf"]], writes=[r_sm["d2"]])
                        em.op("dve", lambda e: e.tensor_copy(out=smb["lob"][:], in_=sm["d2"][:]),
                              reads=[r_sm["d2"]], writes=[r_sm["lob"]])
                        em.op("dve", lambda e: e.tensor_copy(out=sm["lof"][:], in_=smb["lob"][:]),
                              reads=[r_sm["lob"]], writes=[r_sm["lof"]])
                        for ri, nm in enumerate(("hif", "midf", "lof")):
                            em.op("dve", lambda e: e.tensor_copy(out=Av[:, 0, :, ri], in_=sm[nm][:]),
                                  reads=[r_sm[nm]], writes=[r_A] if ri == 0 else [], pwrites=[] if ri == 0 else [r_A])
                            em.op("dve", lambda e: e.tensor_scalar(out=Av[:, 1, :, 3 + ri], in0=sm[nm][:], scalar1=-1.0,
                                                                   scalar2=None, op0=ALU.mult),
                                  reads=[r_sm[nm]], pwrites=[r_A])
                        em.op("pe", lambda e: e.transpose(pst[0:96, 0:128], A[:, :], ident_f[:]),
                              reads=[r_A, r_const], writes=[r_pst])
                        em.op("act", lambda e: e.activation(out=augT[:, i * 128:(i + 1) * 128], in_=pst[0:96, 0:128],
                                                            func=AF.Copy), reads=[r_pst], pwrites=[r_augT])
                    em.dma("sp", aug_d[:, :], augT[:], reads=[r_augT], writes=[R["aug_d"]])
            em.barrier()

    def attn_alloc(ph, pfx):
        o = dict(
            sps=[ps(ph, f"{pfx}_s{i}") for i in range(3)], r_sps=[Res() for _ in range(3)],
            pexp=[sb(ph, f"{pfx}_p{i}", [128, 512], BF16) for i in range(3)], r_pexp=[Res() for _ in range(3)],
            ops=[ps(ph, f"{pfx}_o{i}") for i in range(2)], r_ops=[Res() for _ in range(2)],
            rden=sb(ph, f"{pfx}_rden", [128, 512], F32), r_rden=Res(),
            oT=[sb(ph, f"{pfx}_oT{i}", [128, 512], BF16) for i in range(2)], r_oT=[Res() for _ in range(2)],
            ctr={"o": 0, "s": 0})
        return o

    def attn_block(o, kT, r_kT, qT, r_qT, q0, Kc, prow, V, r_V, odd, n_ts, col_lo_fn, diag_fn, mask_mm_fn,
                   out_ap, r_out, store_q="sp"):
        ob = o["ctr"]["o"] % 2
        o["ctr"]["o"] += 1
        ops, r_ops = o["ops"][ob], o["r_ops"][ob]
        oT, r_oT = o["oT"][ob], o["r_oT"][ob]
        vc = 64 if odd else 0
        base = o["ctr"]["s"]
        o["ctr"]["s"] += n_ts
        has_mask = mask_mm_fn is not None

        def emit_S(i):
            lo = col_lo_fn(i)
            s = (base + i) % 3
            sps, r_sps = o["sps"][s], o["r_sps"][s]
            em.op("pe", lambda e: e.matmul(sps[:, lo:512], lhsT=kT[prow:prow + Kc, i * 128:(i + 1) * 128],
                                           rhs=qT[prow:prow + Kc, q0 + lo:q0 + 512], start=True, stop=not has_mask),
                  reads=[r_kT, r_qT], writes=[r_sps], inc=not has_mask)
            if has_mask:
                mask_mm_fn(i, lo, sps, r_sps)

        def emit_rest(i):
            lo = col_lo_fn(i)
            s = (base + i) % 3
            sps, r_sps, pexp, r_pexp = o["sps"][s], o["r_sps"][s], o["pexp"][s], o["r_pexp"][s]
            em.op("act", lambda e: e.activation(out=pexp[:, lo:512], in_=sps[:, lo:512], func=AF.Exp),
                  reads=[r_sps], writes=[r_pexp])
            dc = diag_fn(i) if diag_fn is not None else None
            if dc is not None:
                em.op("pool", lambda e: e.tensor_tensor(out=pexp[:, dc:dc + 128], in0=pexp[:, dc:dc + 128],
                                                        in1=tri_b[:, :], op=ALU.mult),
                      reads=[r_pexp, r_const], writes=[r_pexp])
            em.op("pe", lambda e: e.matmul(ops[:, lo:512], lhsT=V[:, i, vc:vc + 128], rhs=pexp[:, lo:512],
                                           start=(i == 0), stop=(i == n_ts - 1)),
                  reads=[r_V, r_pexp], writes=[r_ops] if i == 0 else [], pwrites=[] if i == 0 else [r_ops],
                  inc=(i == n_ts - 1))

        emit_S(0)
        for i in range(n_ts):
            if i + 1 < n_ts:
                emit_S(i + 1)
            emit_rest(i)
        (orow, drow) = (64, 0) if odd else (0, 64)
        em.op("dve", lambda e: e.reciprocal(out=o["rden"][orow:orow + 64, :], in_=ops[drow:drow + 64, :]),
              reads=[r_ops], writes=[o["r_rden"]])
        em.op("dve", lambda e: e.tensor_tensor(out=oT[orow:orow + 64, :], in0=ops[orow:orow + 64, :],
                                               in1=o["rden"][orow:orow + 64, :], op=ALU.mult),
              reads=[r_ops, o["r_rden"]], writes=[r_oT])
        em.dma(store_q, out_ap, oT[orow:orow + 64, :], reads=[r_oT], pwrites=[r_out])

    def phase_fox():
        with ExitStack() as ph:
            o = attn_alloc(ph, "fx")
            qa = [sb(ph, f"fx_q{i}", [70, T], BF16) for i in range(2)]
            ka = [sb(ph, f"fx_k{i}", [70, T], BF16) for i in range(2)]
            r_qa = [Res() for _ in range(2)]
            r_ka = [Res() for _ in range(2)]
            V = sb(ph, "fx_V", [128, NTQ, 768], BF16)
            r_V = Res()
            em.dma("sp", V[:], fv_d.rearrange("(i p) c -> p i c", p=128), reads=[R["fv_d"]], writes=[r_V])

            def load_head(h):
                b = h % 2
                em.dma("sp", qa[b][0:64, :], fqT_d[h * 64:(h + 1) * 64, :], reads=[R["fqT_d"]], writes=[r_qa[b]])
                em.dma("sp", qa[b][64:70, :], aug_d[h * 6:h * 6 + 6, :], reads=[R["aug_d"]], pwrites=[r_qa[b]])
                em.dma("sp", ka[b][0:64, :], fkT_d[h * 64:(h + 1) * 64, :], reads=[R["fkT_d"]], writes=[r_ka[b]])
                em.dma("sp", ka[b][64:70, :], aug_d[48 + h * 6:48 + h * 6 + 6, :], reads=[R["aug_d"]],
                       pwrites=[r_ka[b]])

            load_head(0)
            for h in range(8):
                if h + 1 < 8:
                    load_head(h + 1)
                b = h % 2
                pair, odd = h // 2, h % 2
                Vp = V[:, :, pair * 192:(pair + 1) * 192]
                for tb in range(NTB):
                    attn_block(o, ka[b], r_ka[b], qa[b], r_qa[b], tb * 512, 70, 0, Vp, r_V, odd, 4 * (tb + 1),
                               lambda i: max(0, (i - 4 * tb) * 128),
                               lambda i: ((i - 4 * tb) * 128 if i >= 4 * tb else None),
                               None, obT_d[h * 64:(h + 1) * 64, tb * 512:(tb + 1) * 512], R["obT_d"])
            em.barrier()

    def phase_dsa():
        with ExitStack() as ph:
            o = attn_alloc(ph, "ds")
            iq = sb(ph, "ds_iq", [128, 4, T], BF16)
            ik2 = sb(ph, "ds_ik", [128, T], BF16)
            wabs = sb(ph, "ds_wabs", [128, NTQ, 8], F32)
            wsgn = sb(ph, "ds_wsgn", [128, NTQ, 8], F32)
            r_iq, r_ik, r_w = Res(), Res(), Res()
            sc = sb(ph, "ds_sc", [128, T], F32)
            junk = sb(ph, "ds_junk", [128, T], BF16)
            r_sc, r_junk = Res(), Res()
            mbs = [[sb(ph, f"ds_mb{s_}_{i}", [128, T], BF16) for i in range(4)] for s_ in range(2)]
            r_mbs = [[Res() for _ in range(4)] for _ in range(2)]
            rl = [sb(ph, f"ds_rl{i}", [128, 512], F32) for i in range(2)]
            r_rl = [Res() for _ in range(2)]
            scps = [ps(ph, f"ds_sp{i}") for i in range(2)]
            r_scps = [Res() for _ in range(2)]
            sm = {n: sb(ph, "ds_" + n, [128, 1], F32) for n in ("lo", "w0", "wk", "mid", "cnt", "gw", "mx", "thr0")}
            r_sm = {n: Res() for n in sm}
            Vp = [sb(ph, f"ds_V{i}", [128, NTQ, 192], BF16) for i in range(2)]
            r_Vp = [Res() for _ in range(2)]
            qh = [sb(ph, f"ds_q{i}", [128, 512], BF16) for i in range(2)]
            kh = [sb(ph, f"ds_k{i}", [128, T], BF16) for i in range(2)]
            r_qh = [Res() for _ in range(2)]
            r_kh = [Res() for _ in range(2)]

            em.dma("sp", iq[:], iqT_d.rearrange("(k p) t -> p k t", p=128), reads=[R["iqT_d"]], writes=[r_iq])
            em.dma("sp", ik2[0:64, :], ikT_d[:, :], reads=[R["ikT_d"]], pwrites=[r_ik])
            em.dma("sp", ik2[64:128, :], ikT_d[:, :], reads=[R["ikT_d"]], pwrites=[r_ik])
            em.dma("sp", wabs[:].rearrange("p a b -> p (a b)"), wabs_d[:, :], reads=[R["w_d"]], pwrites=[r_w])
            em.dma("sp", wsgn[:].rearrange("p a b -> p (a b)"), wsgn_d[:, :], reads=[R["w_d"]], pwrites=[r_w])
            em.op("dve", lambda e: e.memset(sm["thr0"][:], -1e29), writes=[r_sm["thr0"]])
            ctr = {"p": 0, "v": 0, "h": 0}
            dv_v = dv_d.rearrange("(i p) c -> p i c", p=128)

            def prep_q(tb, qi, mb, r_mb):
                g = tb * 4 + qi
                L2 = 128 * (g + 1)
                L1 = L2 - 64
                for kb in range((L2 + 511) // 512):
                    c0 = kb * 512
                    cw = min(512, L2 - c0)
                    for h in range(8):
                        pair, prow = h // 2, (h % 2) * 64
                        p = ctr["p"] % 2
                        ctr["p"] += 1
                        em.op("pe", lambda e: e.matmul(scps[p][:, 0:cw],
                                                       lhsT=iq[prow:prow + 64, pair, g * 128:(g + 1) * 128],
                                                       rhs=ik2[prow:prow + 64, c0:c0 + cw], start=True, stop=True),
                              reads=[r_iq, r_ik], writes=[r_scps[p]])
                        em.op("act", lambda e: e.activation(out=rl[p][:, 0:cw], in_=scps[p][:, 0:cw], func=AF.Relu,
                                                            scale=wabs[:, g, h:h + 1]),
                              reads=[r_scps[p], r_w], writes=[r_rl[p]])
                        if h == 0:
                            em.op("dve", lambda e: e.tensor_scalar(out=sc[:, c0:c0 + cw], in0=rl[p][:, 0:cw],
                                                                   scalar1=wsgn[:, g, 0:1], scalar2=None,
                                                                   op0=ALU.mult),
                                  reads=[r_rl[p], r_w], writes=[r_sc] if kb == 0 else [],
                                  pwrites=[] if kb == 0 else [r_sc])
                        else:
                            em.op("dve", lambda e: e.scalar_tensor_tensor(out=sc[:, c0:c0 + cw], in0=rl[p][:, 0:cw],
                                                                          scalar=wsgn[:, g, h:h + 1],
                                                                          in1=sc[:, c0:c0 + cw], op0=ALU.mult,
                                                                          op1=ALU.add),
                                  reads=[r_rl[p], r_w, r_sc], pwrites=[r_sc])
                em.op("dve", lambda e: e.memset(sc[0:64, L1:L2], -1e30), reads=[r_sc], pwrites=[r_sc])
                if L2 > TOPK:
                    em.op("dve", lambda e: e.tensor_reduce(out=sm["mx"][:], in_=sc[:, 0:L2], axis=AX.X, op=ALU.max),
                          reads=[r_sc], writes=[r_sm["mx"]])
                    em.op("dve", lambda e: e.tensor_reduce(out=sm["lo"][:], in_=sc[:, 0:L1], axis=AX.X, op=ALU.min),
                          reads=[r_sc], writes=[r_sm["lo"]])
                    em.op("dve", lambda e: e.tensor_tensor(out=sm["w0"][:], in0=sm["mx"][:], in1=sm["lo"][:],
                                                           op=ALU.subtract),
                          reads=[r_sm["mx"], r_sm["lo"]], writes=[r_sm["w0"]])
                    em.op("dve", lambda e: e.tensor_scalar(out=sm["w0"][:], in0=sm["w0"][:], scalar1=1.0001,
                                                           scalar2=1e-12, op0=ALU.mult, op1=ALU.add),
                          reads=[r_sm["w0"]], writes=[r_sm["w0"]])
                    for k in range(bisect_iters):
                        em.op("dve", lambda e: e.scalar_tensor_tensor(out=sm["mid"][:], in0=sm["w0"][:],
                                                                      scalar=2.0 ** -(k + 1), in1=sm["lo"][:],
                                                                      op0=ALU.mult, op1=ALU.add),
                              reads=[r_sm["w0"], r_sm["lo"]], writes=[r_sm["mid"]])
                        em.op("dve", lambda e: e.tensor_scalar(out=junk[:, 0:L2], in0=sc[:, 0:L2],
                                                               scalar1=sm["mid"][:, 0:1], scalar2=None,
                                                               op0=ALU.is_ge, op1=ALU.add,
                                                               accum_out=sm["cnt"][:, 0:1]),
                              reads=[r_sc, r_sm["mid"]], writes=[r_junk, r_sm["cnt"]])
                        em.op("dve", lambda e: e.scalar_tensor_tensor(out=sm["gw"][:], in0=sm["cnt"][:],
                                                                      scalar=float(TOPK), in1=sm["w0"][:],
                                                                      op0=ALU.is_ge, op1=ALU.mult),
                              reads=[r_sm["cnt"], r_sm["w0"]], writes=[r_sm["gw"]])
                        em.op("dve", lambda e: e.scalar_tensor_tensor(out=sm["lo"][:], in0=sm["gw"][:],
                                                                      scalar=2.0 ** -(k + 1), in1=sm["lo"][:],
                                                                      op0=ALU.mult, op1=ALU.add),
                              reads=[r_sm["gw"], r_sm["lo"]], writes=[r_sm["lo"]])
                    thr, r_thr = sm["lo"], r_sm["lo"]
                else:
                    thr, r_thr = sm["thr0"], r_sm["thr0"]
                em.op("dve", lambda e: e.tensor_scalar(out=mb[qi][:, 0:L2], in0=sc[:, 0:L2], scalar1=thr[:, 0:1],
                                                       scalar2=NEG, op0=ALU.is_lt, op1=ALU.mult),
                      reads=[r_sc, r_thr], writes=[r_mb[qi]])

            def attend_pair(tb, pair, mb, r_mb):
                n_ts = 4 * (tb + 1)


                def mask_mm(i, lo, sps, r_sps):
                    q_first = lo // 128
                    for qq in range(q_first, 4):
                        em.op("pe", lambda e: e.matmul(sps[:, qq * 128:(qq + 1) * 128],
                                                       lhsT=mb[qq][:, i * 128:(i + 1) * 128], rhs=ident_b[:, :],
                                                       start=False, stop=(qq == 3)),
                              reads=[r_mb[qq], r_const], pwrites=[r_sps], inc=(qq == 3))

                vb = ctr["v"] % 2
                ctr["v"] += 1
                em.dma("sp", Vp[vb][:, 0:n_ts, :], dv_v[:, 0:n_ts, pair * 192:(pair + 1) * 192],
                       reads=[R["dv_d"]], writes=[r_Vp[vb]])
                for odd in range(2):
                    h = pair * 2 + odd
                    prow = odd * 64
                    hb = ctr["h"] % 2
                    ctr["h"] += 1
                    em.dma("sp", qh[hb][prow:prow + 64, :], dqT_d[h * 64:(h + 1) * 64, tb * 512:(tb + 1) * 512],
                           reads=[R["dqT_d"]], writes=[r_qh[hb]])
                    em.dma("sp", kh[hb][prow:prow + 64, 0:n_ts * 128], dkT_d[h * 64:(h + 1) * 64, 0:n_ts * 128],
                           reads=[R["dkT_d"]], writes=[r_kh[hb]])
                    attn_block(o, kh[hb], r_kh[hb], qh[hb], r_qh[hb], 0, 64, prow, Vp[vb], r_Vp[vb], odd, n_ts,
                               lambda i: max(0, (i - 4 * tb) * 128), None, mask_mm,
                               oaT_d[h * 64:(h + 1) * 64, tb * 512:(tb + 1) * 512], R["oaT_d"], store_q="pool")

            order = list(range(NTB - 1, -1, -1))
            for qi in range(4):
                prep_q(order[0], qi, mbs[0], r_mbs[0])
            for n_, tb in enumerate(order):
                for qi in range(4):
                    if n_ + 1 < NTB:
                        prep_q(order[n_ + 1], qi, mbs[(n_ + 1) % 2], r_mbs[(n_ + 1) % 2])
                    attend_pair(tb, qi, mbs[n_ % 2], r_mbs[n_ % 2])
            em.barrier()

    def ln_alloc(ph, pfx, g_d, b_d):
        o = dict(
            y=[sb(ph, f"{pfx}_y{i}", [128, D], F32) for i in range(2)], r_y=[Res() for _ in range(2)],
            xo=[sb(ph, f"{pfx}_xo{i}", [128, D], F32) for i in range(2)], r_xo=[Res() for _ in range(2)],
            xr=[sb(ph, f"{pfx}_xr{i}", [128, D], F32) for i in range(2)], r_xr=[Res() for _ in range(2)],
            st=sb(ph, f"{pfx}_st", [128, 6 * (D // 512)], F32), r_st=Res(),
            mv=sb(ph, f"{pfx}_mv", [128, 2], F32), r_mv=Res(),
            sd=sb(ph, f"{pfx}_sd", [128, 1], F32), r_sd=Res(),
            gb=sb(ph, f"{pfx}_gb", [128, D], F32), bb=sb(ph, f"{pfx}_bb", [128, D], F32), r_gb=Res(),
            xb=sb(ph, f"{pfx}_xb", [128, D], BF16), r_xb=Res(),
            tp=ps(ph, f"{pfx}_tp", [128, D], BF16), r_tp=Res(),
            stage=[sb(ph, f"{pfx}_sg{i}", [128, KT, 512], BF16) for i in range(2)], r_stage=[Res() for _ in range(2)],
            n=0)
        em.dma("sp", o["gb"][:], g_d.partition_broadcast(128), pwrites=[o["r_gb"]])
        em.dma("sp", o["bb"][:], b_d.partition_broadcast(128), pwrites=[o["r_gb"]])
        return o

    def ln_stage(o, i, halves, xres_d, store_d, r_store, xT_dst, r_xT_dst, after_fn=None):
        b = o["n"] % 2
        o["n"] += 1
        y, r_y, xo, r_xo, xr, r_xr = o["y"][b], o["r_y"][b], o["xo"][b], o["r_xo"][b], o["xr"][b], o["r_xr"][b]
        em.dma("sp", xr[:], xres_d[i * 128:(i + 1) * 128, :], reads=[R["x1_d"], R["xcur_d"]], writes=[r_xr])
        for hf, (pa, r_pa) in enumerate(halves):
            sl = slice(hf * 512, (hf + 1) * 512)
            em.op("dve", lambda e: e.scalar_tensor_tensor(out=y[:, sl], in0=xr[:, sl], scalar=alpha, in1=pa,
                                                          op0=ALU.mult, op1=ALU.add),
                  reads=[r_xr, r_pa], writes=[r_y] if hf == 0 else [], pwrites=[] if hf == 0 else [r_y])
        for c in range(D // 512):
            em.op("dve", lambda e: e.bn_stats(out=o["st"][:, c * 6:(c + 1) * 6], in_=y[:, c * 512:(c + 1) * 512]),
                  reads=[r_y], writes=[o["r_st"]] if c == 0 else [], pwrites=[] if c == 0 else [o["r_st"]])
        em.op("dve", lambda e: e.bn_aggr(out=o["mv"][:, 0:2], in_=o["st"][:, :]), reads=[o["r_st"]], writes=[o["r_mv"]])
        em.op("dve", lambda e: e.tensor_scalar(out=o["sd"][:], in0=o["mv"][:, 1:2], scalar1=LN_EPS, scalar2=None,
                                               op0=ALU.add), reads=[o["r_mv"]], writes=[o["r_sd"]])
        em.op("act", lambda e: e.activation(out=o["sd"][:], in_=o["sd"][:], func=AF.Sqrt),
              reads=[o["r_sd"]], writes=[o["r_sd"]])
        em.op("dve", lambda e: e.reciprocal(out=o["sd"][:], in_=o["sd"][:]), reads=[o["r_sd"]], writes=[o["r_sd"]])
        em.op("dve", lambda e: e.tensor_scalar(out=y[:], in0=y[:], scalar1=o["mv"][:, 0:1], scalar2=o["sd"][:, 0:1],
                                               op0=ALU.subtract, op1=ALU.mult),
              reads=[r_y, o["r_mv"], o["r_sd"]], writes=[r_y])
        em.op("pool", lambda e: e.tensor_tensor(out=xo[:], in0=y[:], in1=o["gb"][:], op=ALU.mult),
              reads=[r_y, o["r_gb"]], writes=[r_xo])
        em.op("pool", lambda e: e.tensor_tensor(out=xo[:], in0=xo[:], in1=o["bb"][:], op=ALU.add),
              reads=[r_xo, o["r_gb"]], writes=[r_xo])
        em.dma("sp", store_d[i * 128:(i + 1) * 128, :], xo[:], reads=[r_xo], pwrites=[r_store])
        if xT_dst is not None:
            tbk, qi = i // 4, i % 4
            stg, r_stg = o["stage"][tbk % 2], o["r_stage"][tbk % 2]
            emit_transposes((o["xb"], o["r_xb"], o["tp"], o["r_tp"], stg, r_stg), xo, r_xo, qi)
            if qi == 3:
                em.dma("sp", xT_dst.rearrange("(k p) t -> p k t", p=128)[:, :, tbk * 512:(tbk + 1) * 512], stg[:],
                       reads=[r_stg], pwrites=[r_xT_dst])
        if after_fn is not None:
            after_fn(i, xo, r_xo, o["xb"], o["r_xb"])

    def phase_merge(l, moe_j):
        with ExitStack() as ph:
            lo_ = ln_alloc(ph, "p4", ln_mix_g[l], ln_mix_b[l])
            Wa = sb(ph, "p4_Wa", [128, 4, D], BF16)
            Wb = sb(ph, "p4_Wb", [128, 4, D], BF16)
            Wo = sb(ph, "p4_Wo", [128, KT, D], BF16)
            r_W = Res()
            oa = [sb(ph, f"p4_oa{i}", [128, 4, 512], BF16) for i in range(2)]
            ob = [sb(ph, f"p4_ob{i}", [128, 4, 512], BF16) for i in range(2)]
            sga = [sb(ph, f"p4_sga{i}", [128, KT, 512], BF16) for i in range(2)]
            sgb = [sb(ph, f"p4_sgb{i}", [128, KT, 512], BF16) for i in range(2)]
            r_in = [Res() for _ in range(2)]
            mg = sb(ph, "p4_mg", [128, KT, 512], BF16)
            r_mg = Res()
            t1 = sb(ph, "p4_t1", [128, 512], F32)
            t2 = sb(ph, "p4_t2", [128, 512], F32)
            r_t1, r_t2 = Res(), Res()
            psa = ps(ph, "p4_psa")
            psb = ps(ph, "p4_psb")
            r_psa, r_psb = Res(), Res()
            nbm = 1 if moe_j is not None else 2
            psm = [ps(ph, f"p4_psm{i}") for i in range(nbm * (D // 512))]
            r_psm = [Res() for _ in range(nbm * (D // 512))]
            em.dma("pool", Wa[:], w_ba[l].rearrange("(k p) d -> p k d", p=128), pwrites=[r_W])
            em.dma("pool", Wb[:], w_bb[l].rearrange("(k p) d -> p k d", p=128), pwrites=[r_W])
            for kt in range(KT):
                em.dma("pool", Wo[:, kt, :], w_out[l][kt * 128:(kt + 1) * 128, :], pwrites=[r_W])
            after = None
            if moe_j is not None:
                rt = sb(ph, "p4_rt", [128, KT, NEXP], F32)
                r_rt = Res()
                em.dma("sp", rt[:], moe_router[moe_j].rearrange("(k p) e -> p k e", p=128), writes=[r_rt])
                tpf = ps(ph, "p4_tpf", [128, D], F32)
                r_tpf = Res()
                xTf = sb(ph, "p4_xTf", [128, D], F32)
                r_xTf = Res()
                pcs = ps(ph, "p4_pcs")
                r_pcs = Res()
                zt = sb(ph, "p4_zt", [128, 2 * D], BF16)
                r_zt = Res()
                r_zf = Res()
                em.op("pool", lambda e: e.memset(zt[:], 0.0), writes=[r_zt])
                for r0 in range(0, NEXP * CAP, 256):
                    em.dma("sp", xe_d[r0:r0 + 256, :].rearrange("(p a) d -> p (a d)", a=2), zt[:], reads=[r_zt],
                           pwrites=[R["xe_d"], r_zf])
                mall = sb(ph, "p4_mall", [128, NTQ, NEXP], F32)
                r_mall = Res()
                gi = sb(ph, "p4_gi", [128, NTQ, 2], F32)
                ii = sb(ph, "p4_ii", [128, NTQ, 2], I32)
                r_gi, r_ii = Res(), Res()
                eoff_i = sb(ph, "p4_eoffi", [128, NEXP], I32)
                eoff = sb(ph, "p4_eoff", [128, NEXP], F32)
                r_eoff = Res()
                em.op("pool", lambda e: e.iota(eoff_i[:], pattern=[[CAP, NEXP]], base=0, channel_multiplier=0),
                      writes=[r_eoff])
                em.op("dve", lambda e: e.tensor_copy(out=eoff[:], in_=eoff_i[:]), reads=[r_eoff], writes=[r_eoff])
                s8 = {n: sb(ph, "p4_" + n, [128, NEXP], F32) for n in ("lg", "is1", "l2", "is2", "pos", "v", "ov", "tmp")}
                s1 = {n: sb(ph, "p4_" + n, [128, 1], F32) for n in ("m1", "m2", "d")}
                r_s = {n: Res() for n in list(s8) + list(s1)}

                def after(i, xo, r_xo, xb, r_xb):
                    for kt in range(KT):
                        em.op("pe", lambda e: e.transpose(tpf[:, kt * 128:(kt + 1) * 128],
                                                          xo[:, kt * 128:(kt + 1) * 128], ident_f[:]),
                              reads=[r_xo, r_const], writes=[r_tpf] if kt == 0 else [],
                              pwrites=[] if kt == 0 else [r_tpf], inc=(kt == KT - 1))
                    em.op("act", lambda e: e.activation(out=xTf[:], in_=tpf[:], func=AF.Copy),
                          reads=[r_tpf], writes=[r_xTf])
                    for kt in range(KT):
                        em.op("pe", lambda e: e.matmul(pcs[:, 0:NEXP], lhsT=xTf[:, kt * 128:(kt + 1) * 128],
                                                       rhs=rt[:, kt, :], start=(kt == 0), stop=(kt == KT - 1)),
                              reads=[r_xTf, r_rt], writes=[r_pcs] if kt == 0 else [],
                              pwrites=[] if kt == 0 else [r_pcs], inc=(kt == KT - 1))
                    em.op("dve", lambda e: e.tensor_copy(out=s8["lg"][:], in_=pcs[:, 0:NEXP]),
                          reads=[r_pcs], writes=[r_s["lg"]])
                    em.op("dve", lambda e: e.tensor_reduce(out=s1["m1"][:], in_=s8["lg"][:], axis=AX.X, op=ALU.max),
                          reads=[r_s["lg"]], writes=[r_s["m1"]])
                    em.op("dve", lambda e: e.tensor_scalar(out=s8["is1"][:], in0=s8["lg"][:], scalar1=s1["m1"][:, 0:1],
                                                           scalar2=None, op0=ALU.is_equal),
                          reads=[r_s["lg"], r_s["m1"]], writes=[r_s["is1"]])
                    em.op("dve", lambda e: e.scalar_tensor_tensor(out=s8["l2"][:], in0=s8["is1"][:], scalar=-1e30,
                                                                  in1=s8["lg"][:], op0=ALU.mult, op1=ALU.add),
                          reads=[r_s["is1"], r_s["lg"]], writes=[r_s["l2"]])
                    em.op("dve", lambda e: e.tensor_reduce(out=s1["m2"][:], in_=s8["l2"][:], axis=AX.X, op=ALU.max),
                          reads=[r_s["l2"]], writes=[r_s["m2"]])
                    em.op("dve", lambda e: e.tensor_scalar(out=s8["is2"][:], in0=s8["l2"][:], scalar1=s1["m2"][:, 0:1],
                                                           scalar2=None, op0=ALU.is_equal),
                          reads=[r_s["l2"], r_s["m2"]], writes=[r_s["is2"]])
                    em.op("dve", lambda e: e.tensor_tensor(out=s1["d"][:], in0=s1["m2"][:], in1=s1["m1"][:],
                                                           op=ALU.subtract),
                          reads=[r_s["m1"], r_s["m2"]], writes=[r_s["d"]])
                    em.op("act", lambda e: e.activation(out=gi[:, i, 1:2], in_=s1["d"][:], func=AF.Sigmoid),
                          reads=[r_s["d"]], pwrites=[r_gi])
                    em.op("dve", lambda e: e.tensor_scalar(out=gi[:, i, 0:1], in0=gi[:, i, 1:2], scalar1=-1.0, scalar2=1.0,
                                                           op0=ALU.mult, op1=ALU.add),
                          reads=[r_gi], pwrites=[r_gi])
                    em.op("dve", lambda e: e.tensor_tensor(out=mall[:, i, :], in0=s8["is1"][:], in1=s8["is2"][:],
                                                           op=ALU.add),
                          reads=[r_s["is1"], r_s["is2"]], pwrites=[r_mall])
                    for j in range(i + 1):
                        em.op("pe", lambda e: e.matmul(pcs[:, 8:8 + NEXP], lhsT=(tri_f if j == i else ones_f)[:, :],
                                                       rhs=mall[:, j, :], start=(j == 0), stop=(j == i)),
                              reads=[r_mall, r_const], writes=[r_pcs] if j == 0 else [],
                              pwrites=[] if j == 0 else [r_pcs], inc=(j == i))
                    em.op("dve", lambda e: e.tensor_scalar(out=s8["pos"][:], in0=pcs[:, 8:8 + NEXP], scalar1=-1.0,
                                                           scalar2=None, op0=ALU.add),
                          reads=[r_pcs], writes=[r_s["pos"]])
                    em.op("dve", lambda e: e.tensor_scalar(out=s8["ov"][:], in0=s8["pos"][:], scalar1=float(CAP),
                                                           scalar2=1e6, op0=ALU.is_ge, op1=ALU.mult),
                          reads=[r_s["pos"]], writes=[r_s["ov"]])
                    em.op("dve", lambda e: e.tensor_tensor(out=s8["v"][:], in0=s8["pos"][:], in1=eoff[:], op=ALU.add),
                          reads=[r_s["pos"], r_eoff], writes=[r_s["v"]])
                    em.op("dve", lambda e: e.tensor_tensor(out=s8["v"][:], in0=s8["v"][:], in1=s8["ov"][:], op=ALU.add),
                          reads=[r_s["v"], r_s["ov"]], writes=[r_s["v"]])
                    for w_, nm in enumerate(("is1", "is2")):
                        em.op("dve", lambda e: e.tensor_tensor(out=s8["tmp"][:], in0=s8[nm][:], in1=s8["v"][:],
                                                               op=ALU.mult),
                              reads=[r_s[nm], r_s["v"]], writes=[r_s["tmp"]])
                        em.op("dve", lambda e: e.tensor_reduce(out=s1["m1"][:], in_=s8["tmp"][:], axis=AX.X, op=ALU.add),
                              reads=[r_s["tmp"]], writes=[r_s["m1"]])
                        em.op("dve", lambda e: e.tensor_copy(out=ii[:, i, w_:w_ + 1], in_=s1["m1"][:]),
                              reads=[r_s["m1"]], pwrites=[r_ii])
                        em.dma_fn("pool", lambda g: g.indirect_dma_start(
                            out=xe_d[:, :], out_offset=bass.IndirectOffsetOnAxis(ap=ii[:, i, w_:w_ + 1], axis=0),
                            in_=xb[:, :], in_offset=None, bounds_check=bc_reg[0], oob_is_err=False),
                            reads=[r_ii, r_xb, r_zf], pwrites=[R["xe_d"]])

            xres_d = x_in if l == 0 else xcur_d

            def load_in(tb):
                b = tb % 2
                tsl = slice(tb * 512, (tb + 1) * 512)
                em.dma("sp", oa[b][:], oaT_d.rearrange("(k p) t -> p k t", p=128)[:, :, tsl], reads=[R["oaT_d"]],
                       writes=[r_in[b]])
                em.dma("sp", ob[b][:], obT_d.rearrange("(k p) t -> p k t", p=128)[:, :, tsl], reads=[R["obT_d"]],
                       pwrites=[r_in[b]])
                em.dma("sp", sga[b][:], sgaT_d.rearrange("(k p) t -> p k t", p=128)[:, :, tsl], reads=[R["sgaT_d"]],
                       pwrites=[r_in[b]])
                em.dma("sp", sgb[b][:], sgbT_d.rearrange("(k p) t -> p k t", p=128)[:, :, tsl], reads=[R["sgbT_d"]],
                       pwrites=[r_in[b]])

            load_in(0)
            for tb in range(NTB):
                if tb + 1 < NTB:
                    load_in(tb + 1)
                b = tb % 2
                for dm in range(KT):
                    for k in range(4):
                        em.op("pe", lambda e: e.matmul(psa[:, :], lhsT=Wa[:, k, dm * 128:(dm + 1) * 128],
                                                       rhs=oa[b][:, k, :], start=(k == 0), stop=(k == 3)),
                              reads=[r_W, r_in[b]], writes=[r_psa] if k == 0 else [], pwrites=[] if k == 0 else [r_psa],
                              inc=(k == 3))
                    for k in range(4):
                        em.op("pe", lambda e: e.matmul(psb[:, :], lhsT=Wb[:, k, dm * 128:(dm + 1) * 128],
                                                       rhs=ob[b][:, k, :], start=(k == 0), stop=(k == 3)),
                              reads=[r_W, r_in[b]], writes=[r_psb] if k == 0 else [], pwrites=[] if k == 0 else [r_psb],
                              inc=(k == 3))
                    em.op("dve", lambda e: e.tensor_tensor(out=t1[:], in0=psa[:, :], in1=sga[b][:, dm, :], op=ALU.mult),
                          reads=[r_psa, r_in[b]], writes=[r_t1])
                    em.op("dve", lambda e: e.tensor_tensor(out=t2[:], in0=psb[:, :], in1=sgb[b][:, dm, :], op=ALU.mult),
                          reads=[r_psb, r_in[b]], writes=[r_t2])
                    em.op("pool", lambda e: e.tensor_tensor(out=mg[:, dm, :], in0=t1[:], in1=t2[:], op=ALU.add),
                          reads=[r_t1, r_t2], writes=[r_mg] if dm == 0 else [], pwrites=[] if dm == 0 else [r_mg])
                for qi in range(4):
                    i = tb * 4 + qi
                    halves = []
                    for hf in range(D // 512):
                        pi = (i % nbm) * (D // 512) + hf
                        for k in range(KT):
                            em.op("pe", lambda e: e.matmul(psm[pi][:, :], lhsT=mg[:, k, qi * 128:(qi + 1) * 128],
                                                           rhs=Wo[:, k, hf * 512:(hf + 1) * 512], start=(k == 0),
                                                           stop=(k == KT - 1)),
                                  reads=[r_mg, r_W], writes=[r_psm[pi]] if k == 0 else [],
                                  pwrites=[] if k == 0 else [r_psm[pi]], inc=(k == KT - 1))
                        halves.append((psm[pi][:, :], r_psm[pi]))
                    ln_stage(lo_, i, halves, xres_d, x1_d, R["x1_d"], x1T_d, R["x1T_d"], after)
            if moe_j is not None:
                em.dma("sp", moe_g_d[:, :], gi[:].rearrange("p a b -> p (a b)"), reads=[r_gi], pwrites=[R["moe_d"]])
                em.dma("sp", moe_i_d[:, :], ii[:].rearrange("p a b -> p (a b)"), reads=[r_ii], pwrites=[R["moe_d"]])
            em.barrier()

    def phase_ffn(l, experts, gated, final):
        with ExitStack() as ph:
            lo_ = ln_alloc(ph, "p5", ln_ffn_g[l], ln_ffn_b[l])
            NQ = TBF // 128
            x1T = sb(ph, "p5_xT", [128, KT, TBF], BF16)
            r_x1T = Res()
            yacc = sb(ph, "p5_yacc", [128, NQ, D], F32)
            r_yacc = Res()
            hT = [sb(ph, f"p5_hT{i}", [128, 4, TBF], BF16) for i in range(2)]
            r_hT = [Res() for _ in range(2)]
            Wg = [sb(ph, f"p5_Wg{i}", [128, KT, 512], BF16) for i in range(2)]
            Wu = [sb(ph, f"p5_Wu{i}", [128, KT, 512], BF16) for i in range(2)]
            Wd = [sb(ph, f"p5_Wd{i}", [128, 4, D], BF16) for i in range(2)]
            r_Wc = [Res() for _ in range(2)]
            gbc = sb(ph, "p5_gbc", [128, TBF], F32)
            r_gbc = Res()
            sg = [sb(ph, f"p5_sg{i}", [128, 512], F32) for i in range(2)]
            r_sg = [Res() for _ in range(2)]
            tm = [sb(ph, f"p5_tm{i}", [128, 512], F32) for i in range(2)]
            r_tm = [Res() for _ in range(2)]
            psg = [ps(ph, f"p5_pg{i}") for i in range(2)]
            psu = [ps(ph, f"p5_pu{i}") for i in range(2)]
            psd = [ps(ph, f"p5_pd{i}") for i in range(2)]
            r_psg = [Res() for _ in range(2)]
            r_psu = [Res() for _ in range(2)]
            r_psd = [Res() for _ in range(2)]
            NCH = FT // 4
            work = [(tbf, ei, c) for tbf in range(NTBF) for ei in range(len(experts)) for c in range(NCH)]
            ctr = {"p": 0, "d": 0}

            def load_chunk(n):
                tbf, ei, c = work[n]
                wg_d, wu_d, wd_d = experts[ei]
                b = n % 2
                em.dma("pool", Wg[b][:], wg_d[:, c * 512:(c + 1) * 512].rearrange("(k p) f -> p k f", p=128),
                       writes=[r_Wc[b]])
                em.dma("pool", Wu[b][:], wu_d[:, c * 512:(c + 1) * 512].rearrange("(k p) f -> p k f", p=128),
                       pwrites=[r_Wc[b]])
                em.dma("pool", Wd[b][:], wd_d[c * 512:(c + 1) * 512, :].rearrange("(k p) d -> p k d", p=128),
                       pwrites=[r_Wc[b]])

            load_chunk(0)
            for n, (tbf, ei, c) in enumerate(work):
                if n + 1 < len(work):
                    load_chunk(n + 1)
                b = n % 2
                t0 = tbf * TBF
                if ei == 0 and c == 0:
                    em.dma("sp", x1T[:], x1T_d.rearrange("(k p) t -> p k t", p=128)[:, :, t0:t0 + TBF],
                           reads=[R["x1T_d"]], writes=[r_x1T])
                if gated and c == 0:
                    em.dma("sp", gbc[:], gateT_d[ei, t0:t0 + TBF].partition_broadcast(128), reads=[R["gateT_d"]],
                           writes=[r_gbc])
                for f4 in range(4):
                    for tsub in range(TBF // 512):
                        p = ctr["p"] % 2
                        ctr["p"] += 1
                        tsl = slice(tsub * 512, (tsub + 1) * 512)
                        for kt in range(KT):
                            em.op("pe", lambda e: e.matmul(psg[p][:, :], lhsT=Wg[b][:, kt, f4 * 128:(f4 + 1) * 128],
                                                           rhs=x1T[:, kt, tsl], start=(kt == 0), stop=(kt == KT - 1)),
                                  reads=[r_Wc[b], r_x1T], writes=[r_psg[p]] if kt == 0 else [],
                                  pwrites=[] if kt == 0 else [r_psg[p]], inc=(kt == KT - 1))
                        for kt in range(KT):
                            em.op("pe", lambda e: e.matmul(psu[p][:, :], lhsT=Wu[b][:, kt, f4 * 128:(f4 + 1) * 128],
                                                           rhs=x1T[:, kt, tsl], start=(kt == 0), stop=(kt == KT - 1)),
                                  reads=[r_Wc[b], r_x1T], writes=[r_psu[p]] if kt == 0 else [],
                                  pwrites=[] if kt == 0 else [r_psu[p]], inc=(kt == KT - 1))
                        em.op("act", lambda e: e.activation(out=sg[p][:], in_=psg[p][:, :], func=AF.Silu),
                              reads=[r_psg[p]], writes=[r_sg[p]])
                        first_h = (f4 == 0 and tsub == 0)
                        if gated:
                            em.op("dve", lambda e: e.tensor_tensor(out=tm[p][:], in0=psu[p][:, :], in1=sg[p][:],
                                                                   op=ALU.mult),
                                  reads=[r_psu[p], r_sg[p]], writes=[r_tm[p]])
                            em.op("pool", lambda e: e.tensor_tensor(out=hT[b][:, f4, tsl], in0=tm[p][:],
                                                                    in1=gbc[:, tsl], op=ALU.mult),
                                  reads=[r_tm[p], r_gbc], writes=[r_hT[b]] if first_h else [],
                                  pwrites=[] if first_h else [r_hT[b]])
                        else:
                            em.op("dve", lambda e: e.tensor_tensor(out=hT[b][:, f4, tsl], in0=psu[p][:, :], in1=sg[p][:],
                                                                   op=ALU.mult),
                                  reads=[r_psu[p], r_sg[p]], writes=[r_hT[b]] if first_h else [],
                                  pwrites=[] if first_h else [r_hT[b]])
                first_acc = (ei == 0 and c == 0)
                for q in range(NQ):
                    for hf in range(D // 512):
                        d = ctr["d"] % 2
                        ctr["d"] += 1
                        for f4 in range(4):
                            em.op("pe", lambda e: e.matmul(psd[d][:, :], lhsT=hT[b][:, f4, q * 128:(q + 1) * 128],
                                                           rhs=Wd[b][:, f4, hf * 512:(hf + 1) * 512], start=(f4 == 0),
                                                           stop=(f4 == 3)),
                                  reads=[r_hT[b], r_Wc[b]], writes=[r_psd[d]] if f4 == 0 else [],
                                  pwrites=[] if f4 == 0 else [r_psd[d]], inc=(f4 == 3))
                        ysl = yacc[:, q, hf * 512:(hf + 1) * 512]
                        if first_acc:
                            em.op("dve", lambda e: e.tensor_copy(out=ysl, in_=psd[d][:, :]),
                                  reads=[r_psd[d]], writes=[r_yacc] if (q == 0 and hf == 0) else [],
                                  pwrites=[] if (q == 0 and hf == 0) else [r_yacc])
                        else:
                            em.op("dve", lambda e: e.tensor_tensor(out=ysl, in0=ysl, in1=psd[d][:, :], op=ALU.add),
                                  reads=[r_psd[d], r_yacc], pwrites=[r_yacc])
                if ei == len(experts) - 1 and c == NCH - 1:
                    for q in range(NQ):
                        i = tbf * NQ + q
                        halves = [(yacc[:, q, hf * 512:(hf + 1) * 512], r_yacc) for hf in range(D // 512)]
                        if final:
                            ln_stage(lo_, i, halves, x1_d, out_d, R["out_d"], None, None)
                        else:
                            ln_stage(lo_, i, halves, x1_d, xcur_d, R["xcur_d"], xT_d, R["xT_d"])
            em.barrier()

    def phase_moe(l, j, final):
        NQ = CAP // 128
        NSUB = CAP // 512
        NCH = FT // 4
        with ExitStack() as ph:
            xeT = sb(ph, "pm_xeT", [128, KT, CAP], BF16)
            r_xeT = Res()
            yacc = sb(ph, "pm_yacc", [128, NQ, D], F32)
            r_yacc = Res()
            hT = [sb(ph, f"pm_hT{i}", [128, 4, CAP], BF16) for i in range(2)]
            r_hT = [Res() for _ in range(2)]
            Wg = [sb(ph, f"pm_Wg{i}", [128, KT, 512], BF16) for i in range(2)]
            Wu = [sb(ph, f"pm_Wu{i}", [128, KT, 512], BF16) for i in range(2)]
            Wd = [sb(ph, f"pm_Wd{i}", [128, 4, D], BF16) for i in range(2)]
            r_Wc = [Res() for _ in range(2)]
            sg = [sb(ph, f"pm_sg{i}", [128, 512], F32) for i in range(2)]
            r_sg = [Res() for _ in range(2)]
            xr = [sb(ph, f"pm_xr{i}", [128, D], BF16) for i in range(2)]
            r_xr = [Res() for _ in range(2)]
            tp = ps(ph, "pm_tp", [128, D], BF16)
            r_tp = Res()
            psg = [ps(ph, f"pm_pg{i}") for i in range(2)]
            psu = [ps(ph, f"pm_pu{i}") for i in range(2)]
            psd = [ps(ph, f"pm_pd{i}") for i in range(2)]
            r_psg = [Res() for _ in range(2)]
            r_psu = [Res() for _ in range(2)]
            r_psd = [Res() for _ in range(2)]
            work = [(e, c) for e in range(NEXP) for c in range(NCH)]
            ctr = {"p": 0, "d": 0, "x": 0}

            def load_chunk(n):
                e, c = work[n]
                b = n % 2
                em.dma("pool", Wg[b][:], moe_wg[j][e][:, c * 512:(c + 1) * 512].rearrange("(k p) f -> p k f", p=128),
                       writes=[r_Wc[b]])
                em.dma("pool", Wu[b][:], moe_wu[j][e][:, c * 512:(c + 1) * 512].rearrange("(k p) f -> p k f", p=128),
                       pwrites=[r_Wc[b]])
                em.dma("pool", Wd[b][:], moe_wd[j][e][c * 512:(c + 1) * 512, :].rearrange("(k p) d -> p k d", p=128),
                       pwrites=[r_Wc[b]])

            load_chunk(0)
            for n, (e_, c) in enumerate(work):
                if n + 1 < len(work):
                    load_chunk(n + 1)
                b = n % 2
                if c == 0:
                    for q in range(NQ):
                        xb_ = ctr["x"] % 2
                        ctr["x"] += 1
                        r0 = e_ * CAP + q * 128
                        em.dma("sp", xr[xb_][:], xe_d[r0:r0 + 128, :], reads=[R["xe_d"]], writes=[r_xr[xb_]])
                        for kt in range(KT):
                            em.op("pe", lambda e: e.transpose(tp[:, kt * 128:(kt + 1) * 128],
                                                              xr[xb_][:, kt * 128:(kt + 1) * 128], ident_b[:]),
                                  reads=[r_xr[xb_], r_const], writes=[r_tp] if kt == 0 else [],
                                  pwrites=[] if kt == 0 else [r_tp], inc=(kt == KT - 1))
                        em.op("dve", lambda e: e.tensor_copy(out=xeT[:, :, q * 128:(q + 1) * 128],
                                                             in_=tp[:].rearrange("p (k c) -> p k c", c=128)),
                              reads=[r_tp], writes=[r_xeT] if q == 0 else [], pwrites=[] if q == 0 else [r_xeT])
                for f4 in range(4):
                    for tsub in range(NSUB):
                        p = ctr["p"] % 2
                        ctr["p"] += 1
                        tsl = slice(tsub * 512, (tsub + 1) * 512)
                        for kt in range(KT):
                            em.op("pe", lambda e: e.matmul(psg[p][:, :], lhsT=Wg[b][:, kt, f4 * 128:(f4 + 1) * 128],
                                                           rhs=xeT[:, kt, tsl], start=(kt == 0), stop=(kt == KT - 1)),
                                  reads=[r_Wc[b], r_xeT], writes=[r_psg[p]] if kt == 0 else [],
                                  pwrites=[] if kt == 0 else [r_psg[p]], inc=(kt == KT - 1))
                        for kt in range(KT):
                            em.op("pe", lambda e: e.matmul(psu[p][:, :], lhsT=Wu[b][:, kt, f4 * 128:(f4 + 1) * 128],
                                                           rhs=xeT[:, kt, tsl], start=(kt == 0), stop=(kt == KT - 1)),
                                  reads=[r_Wc[b], r_xeT], writes=[r_psu[p]] if kt == 0 else [],
                                  pwrites=[] if kt == 0 else [r_psu[p]], inc=(kt == KT - 1))
                        em.op("act", lambda e: e.activation(out=sg[p][:], in_=psg[p][:, :], func=AF.Silu),
                              reads=[r_psg[p]], writes=[r_sg[p]])
                        first_h = (f4 == 0 and tsub == 0)
                        em.op("dve", lambda e: e.tensor_tensor(out=hT[b][:, f4, tsl], in0=psu[p][:, :], in1=sg[p][:],
                                                               op=ALU.mult),
                              reads=[r_psu[p], r_sg[p]], writes=[r_hT[b]] if first_h else [],
                              pwrites=[] if first_h else [r_hT[b]])
                for q in range(NQ):
                    for hf in range(D // 512):
                        d = ctr["d"] % 2
                        ctr["d"] += 1
                        for f4 in range(4):
                            em.op("pe", lambda e: e.matmul(psd[d][:, :], lhsT=hT[b][:, f4, q * 128:(q + 1) * 128],
                                                           rhs=Wd[b][:, f4, hf * 512:(hf + 1) * 512], start=(f4 == 0),
                                                           stop=(f4 == 3)),
                                  reads=[r_hT[b], r_Wc[b]], writes=[r_psd[d]] if f4 == 0 else [],
                                  pwrites=[] if f4 == 0 else [r_psd[d]], inc=(f4 == 3))
                        ysl = yacc[:, q, hf * 512:(hf + 1) * 512]
                        if c == 0:
                            em.op("act", lambda e: e.activation(out=ysl, in_=psd[d][:, :], func=AF.Copy),
                                  reads=[r_psd[d]], writes=[r_yacc] if (q == 0 and hf == 0) else [],
                                  pwrites=[] if (q == 0 and hf == 0) else [r_yacc])
                        else:
                            em.op("dve", lambda e: e.tensor_tensor(out=ysl, in0=ysl, in1=psd[d][:, :], op=ALU.add),
                                  reads=[r_psd[d], r_yacc], pwrites=[r_yacc])
                if c == NCH - 1:
                    em.dma("sp", ye_d[e_ * CAP:(e_ + 1) * CAP, :].rearrange("(q p) d -> p q d", p=128), yacc[:],
                           reads=[r_yacc], pwrites=[R["ye_d"]])
            em.barrier()
        with ExitStack() as ph:
            lo_ = ln_alloc(ph, "pc", ln_ffn_g[l], ln_ffn_b[l])
            gi = sb(ph, "pc_gi", [128, NTQ, 2], F32)
            ii = sb(ph, "pc_ii", [128, NTQ, 2], I32)
            r_gi = Res()
            em.dma("sp", gi[:].rearrange("p a b -> p (a b)"), moe_g_d[:, :], reads=[R["moe_d"]], pwrites=[r_gi])
            em.dma("sp", ii[:].rearrange("p a b -> p (a b)"), moe_i_d[:, :], reads=[R["moe_d"]], pwrites=[r_gi])
            y1 = [sb(ph, f"pc_y1{i}", [128, D], F32) for i in range(2)]
            y2 = [sb(ph, f"pc_y2{i}", [128, D], F32) for i in range(2)]
            r_y1 = [Res() for _ in range(2)]
            r_y2 = [Res() for _ in range(2)]
            for i in range(2):
                em.op("pool", lambda e: e.memset(y1[i][:], 0.0), writes=[r_y1[i]])
                em.op("pool", lambda e: e.memset(y2[i][:], 0.0), writes=[r_y2[i]])
            def gather(i):
                b = i % 2
                em.dma_fn("pool", lambda g: g.indirect_dma_start(
                    out=y1[b][:, :], out_offset=None, in_=ye_d[:, :],
                    in_offset=bass.IndirectOffsetOnAxis(ap=ii[:, i, 0:1], axis=0),
                    bounds_check=bc_reg[0], oob_is_err=False),
                    reads=[R["ye_d"], r_gi], writes=[r_y1[b]])
                em.dma_fn("pool", lambda g: g.indirect_dma_start(
                    out=y2[b][:, :], out_offset=None, in_=ye_d[:, :],
                    in_offset=bass.IndirectOffsetOnAxis(ap=ii[:, i, 1:2], axis=0),
                    bounds_check=bc_reg[0], oob_is_err=False),
                    reads=[R["ye_d"], r_gi], writes=[r_y2[b]])

            gather(0)
            for i in range(NTQ):
                b = i % 2
                if i + 1 < NTQ:
                    gather(i + 1)
                em.op("dve", lambda e: e.tensor_scalar(out=y1[b][:], in0=y1[b][:], scalar1=gi[:, i, 0:1], scalar2=None,
                                                       op0=ALU.mult), reads=[r_y1[b], r_gi], writes=[r_y1[b]])
                em.op("dve", lambda e: e.scalar_tensor_tensor(out=y1[b][:], in0=y2[b][:], scalar=gi[:, i, 1:2],
                                                              in1=y1[b][:], op0=ALU.mult, op1=ALU.add),
                      reads=[r_y1[b], r_y2[b], r_gi], writes=[r_y1[b]])
                halves = [(y1[b][:, hf * 512:(hf + 1) * 512], r_y1[b]) for hf in range(D // 512)]
                if final:
                    ln_stage(lo_, i, halves, x1_d, out_d, R["out_d"], None, None)
                else:
                    ln_stage(lo_, i, halves, x1_d, xcur_d, R["xcur_d"], xT_d, R["xT_d"])
            em.barrier()

    bc_reg[0] = nc.gpsimd.to_reg(NEXP * CAP - 1)
    phase_tables()
    phase_x0()
    for l in range(DEPTH):
        j = l // 2
        is_moe = (l % 2 == 1)
        phase_proj(l)
        phase_fox()
        phase_dsa()
        phase_merge(l, j if is_moe else None)
        if is_moe:
            phase_moe(l, j, l == DEPTH - 1)
        else:
            phase_ffn(l, [(ffn_wg[j], ffn_wu[j], ffn_wd[j])], False, l == DEPTH - 1)
    top.close()
    return nc, em


_INPUT_ORDER = ["x", "positions", "w_in", "b_forget", "w_branch_a", "w_branch_b", "w_out", "ln_mix_g", "ln_mix_b",
                "ln_ffn_g", "ln_ffn_b", "ffn_w_gate", "ffn_w_up", "ffn_w_down", "moe_router", "moe_w_gate",
                "moe_w_up", "moe_w_down"]


def kernel(**inputs):
    x = np.asarray(inputs["x"])
    B, T, D = x.shape
    depth = int(np.asarray(inputs["w_in"]).shape[0])
    DFF = int(np.asarray(inputs["ffn_w_gate"]).shape[-1])
    nc, em = build_program(T, D, DFF, depth)
    shared = {k: np.ascontiguousarray(np.asarray(inputs[k])) for k in _INPUT_ORDER if k not in ("x", "positions")}
    in_maps = []
    for b in range(B):
        m = dict(shared)
        m["x"] = np.ascontiguousarray(x[b])
        m["positions"] = np.ascontiguousarray(np.asarray(inputs["positions"])[b:b + 1]).astype(np.int32)
        in_maps.append(m)
    res = run_bass_kernel_spmd(nc, in_maps, core_ids=list(range(B)))
    return np.stack([np.asarray(r["out"]) for r in res.results], axis=0).astype(np.float32)
```
